# Optimizing a Trainium2 kernel written in Bass

```python
import jax
import jax.numpy as jnp
from jax import lax
import numpy as np

D_MODEL = 1024
BATCH = 8
SEQ = 2048
DEPTH = 2

GRID_W = 64
CTX_LEN = 256
D_LRU = 512
N_LRU_HEADS = 8
LRU_HEAD_DIM = D_LRU // N_LRU_HEADS
CONV_W = 4
CONV_LEFT = 2
LRU_C = 8.0
D_POOL = 512
POOL_WINDOWS = (2, 4, 8, 16)
N_POOL_GROUPS = len(POOL_WINDOWS)
POOL_GROUP_DIM = D_POOL // N_POOL_GROUPS
D_IN = 2 * D_LRU + D_POOL
D_MIX = D_LRU + D_POOL
N_EXPERTS = 32
TOP_K = 4
D_EXPERT = D_MODEL
SWIGLU_LIMIT = 7.0
SWIGLU_ALPHA = 1.702
LN_EPS = 1e-5

kernel_name = 'hybrid_lru_pool_moe_dit'


def layer_norm(x, g=None, b=None):
    xf = x.astype(jnp.float32)
    mu = jnp.mean(xf, axis=-1, keepdims=True)
    var = jnp.mean(jnp.square(xf - mu), axis=-1, keepdims=True)
    y = (xf - mu) * lax.rsqrt(var + LN_EPS)
    if g is not None:
        y = y * g.astype(jnp.float32) + b.astype(jnp.float32)
    return y.astype(x.dtype)


def modulate(x, shift, scale):
    return x * (1 + scale) + shift


def grid_sincos(rows, cols, d):
    quarter = d // 4
    omega = 1.0 / (10000.0 ** (jnp.arange(quarter, dtype=jnp.float32) / quarter))

    def emb1d(n):
        ang = jnp.arange(n, dtype=jnp.float32)[:, None] * omega[None, :]
        return jnp.concatenate([jnp.sin(ang), jnp.cos(ang)], axis=-1)

    er = jnp.broadcast_to(emb1d(rows)[:, None, :], (rows, cols, d // 2))
    ec = jnp.broadcast_to(emb1d(cols)[None, :, :], (rows, cols, d // 2))
    return jnp.concatenate([er, ec], axis=-1).reshape(rows * cols, d)


def conv_centred(u, w, b):
    L = u.shape[1]
    up = jnp.pad(u, ((0, 0), (CONV_LEFT, CONV_W - 1 - CONV_LEFT), (0, 0)))
    y = b
    for k in range(CONV_W):
        y = y + up[:, k:k + L, :] * w[k]
    return y


def block_diag(u, w, b):
    B, L, _ = u.shape
    uh = u.reshape(B, L, N_LRU_HEADS, LRU_HEAD_DIM)
    return jnp.einsum('blhi,hij->blhj', uh, w).reshape(B, L, D_LRU) + b


def lru_coeffs(u, wa, ba, wx, bx, lam):
    uf = u.astype(jnp.float32)
    r = jax.nn.sigmoid(block_diag(uf, wa.astype(jnp.float32), ba.astype(jnp.float32)))
    i = jax.nn.sigmoid(block_diag(uf, wx.astype(jnp.float32), bx.astype(jnp.float32)))
    log_a = -LRU_C * r * jax.nn.softplus(-lam.astype(jnp.float32))
    a = jnp.exp(log_a)
    bterm = jnp.sqrt(-jnp.expm1(2.0 * log_a)) * (i * uf)
    return a, bterm


def _combine(e1, e2):
    a1, b1 = e1
    a2, b2 = e2
    return a1 * a2, a2 * b1 + b2


def linear_scan(a, bterm, h0, reverse):
    edge = -1 if reverse else 0
    bterm = bterm.at[:, edge].add(a[:, edge] * h0)
    _, h = lax.associative_scan(_combine, (a, bterm), reverse=reverse, axis=1)
    return h


def rglru_bidir(uc, ul, ga_w, ga_b, gx_w, gx_b, lam):
    hc_dirs = []
    hl_sum = None
    for d, rev in enumerate((False, True)):
        a, bt = lru_coeffs(uc, ga_w[d], ga_b[d], gx_w[d], gx_b[d], lam[d])
        h_c = linear_scan(a, bt, jnp.zeros_like(a[:, 0]), rev)
        h_end = h_c[:, 0] if rev else h_c[:, -1]
        a, bt = lru_coeffs(ul, ga_w[d], ga_b[d], gx_w[d], gx_b[d], lam[d])
        h_l = linear_scan(a, bt, h_end, rev)
        hc_dirs.append(h_c)
        hl_sum = h_l if hl_sum is None else hl_sum + h_l
    return hc_dirs, hl_sum


def multiscale_pool(u, w, b, scale, grid_w):
    B, L, _ = u.shape
    t = jnp.arange(L)
    if grid_w is None:
        seg_lo, seg_hi = 0, L
    else:
        seg_lo = (t // grid_w) * grid_w
        seg_hi = seg_lo + grid_w
    uf = u.astype(jnp.float32)
    prefix = jnp.pad(jnp.cumsum(uf, axis=1), ((0, 0), (1, 0), (0, 0)))
    diffs = []
    for g, win in enumerate(POOL_WINDOWS):
        lo = jnp.maximum(t - win // 2, seg_lo)
        hi = jnp.minimum(t + win // 2, seg_hi)
        sl = slice(g * POOL_GROUP_DIM, (g + 1) * POOL_GROUP_DIM)
        pg = prefix[:, :, sl]
        cnt = (hi - lo).astype(jnp.float32)[None, :, None]
        diffs.append((pg[:, hi] - pg[:, lo]) / cnt - uf[:, :, sl])
    dmat = jnp.stack(diffs, axis=2)
    y = jnp.einsum('blgi,gij->blgj', dmat, w.astype(jnp.float32)).reshape(B, L, D_POOL)
    y = (y + b.astype(jnp.float32)) * scale.astype(jnp.float32)
    return y.astype(u.dtype)


def token_mixer(hl, hc, w_in, conv_w, conv_b, ga_w, ga_b, gx_w, gx_b, lam,
                pool_w, pool_b, pool_scale, w_out, with_ctx_out):
    zl = hl @ w_in
    zc = hc @ w_in
    xr_l, y_l, xp_l = jnp.split(zl, [D_LRU, 2 * D_LRU], axis=-1)
    xr_c, y_c, xp_c = jnp.split(zc, [D_LRU, 2 * D_LRU], axis=-1)
    ul = conv_centred(xr_l, conv_w, conv_b)
    uc = conv_centred(xr_c, conv_w, conv_b)
    hc_dirs, hr_l = rglru_bidir(uc, ul, ga_w, ga_b, gx_w, gx_b, lam)
    lat = jnp.concatenate(
        [hr_l.astype(hl.dtype) * jax.nn.gelu(y_l),
         multiscale_pool(xp_l, pool_w, pool_b, pool_scale, GRID_W)], axis=-1) @ w_out
    if not with_ctx_out:
        return lat, None
    hr_c = (hc_dirs[0] + hc_dirs[1]).astype(hc.dtype)
    ctx_out = jnp.concatenate(
        [hr_c * jax.nn.gelu(y_c),
         multiscale_pool(xp_c, pool_w, pool_b, pool_scale, None)], axis=-1) @ w_out
    return lat, ctx_out


def moe_ffn(h, router_w, router_b, w1, b1, w2, b2):
    logits = (h @ router_w + router_b).astype(jnp.float32)
    top_v, top_i = lax.top_k(logits, TOP_K)
    top_p = jax.nn.softmax(top_v, axis=-1)
    gates = jnp.sum(jax.nn.one_hot(top_i, N_EXPERTS, dtype=jnp.float32) * top_p[..., None],
                    axis=1).astype(h.dtype)
    out = jnp.zeros_like(h)
    for e in range(N_EXPERTS):
        gu = h @ w1[e] + b1[e]
        glu = jnp.minimum(gu[:, :D_EXPERT], SWIGLU_LIMIT)
        lin = jnp.clip(gu[:, D_EXPERT:], -SWIGLU_LIMIT, SWIGLU_LIMIT)
        act = glu * jax.nn.sigmoid(SWIGLU_ALPHA * glu) * (lin + 1)
        out = out + gates[:, e:e + 1] * (act @ w2[e] + b2[e])
    return out


def setup_inputs(seed: int = 0) -> dict:
    key = jax.random.key(seed)
    ks = iter(jax.random.split(key, 40))

    def nrm(shape, s):
        return jax.random.normal(next(ks), shape, jnp.float32) * s

    beta = (8.0 * DEPTH) ** -0.25
    u = jax.random.uniform(next(ks), (DEPTH, 2, D_LRU), jnp.float32, 0.9, 0.999)
    s = u ** (1.0 / LRU_C)
    lru_lambda = jnp.log(s) - jnp.log1p(-s)
    return {
        'x': nrm((BATCH, SEQ, D_MODEL), 1.0),
        'c': nrm((BATCH, D_MODEL), 1.0),
        'ctx': nrm((BATCH, CTX_LEN, D_MODEL), 1.0),
        'c_ctx': nrm((D_MODEL,), 1.0),
        'w_mod': nrm((DEPTH, D_MODEL, 6 * D_MODEL), D_MODEL ** -0.5),
        'b_mod': nrm((DEPTH, 6 * D_MODEL), 0.01),
        'w_in': nrm((DEPTH, D_MODEL, D_IN), D_MODEL ** -0.5),
        'conv_w': nrm((DEPTH, CONV_W, D_LRU), CONV_W ** -0.5),
        'conv_b': nrm((DEPTH, D_LRU), 0.01),
        'gate_a_w': nrm((DEPTH, 2, N_LRU_HEADS, LRU_HEAD_DIM, LRU_HEAD_DIM), LRU_HEAD_DIM ** -0.5),
        'gate_a_b': nrm((DEPTH, 2, D_LRU), 0.01),
        'gate_x_w': nrm((DEPTH, 2, N_LRU_HEADS, LRU_HEAD_DIM, LRU_HEAD_DIM), LRU_HEAD_DIM ** -0.5),
        'gate_x_b': nrm((DEPTH, 2, D_LRU), 0.01),
        'lru_lambda': lru_lambda,
        'pool_w': nrm((DEPTH, N_POOL_GROUPS, POOL_GROUP_DIM, POOL_GROUP_DIM), POOL_GROUP_DIM ** -0.5),
        'pool_b': nrm((DEPTH, D_POOL), 0.01),
        'pool_scale': 1.0 + nrm((DEPTH, D_POOL), 0.02),
        'w_out': nrm((DEPTH, D_MIX, D_MODEL), beta * D_MIX ** -0.5),
        'ln1_g': 1.0 + nrm((DEPTH, D_MODEL), 0.02),
        'ln1_b': nrm((DEPTH, D_MODEL), 0.02),
        'router_w': nrm((DEPTH, D_MODEL, N_EXPERTS), D_MODEL ** -0.5),
        'router_b': nrm((DEPTH, N_EXPERTS), 0.01),
        'exp_w1': nrm((DEPTH, N_EXPERTS, D_MODEL, 2 * D_EXPERT), D_MODEL ** -0.5),
        'exp_b1': nrm((DEPTH, N_EXPERTS, 2 * D_EXPERT), 0.01),
        'exp_w2': nrm((DEPTH, N_EXPERTS, D_EXPERT, D_MODEL), beta * D_EXPERT ** -0.5),
        'exp_b2': nrm((DEPTH, N_EXPERTS, D_MODEL), 0.01),
        'ln2_g': 1.0 + nrm((DEPTH, D_MODEL), 0.02),
        'ln2_b': nrm((DEPTH, D_MODEL), 0.02),
    }


def reference(x, c, ctx, c_ctx, w_mod, b_mod, w_in, conv_w, conv_b, gate_a_w, gate_a_b,
              gate_x_w, gate_x_b, lru_lambda, pool_w, pool_b, pool_scale, w_out, ln1_g, ln1_b,
              router_w, router_b, exp_w1, exp_b1, exp_w2, exp_b2, ln2_g, ln2_b):
    B, S, D = x.shape
    ROWS = S // GRID_W
    alpha = (2.0 * DEPTH) ** 0.25
    xl = layer_norm(x + grid_sincos(ROWS, GRID_W, D).astype(x.dtype))
    xc = layer_norm(ctx)
    silu_c = jax.nn.silu(c)
    silu_cc = jax.nn.silu(c_ctx)
    for l in range(DEPTH):
        with_ctx = l < DEPTH - 1
        mod_l = silu_c @ w_mod[l] + b_mod[l]
        mod_c = silu_cc @ w_mod[l] + b_mod[l]
        sh1_l, sc1_l, g1_l, sh2_l, sc2_l, g2_l = jnp.split(mod_l[:, None, :], 6, axis=-1)
        sh1_c, sc1_c, g1_c, sh2_c, sc2_c, g2_c = jnp.split(mod_c, 6, axis=-1)

        ml, mc = token_mixer(modulate(xl, sh1_l, sc1_l), modulate(xc, sh1_c, sc1_c),
                             w_in[l], conv_w[l], conv_b[l], gate_a_w[l], gate_a_b[l],
                             gate_x_w[l], gate_x_b[l], lru_lambda[l], pool_w[l], pool_b[l],
                             pool_scale[l], w_out[l], with_ctx)
        xl = layer_norm(alpha * xl + g1_l * ml, ln1_g[l], ln1_b[l])
        hl2 = modulate(xl, sh2_l, sc2_l).reshape(-1, D)
        if with_ctx:
            xc = layer_norm(alpha * xc + g1_c * mc, ln1_g[l], ln1_b[l])
            hc2 = modulate(xc, sh2_c, sc2_c).reshape(-1, D)
            tokens = jnp.concatenate([hl2, hc2], axis=0)
        else:
            tokens = hl2

        f = moe_ffn(tokens, router_w[l], router_b[l], exp_w1[l], exp_b1[l], exp_w2[l], exp_b2[l])
        xl = layer_norm(alpha * xl + g2_l * f[:B * S].reshape(B, S, D), ln2_g[l], ln2_b[l])
        if with_ctx:
            fc = f[B * S:].reshape(B, -1, D)
            xc = layer_norm(alpha * xc + g2_c * fc, ln2_g[l], ln2_b[l])
    return xl
```

```python
import contextlib
import numpy as np
import concourse.bass as bass
import concourse.mybir as mybir
from concourse.bass_utils import run_bass_kernel_spmd

F32 = mybir.dt.float32
BF16 = mybir.dt.bfloat16
ALU = mybir.AluOpType
AF = mybir.ActivationFunctionType
AX = mybir.AxisListType

D = 1024
NCH = 8
T = 2304
CTX = 256
SEQ = 2048
DEPTH = 2
NE = 32
TL = [(0, 256, 1), (256, 512, 0), (768, 512, 0), (1280, 512, 0), (1792, 512, 0)]
ALPHA = float((2.0 * DEPTH) ** 0.25)
LN_EPS = 1e-5
S7 = float(1.0 / (1.0 + np.exp(-1.702 * 7.0)))
NV = 644
V_BMOD, V_CW, V_CB, V_GAB, V_GXB, V_LAM, V_PB, V_PS = 0, 48, 64, 68, 76, 84, 92, 96
V_L1G, V_L1B, V_L2G, V_L2B, V_B1 = 100, 108, 116, 124, 132
ND = 192 + 8 + 256 + 256 + 8
DV_MOD, DV_C8, DV_B1S, DV_B1B1, DV_C8X2 = 0, 192, 200, 456, 712
PLEN = 288 + 32 * 96
XRLEN = 2320
CLIP_ENG = "pool"
FINAL_ENG = "pool"


class Sched:
    ENGS = ("pe", "act", "dve", "pool", "sp")

    def __init__(self, nc, stack):
        self.nc = nc
        self.stack = stack
        self.q = {e: [] for e in self.ENGS}
        self.sems = {}
        self.cnt = {}
        self.known = {e: {} for e in self.ENGS}
        self.res = {}

    def sem(self, key):
        if key not in self.sems:
            name = "s_" + "_".join(str(k) for k in (key if isinstance(key, tuple) else (key,)))
            self.sems[key] = self.stack.enter_context(self.nc.semaphore(name))
        return self.sems[key]

    def _deps(self, R, W):
        deps = {}

        def add(k, c):
            if deps.get(k, 0) < c:
                deps[k] = c

        for r in R:
            ent = self.res.get(r)
            if ent and ent[0]:
                add(*ent[0])
        for w in W:
            ent = self.res.get(w)
            if ent:
                if ent[0]:
                    add(*ent[0])
                for k, c in ent[1].items():
                    add(k, c)
        return deps

    def _commit(self, tok, R, W):
        for r in R:
            ent = self.res.setdefault(r, [None, {}])
            if ent[1].get(tok[0], 0) < tok[1]:
                ent[1][tok[0]] = tok[1]
        for w in W:
            self.res[w] = [tok, {}]

    def _waits(self, eng, deps):
        kn = self.known[eng]
        for k, c in deps.items():
            if k == eng and eng == "pe":
                continue
            if kn.get(k, 0) < c:
                self.sem(k)
                self.q[eng].append(("w", k, c))
                kn[k] = c

    def op(self, eng, fn, R=(), W=(), after=()):
        deps = self._deps(R, W)
        for k, c in after:
            if deps.get(k, 0) < c:
                deps[k] = c
        self._waits(eng, deps)
        self.sem(eng)
        self.cnt[eng] = self.cnt.get(eng, 0) + 1
        tok = (eng, self.cnt[eng])
        self.q[eng].append(("o", fn))
        self._commit(tok, R, W)
        return tok

    def dma(self, q, out, in_, R=(), W=(), key=None):
        self._waits(q, self._deps(R, W))
        self.sem(key)
        self.cnt[key] = self.cnt.get(key, 0) + 16
        tok = (key, self.cnt[key])
        self.q[q].append(("d", out, in_, key))
        self._commit(tok, R, W)
        return tok

    def barrier(self):
        snap = dict(self.cnt)
        for eng in self.ENGS:
            kn = self.known[eng]
            for k, c in snap.items():
                if k == eng:
                    continue
                if kn.get(k, 0) < c:
                    self.q[eng].append(("w", k, c))
                    kn[k] = c

    def replay(self, eng, h):
        for it in self.q[eng]:
            if it[0] == "w":
                h.wait_ge(self.sems[it[1]], it[2])
            elif it[0] == "o":
                it[1](h).then_inc(self.sems[eng], 1)
            else:
                h.dma_start(out=it[1], in_=it[2]).then_inc(self.sems[it[3]], 16)


def build_program(stop=None):
    nc = bass.Bass("TRN2", target_bir_lowering=False)
    dram = {}

    def din(name, shape):
        dram[name] = nc.dram_tensor(name, list(shape), F32, kind="ExternalInput").ap()
        return dram[name]

    xT = din("xT", [128, NCH, T])
    posT = din("posT", [128, NCH, T])
    cT = din("cT", [128, 16])
    vecs = din("vecs", [128, DEPTH, NV])
    ident_d = din("ident", [128, 128])
    rb_d = din("rb", [DEPTH, NE])
    rw_d = din("rw", [DEPTH, D, NE])
    w_mod = din("w_mod", [DEPTH, D, 6 * D])
    w_in = din("w_in", [DEPTH, D, 1536])
    w_out = din("w_out", [DEPTH, D, D])
    gbd = din("gbd", [DEPTH, 4, 128, 4, 128])
    pool_w = din("pool_w", [DEPTH, 4, 128, 128])
    rcnt_d = din("rcnt", [4, PLEN])
    w1_d = din("exp_w1", [DEPTH, NE, D, 2 * D])
    w2_d = din("exp_w2", [DEPTH, NE, D, D])
    b2_d = din("exp_b2", [DEPTH, NE, D])
    outT = nc.dram_tensor("outT", [128, NCH, SEQ], F32, kind="ExternalOutput").ap()
    gscr = nc.dram_tensor("gscr", [DEPTH, NE, T], F32, kind="Internal").ap()
    dbg_out = {}

    stack = contextlib.ExitStack()
    with stack:
        NPERS = 18432 + 9216 + DEPTH * NV + DEPTH * ND + 128 + 128 + 32 * DEPTH + 16 + 8 + 64
        NPH = 20900
        AR = stack.enter_context(nc.sbuf_tensor("arena", [128, NPERS + NPH], F32))
        ps = [stack.enter_context(nc.psum_tensor(f"ps{i}", [128, 512], F32)) for i in range(8)]
        S = Sched(nc, stack)

        off = [0]

        def palloc(n):
            o = off[0]
            off[0] += n
            return AR[:, o:o + n]

        X2 = palloc(18432)
        X = X2.rearrange("p (c t) -> p c t", c=NCH)
        H = palloc(9216).bitcast(BF16).rearrange("p (c t) -> p c t", c=NCH)
        VEC = palloc(DEPTH * NV).rearrange("p (l v) -> p l v", l=DEPTH)
        DV = palloc(DEPTH * ND).rearrange("p (l v) -> p l v", l=DEPTH)
        ONES = palloc(128)
        IDENT = palloc(128)
        RB = palloc(32 * DEPTH).rearrange("p (l e) -> p l e", l=DEPTH)
        CS = palloc(16)
        CBF = palloc(8).bitcast(BF16).rearrange("p (k s) -> p k s", s=2)
        ONE1 = palloc(8)
        PH0 = off[0]
        assert off[0] <= NPERS

        def phase_alloc():
            o = [PH0]

            def a(n):
                r = AR[:, o[0]:o[0] + n]
                o[0] += n
                assert o[0] <= NPERS + NPH, (o[0], NPERS + NPH)
                return r
            return a

        def vcol(l, j):
            return VEC[:, l, j:j + 1]

        def dcol(l, j):
            return DV[:, l, j:j + 1]

        def modc(l, which, c, s):
            return dcol(l, DV_MOD + (which * 8 + c) * 2 + s)

        def ts(eng, out, in0, s1, s2, op0, op1, R, W):
            if op1 is None:
                return S.op(eng, lambda h: h.tensor_scalar(out=out, in0=in0, scalar1=s1, scalar2=None, op0=op0), R, W)
            return S.op(eng, lambda h: h.tensor_scalar(out=out, in0=in0, scalar1=s1, scalar2=s2, op0=op0, op1=op1), R, W)

        def stt(out, in0, sc, in1, op0, op1, R, W):
            return S.op("dve", lambda h: h.scalar_tensor_tensor(out=out, in0=in0, scalar=sc, in1=in1, op0=op0, op1=op1), R, W)

        def tt(out, in0, in1, op, R, W, eng="dve"):
            return S.op(eng, lambda h: h.tensor_tensor(out=out, in0=in0, in1=in1, op=op), R, W)

        def act(out, in_, func, R, W, bias=None, scale=None, after=()):
            kw = {}
            if bias is not None:
                kw["bias"] = bias
            if scale is not None:
                kw["scale"] = scale
            return S.op("act", lambda h: h.activation(out=out, in_=in_, func=func, **kw), R, W, after)

        def opm(eng, method, R, W, *a, **k):
            return S.op(eng, lambda h: getattr(h, method)(*a, **k), R, W)

        def mm1(out, lhsT, rhs, start, stop, R, W):
            return S.op("pe", lambda h: h.matmul(out, lhsT=lhsT, rhs=rhs, start=start, stop=stop), R, W)

        def mm_group(out, pairs, R, W):
            n = len(pairs)

            def fn(h):
                inst = None
                for i, (l_, r_) in enumerate(pairs):
                    inst = h.matmul(out, lhsT=l_, rhs=r_, start=(i == 0), stop=(i == n - 1))
                return inst
            return S.op("pe", fn, R, W)

        S.dma("sp", VEC, vecs, W=["VEC"], key="ld_vec")
        S.dma("sp", IDENT, ident_d, W=["IDENT"], key="ld_ident")
        S.dma("sp", CS, cT, W=["CS"], key="ld_cs")
        for l in range(DEPTH):
            S.dma("sp", RB[:, l, :], rb_d[l:l + 1, :].broadcast_to([128, NE]), W=[("RB", l)], key=("ld_rb", l))
        opm("dve", "memset", [], ["ONES"], ONES, 1.0 / D)
        opm("dve", "memset", [], ["ONE1"], ONE1, 1.0)
        act(CBF, CS.rearrange("p (k s) -> p k s", s=2), AF.Silu, R=["CS"], W=["CBF"])

        WMB = NPERS + NPH - 256 - 4096
        WM = [AR[:, WMB + i * 2048:WMB + (i + 1) * 2048].bitcast(BF16).rearrange("p (k f) -> p k f", k=8) for i in range(2)]

        def adaln(l):
            psm = ps[2 + l]
            for n in range(12):
                slot = (l * 12 + n) % 2
                S.dma("pool", WM[slot], w_mod[l, :, n * 512:(n + 1) * 512].rearrange("(k p) f -> p k f", p=128),
                      W=[("WM", slot)], key=("ld_wm", slot))
                for qq in range(4):
                    j = n * 4 + qq
                    mm_group(psm[:, j * 2:j * 2 + 2],
                             [(WM[slot][:, kc, qq * 128:(qq + 1) * 128], CBF[:, kc, :]) for kc in range(8)],
                             R=[("WM", slot), "CBF"], W=[("psm", l, j)])
            psv = psm[:, 0:96].rearrange("p (j s) -> p j s", s=2)
            dvm = DV[:, l, DV_MOD:DV_MOD + 96].rearrange("p (j s) -> p j s", s=2)
            allj = [("psm", l, j) for j in range(48)]
            for s in range(2):
                tt(dvm[:, :, s], psv[:, :, s], VEC[:, l, V_BMOD:V_BMOD + 48], ALU.add, R=allj + ["VEC"], W=[("DV", l)])
            for which in (1, 4):
                o = DV_MOD + which * 16
                ts("dve", DV[:, l, o:o + 16], DV[:, l, o:o + 16], 1.0, None, ALU.add, None, R=[("DV", l)], W=[("DV", l)])
            c8 = DV[:, l, DV_C8:DV_C8 + 8]
            act(c8, VEC[:, l, V_LAM:V_LAM + 8], AF.Exp, R=["VEC"], W=[("DV", l)], scale=-1.0)
            ts("dve", c8, c8, 1.0, None, ALU.add, None, R=[("DV", l)], W=[("DV", l)])
            act(c8, c8, AF.Ln, R=[("DV", l)], W=[("DV", l)])
            ts("dve", c8, c8, -8.0, None, ALU.mult, None, R=[("DV", l)], W=[("DV", l)])
            b1v = VEC[:, l, V_B1:V_B1 + 512].rearrange("p (e j) -> p e j", j=16)
            ts("dve", DV[:, l, DV_B1S:DV_B1S + 256].rearrange("p (e j) -> p e j", j=8), b1v[:, :, 0:8], 1.702, None,
               ALU.mult, None, R=["VEC"], W=[("DV", l)])
            ts("dve", DV[:, l, DV_B1B1:DV_B1B1 + 256].rearrange("p (e j) -> p e j", j=8), b1v[:, :, 8:16], 1.0, None,
               ALU.add, None, R=["VEC"], W=[("DV", l)])
            ts("dve", DV[:, l, DV_C8X2:DV_C8X2 + 8], c8, 2.0, None, ALU.mult, None, R=[("DV", l)], W=[("DV", l)])


        def ln_phase(tiles, l_aff, aff_base, l_mod, which_sh, final, router_l=None, entry=False):
            pa = phase_alloc()
            GT = pa(T) if True else None
            SQ = [pa(512) for _ in range(2)]
            M2 = pa(512)
            TMP = [pa(512) for _ in range(8)]
            RSTDS = pa(512)
            H32 = pa(4096).rearrange("p (c t) -> p c t", c=8) if router_l is not None else None
            POSB = [pa(512) for _ in range(2)] if entry else None
            LGS = pa(256)
            RT = pa(256)
            B2 = pa(1024) if router_l is not None else None
            ksq = [0]
            def stats_a(ti):
                t0, n, s = tiles[ti]
                tk = t0
                if entry:
                    S.dma("sp", X[:, :, t0:t0 + n], xT[:, :, t0:t0 + n], W=[("X", c, tk) for c in range(NCH)], key=("ld_x", ti))
                    for c in range(NCH):
                        if s == 0:
                            pb = POSB[c % 2]
                            S.dma("sp", pb[:, 0:n], posT[:, c, t0:t0 + n], W=[("POSB", c % 2)], key=("ld_pos", c % 2))
                            tt(X[:, c, t0:t0 + n], X[:, c, t0:t0 + n], pb[:, 0:n], ALU.add,
                               R=[("X", c, tk), ("POSB", c % 2)], W=[("X", c, tk)])
                s1, s2 = ps[0], ps[1]
                for c in range(NCH):
                    sl = ksq[0] % 2
                    ksq[0] += 1
                    act(SQ[sl][:, 0:n], X[:, c, t0:t0 + n], AF.Square, R=[("X", c, tk)], W=[("SQ", sl)])
                    mm1(s1[:, 0:n], ONES, X[:, c, t0:t0 + n], c == 0, c == NCH - 1, R=[("X", c, tk), "ONES"], W=["ps0"])
                    mm1(s2[:, 0:n], ONES, SQ[sl][:, 0:n], c == 0, c == NCH - 1, R=[("SQ", sl), "ONES"], W=["ps1"])

            def stats_b(ti):
                t0, n, s = tiles[ti]
                s1, s2 = ps[0], ps[1]
                act(M2[:, 0:n], s1[:, 0:n], AF.Square, R=["ps0"], W=["M2"])
                stt(M2[:, 0:n], s2[:, 0:n], LN_EPS, M2[:, 0:n], ALU.add, ALU.subtract, R=["ps1", "M2"], W=["M2"])
                act(M2[:, 0:n], M2[:, 0:n], AF.Sqrt, R=["M2"], W=["M2"])
                rb_, nb_ = 4 + ti % 2, 6 + ti % 2
                RSTD, NMR = ps[rb_], ps[nb_]
                krs, knm = f"ps{rb_}", f"ps{nb_}"
                opm("dve", "reciprocal", ["M2"], ["RSTDS"], out=RSTDS[:, 0:n], in_=M2[:, 0:n])
                stt(NMR[:, 0:n], s1[:, 0:n], -1.0, RSTDS[:, 0:n], ALU.mult, ALU.mult, R=["ps0", "RSTDS"], W=[knm])
                opm("act", "copy", ["RSTDS"], [krs], RSTD[:, 0:n], RSTDS[:, 0:n])

            stats_a(0)
            stats_b(0)
            for ti, (t0, n, s) in enumerate(tiles):
                tk = t0
                if ti + 1 < len(tiles):
                    stats_a(ti + 1)
                rb_, nb_ = 4 + ti % 2, 6 + ti % 2
                RSTD, NMR = ps[rb_], ps[nb_]
                krs, knm = f"ps{rb_}", f"ps{nb_}"
                tms = [TMP[c][:, 0:n] for c in range(NCH)]
                tks = [("TMP", c) for c in range(NCH)]
                for c in range(NCH):
                    tt(tms[c], X[:, c, t0:t0 + n], RSTD[:, 0:n], ALU.mult, R=[("X", c, tk), krs], W=[tks[c]])
                for c in range(NCH):
                    tt(tms[c], tms[c], NMR[:, 0:n], ALU.add, R=[tks[c], knm], W=[tks[c]])
                if l_aff is not None:
                    for c in range(NCH):
                        ts("dve", tms[c], tms[c], vcol(l_aff, aff_base + c), vcol(l_aff, aff_base + 8 + c), ALU.mult, ALU.add,
                           R=[tks[c], "VEC"], W=[tks[c]])
                for c in range(NCH):
                    if l_mod is not None:
                        sc_ = modc(l_mod, which_sh + 1, c, s)
                        sh_ = modc(l_mod, which_sh, c, s)
                        ts("pool", H[:, c, t0:t0 + n], tms[c], sc_, sh_, ALU.mult, ALU.add, R=[tks[c], ("DV", l_mod)], W=[("H", tk)])
                        if router_l is not None:
                            ts("pool", H32[:, c, 0:n], tms[c], sc_, sh_, ALU.mult, ALU.add, R=[tks[c], ("DV", l_mod)], W=["H32"])
                    opm("act", "mul", [tks[c]], [("X", c, tk)], X[:, c, t0:t0 + n], tms[c], 1.0 if final else ALPHA)
                if router_l is not None:
                    RW = router_l["RW"]
                    rl = router_l["l"]
                    nsub = n // 128
                    subs = list(range(nsub))
                    for k in subs:
                        mm_group(ps[2][:, k * 32:(k + 1) * 32], [(H32[:, kc, k * 128:(k + 1) * 128], RW[:, kc, :]) for kc in range(8)],
                                 R=["H32", "RW"], W=["ps2"])
                    lgs = [LGS[:, k * 64:k * 64 + 32] for k in subs]
                    m8s = [LGS[:, k * 64 + 32:k * 64 + 40] for k in subs]
                    nmxs = [LGS[:, k * 64 + 40:k * 64 + 41] for k in subs]
                    sms = [LGS[:, k * 64 + 41:k * 64 + 42] for k in subs]
                    msks = [RT[:, k * 64:k * 64 + 32] for k in subs]
                    ees = [RT[:, k * 64 + 32:k * 64 + 64] for k in subs]
                    for k in subs:
                        tt(lgs[k], ps[2][:, k * 32:(k + 1) * 32], RB[:, rl, :], ALU.add, R=["ps2", ("RB", rl)], W=[("LG", k)])
                    for k in subs:
                        opm("dve", "max", [("LG", k)], [("M8", k)], out=m8s[k], in_=lgs[k])
                    for k in subs:
                        ts("dve", nmxs[k], m8s[k][:, 0:1], -1.0, None, ALU.mult, None, R=[("M8", k)], W=[("NMX", k)])
                    for k in subs:
                        ts("dve", msks[k], lgs[k], m8s[k][:, 3:4], None, ALU.is_ge, None, R=[("LG", k), ("M8", k)], W=[("MSK", k)])
                    for k in subs:
                        act(ees[k], lgs[k], AF.Exp, R=[("LG", k), ("NMX", k)], W=[("EE", k)], bias=nmxs[k], scale=1.0)
                    for k in subs:
                        tt(ees[k], ees[k], msks[k], ALU.mult, R=[("MSK", k), ("EE", k)], W=[("EE", k)])
                    for k in subs:
                        opm("dve", "tensor_reduce", [("EE", k)], [("SM", k)], out=sms[k], in_=ees[k], axis=AX.X, op=ALU.add)
                    for k in subs:
                        opm("dve", "reciprocal", [("SM", k)], [("SM", k)], out=sms[k], in_=sms[k])
                    for k in subs:
                        ts("dve", ees[k], ees[k], sms[k], None, ALU.mult, None, R=[("EE", k), ("SM", k)], W=[("EE", k)])
                    for k in subs:
                        opm("pe", "transpose", [("EE", k), "IDENT"], ["ps3"], ps[3][0:32, k * 128:(k + 1) * 128], ees[k], IDENT)
                    opm("act", "copy", ["ps3"], ["GT"], GT[0:32, t0:t0 + n], ps[3][0:32, 0:n])
                if ti + 1 < len(tiles):
                    stats_b(ti + 1)
            if router_l is not None:
                lr = router_l["l"]
                S.dma("sp", gscr[lr], GT[0:32, :], R=["GT"], W=["GSCR"], key="st_gscr")
                S.dma("sp", B2[0:32, :], b2_d[lr], W=["B2"], key="ld_b2")
                for ti, (t0, n, s) in enumerate(tiles):
                    for dch in range(NCH):
                        b = 4 + dch % 4
                        mm_group(ps[b][:, 0:n], [(B2[0:32, dch * 128:(dch + 1) * 128], GT[0:32, t0:t0 + n])], R=["B2", "GT"], W=[f"ps{b}"])
                        stt(X[:, dch, t0:t0 + n], ps[b][:, 0:n], modc(lr, 5, dch, s), X[:, dch, t0:t0 + n], ALU.mult, ALU.add,
                            R=[f"ps{b}", ("X", dch, t0), ("DV", lr)], W=[("X", dch, t0)])
            return GT

        def mixer_phase(l):
            pa = phase_alloc()
            U = pa(XRLEN)
            RA = pa(XRLEN)
            IB = pa(XRLEN)
            QH = [pa(XRLEN), pa(XRLEN)]
            UBF = pa(1152).bitcast(BF16)
            MIXC = [pa(1152).bitcast(BF16) for _ in range(2)]
            WI = [pa(512).bitcast(BF16).rearrange("p (k f) -> p k f", k=8) for _ in range(2)]
            WO = [pa(512).bitcast(BF16) for _ in range(2)]
            GB = [pa(256).bitcast(BF16).rearrange("p (m f) -> p m f", m=4) for _ in range(2)]
            mtiles = TL if l < DEPTH - 1 else TL[1:]
            wi_n = [0]
            wo_n = [0]

            def load_wi(j):
                slot = wi_n[0] % 2
                wi_n[0] += 1
                S.dma("pool", WI[slot], w_in[l, :, j * 128:(j + 1) * 128].rearrange("(k p) f -> p k f", p=128),
                      W=[("WI", slot)], key=("ld_wi", slot))
                return slot

            def load_wo(kc):
                slot = wo_n[0] % 2
                wo_n[0] += 1
                S.dma("pool", WO[slot], w_out[l, kc * 128:(kc + 1) * 128, :], W=[("WO", slot)], key=("ld_wo", slot))
                return slot

            def zmm(out, slot, t0, n, R, W):
                return mm_group(out, [(WI[slot][:, kc, :], H[:, kc, t0:t0 + n]) for kc in range(8)],
                                R=[("WI", slot), ("H", t0)] + R, W=W)

            def wout_acc_pair(MX, slots):
                for ti, (t0, n, s) in enumerate(mtiles):
                    for dch in range(NCH):
                        b = 4 + (dch % 4)
                        mm_group(ps[b][:, 0:n], [(WO[slots[i]][:, dch * 128:(dch + 1) * 128], MX[i][:, t0:t0 + n]) for i in range(2)],
                                 R=[("WO", slots[0]), ("WO", slots[1]), ("MIXC", 0, t0), ("MIXC", 1, t0)], W=[f"ps{b}"])
                        stt(X[:, dch, t0:t0 + n], ps[b][:, 0:n], modc(l, 2, dch, s), X[:, dch, t0:t0 + n], ALU.mult, ALU.add,
                            R=[f"ps{b}", ("X", dch, t0), ("DV", l)], W=[("X", dch, t0)])

            woslots = [None, None]
            for c in range(4):
                sx = load_wi(c)
                sy = load_wi(4 + c)
                gslot = c % 2
                S.dma("pool", GB[gslot], gbd[l, c], W=[("GB", gslot)], key=("ld_gb", gslot))
                woslots[c % 2] = load_wo(c)
                XR = QH[0]
                for (a, b) in ((0, 4), (260, 264), (2312, 2316)):
                    opm("dve", "memset", [], [("QH", 0)], XR[:, a:b], 0.0)
                for ti, (t0, n, s) in enumerate(TL):
                    b = ti % 2
                    zmm(ps[b][:, 0:n], sx, t0, n, [], [f"ps{b}"])
                    po = t0 + 4 if s == 1 else t0 + 8
                    opm("act", "copy", [f"ps{b}"], [("QH", 0)], XR[:, po:po + n], ps[b][:, 0:n])
                for (u0, un, xb) in ((0, 256, 2), (256, 2048, 262)):
                    for k in range(4):
                        wk = vcol(l, V_CW + c * 4 + k)
                        src = XR[:, xb + k:xb + k + un]
                        if k == 0:
                            ts("dve", U[:, u0:u0 + un], src, wk, vcol(l, V_CB + c), ALU.mult, ALU.add,
                               R=[("QH", 0), "VEC"], W=["U"])
                        else:
                            stt(U[:, u0:u0 + un], src, wk, U[:, u0:u0 + un], ALU.mult, ALU.add, R=[("QH", 0), "U", "VEC"], W=["U"])
                opm("act", "copy", ["U"], ["UBF"], UBF[:, 0:T], U[:, 0:T])
                for d in range(2):
                    Q = QH[d]
                    for ti, (t0, n, s) in enumerate(TL):
                        ba, bx = 2 * (ti % 2), 2 * (ti % 2) + 1
                        mm_group(ps[ba][:, 0:n], [(GB[gslot][:, d * 2 + 0, :], UBF[:, t0:t0 + n])], R=[("GB", gslot), "UBF"], W=[f"ps{ba}"])
                        mm_group(ps[bx][:, 0:n], [(GB[gslot][:, d * 2 + 1, :], UBF[:, t0:t0 + n])], R=[("GB", gslot), "UBF"], W=[f"ps{bx}"])
                        act(RA[:, t0:t0 + n], ps[ba][:, 0:n], AF.Sigmoid, R=[f"ps{ba}", "VEC"], W=["RA"] + [("T1", tj) for tj in range(5)],
                            bias=vcol(l, V_GAB + d * 4 + c))
                        act(IB[:, t0:t0 + n], ps[bx][:, 0:n], AF.Sigmoid, R=[f"ps{bx}", "VEC"], W=["IB"] + [("T2", tj) for tj in range(5)],
                            bias=vcol(l, V_GXB + d * 4 + c))
                    act(Q[:, 0:T], RA[:, 0:T], AF.Exp, R=["RA", ("DV", l)], W=[("QH", d)], scale=dcol(l, DV_C8X2 + d * 4 + c))
                    act(RA[:, 0:T], RA[:, 0:T], AF.Exp, R=["RA", ("DV", l)], W=["RA"], scale=dcol(l, DV_C8 + d * 4 + c))
                    act(Q[:, 0:T], Q[:, 0:T], AF.Sqrt, R=[("QH", d), "ONE1"], W=[("QH", d)], scale=-1.0, bias=ONE1[:, 0:1])
                    tt(IB[:, 0:T], IB[:, 0:T], U[:, 0:T], ALU.mult, R=["IB", "U"], W=["IB"])
                    tt(IB[:, 0:T], IB[:, 0:T], Q[:, 0:T], ALU.mult, R=["IB", ("QH", d)], W=["IB"])
                    if d == 0:
                        opm("dve", "tensor_tensor_scan", ["RA", "IB"], [("QH", d)], out=Q[:, 0:T], data0=RA[:, 0:T],
                            data1=IB[:, 0:T], initial=0.0, op0=ALU.mult, op1=ALU.add)
                    else:
                        opm("dve", "tensor_tensor_scan", ["RA", "IB"], [("QH", d)], out=Q[:, 0:CTX][:, ::-1],
                            data0=RA[:, 0:CTX][:, ::-1], data1=IB[:, 0:CTX][:, ::-1], initial=0.0, op0=ALU.mult, op1=ALU.add)
                        scan_tok = opm("dve", "tensor_tensor_scan", ["RA", "IB", ("QH", d)], [("QH", d)], out=Q[:, CTX:T][:, ::-1],
                            data0=RA[:, CTX:T][:, ::-1], data1=IB[:, CTX:T][:, ::-1], initial=Q[:, 0:1], op0=ALU.mult, op1=ALU.add)
                for ti, (t0, n, s) in enumerate(mtiles):
                    b = ti % 2
                    pY = ps[b][:, 0:n]
                    zmm(pY, sy, t0, n, [], [f"ps{b}"])
                    t1 = RA[:, t0:t0 + n]
                    t2 = IB[:, t0:t0 + n]
                    k1, k2 = ("T1", ti), ("T2", ti)
                    act(t1, pY, AF.Square, R=[f"ps{b}"], W=[k1], after=[scan_tok])
                    ts("dve", t1, t1, 0.044715, 1.0, ALU.mult, ALU.add, R=[k1], W=[k1])
                    tt(t1, t1, pY, ALU.mult, R=[k1, f"ps{b}"], W=[k1])
                    act(t1, t1, AF.Sigmoid, R=[k1], W=[k1], scale=1.5957691216057308)
                    tt(t1, t1, pY, ALU.mult, R=[k1, f"ps{b}"], W=[k1])
                    tt(t2, QH[0][:, t0:t0 + n], QH[1][:, t0:t0 + n], ALU.add, R=[("QH", 0), ("QH", 1)], W=[k2])
                    tt(MIXC[c % 2][:, t0:t0 + n], t1, t2, ALU.mult, R=[k1, k2], W=[("MIXC", c % 2, t0)])
                if c % 2 == 1:
                    wout_acc_pair(MIXC, woslots)
            S.barrier()
            pa = phase_alloc()
            P0 = pa(PLEN)
            P1 = pa(PLEN)
            P2 = pa(PLEN)
            RC = pa(PLEN)
            DD = pa(1152).bitcast(BF16)
            MIXC = [pa(1152).bitcast(BF16) for _ in range(2)]
            WI = [pa(512).bitcast(BF16).rearrange("p (k f) -> p k f", k=8) for _ in range(2)]
            WO = [pa(512).bitcast(BF16) for _ in range(2)]
            PW = [pa(64).bitcast(BF16) for _ in range(2)]

            def lat3(P):
                return P[:, 288:PLEN].rearrange("p (r w) -> p r w", w=96)[:, :, 16:80]

            for g in range(4):
                sx = load_wi(8 + g)
                woslots[g % 2] = load_wo(4 + g)
                pslot = g % 2
                S.dma("pool", PW[pslot], pool_w[l, g], W=[("PW", pslot)], key=("ld_pw", pslot))
                S.dma("sp", RC, rcnt_d[g:g + 1, :].broadcast_to([128, PLEN]), W=["RC"], key="ld_rc")
                opm("dve", "memset", [], ["P0"], P0, 0.0)
                for ti, (t0, n, s) in enumerate(TL):
                    b = ti % 2
                    zmm(ps[b][:, 0:n], sx, t0, n, [], [f"ps{b}"])
                    if s == 1:
                        opm("act", "copy", [f"ps{b}"], ["P0"], P0[:, 16:272], ps[b][:, 0:256])
                    else:
                        r0 = (t0 - CTX) // 64
                        opm("act", "copy", [f"ps{b}"], ["P0"], lat3(P0)[:, r0:r0 + 8, :],
                            ps[b][:, 0:512].rearrange("p (r w) -> p r w", w=64))
                src, sname = P0, "P0"
                dsts = [(P1, "P1"), (P2, "P2")]
                for i in range(g + 1):
                    dst, dname = dsts[i % 2]
                    if i == 0:
                        tt(dst[:, 1:PLEN], src[:, 0:PLEN - 1], src[:, 1:PLEN], ALU.add, R=[sname], W=[dname])
                    else:
                        hw = 1 << (i - 1)
                        tt(dst[:, hw:PLEN - hw], src[:, 0:PLEN - 2 * hw], src[:, 2 * hw:PLEN], ALU.add, R=[sname], W=[dname])
                    src, sname = dst, dname
                oth, oname = dsts[(g + 1) % 2]
                tt(oth[:, 16:PLEN - 16], src[:, 16:PLEN - 16], RC[:, 16:PLEN - 16], ALU.mult, R=[sname, "RC"], W=[oname])
                tt(DD[:, 0:256], oth[:, 16:272], P0[:, 16:272], ALU.subtract, R=[oname, "P0"], W=["DD"])
                tt(DD[:, 256:T].rearrange("p (r w) -> p r w", w=64), lat3(oth), lat3(P0), ALU.subtract, R=[oname, "P0"], W=["DD"])
                for ti, (t0, n, s) in enumerate(mtiles):
                    b = 2 + ti % 2
                    mm_group(ps[b][:, 0:n], [(PW[pslot], DD[:, t0:t0 + n])], R=[("PW", pslot), "DD"], W=[f"ps{b}"])
                    ts("dve", MIXC[g % 2][:, t0:t0 + n], ps[b][:, 0:n], vcol(l, V_PB + g), vcol(l, V_PS + g), ALU.add, ALU.mult,
                       R=[f"ps{b}", "VEC"], W=[("MIXC", g % 2, t0)])
                if g % 2 == 1:
                    wout_acc_pair(MIXC, woslots)
            S.barrier()

        def moe_phase(l, GT_unused):
            pa = phase_alloc()
            W1 = [pa(4096).bitcast(BF16).rearrange("p (k f) -> p k f", k=8) for _ in range(2)]
            W2 = [pa(2048).bitcast(BF16).rearrange("p (k f) -> p k f", k=4) for _ in range(2)]
            ACTB = [pa(1024).bitcast(BF16).rearrange("p (k t) -> p k t", k=4) for _ in range(2)]
            SB = [pa(512) for _ in range(2)]
            TB = [pa(512) for _ in range(2)]
            GB_ = [pa(512) for _ in range(2)]
            GS = [pa(512) for _ in range(3)]
            mtiles = (TL[1:] + TL[:1]) if l < DEPTH - 1 else TL[1:]
            pk = [0]
            ok = [0]
            gk = [0]
            ak = [0]
            NU = 2 * NE

            def load_w1(uidx):
                e, hf = uidx // 2, uidx % 2
                ws = uidx % 2
                for half, c0 in ((0, hf * 512), (1, 1024 + hf * 512)):
                    S.dma("pool", W1[ws][:, :, half * 512:(half + 1) * 512],
                          w1_d[l, e, :, c0:c0 + 512].rearrange("(k p) f -> p k f", p=128), W=[("W1", ws, half)], key=("ld_w1", ws, half))

            def load_w2(uidx):
                e, hf = uidx // 2, uidx % 2
                ws = uidx % 2
                S.dma("pool", W2[ws], w2_d[l, e, hf * 512:(hf + 1) * 512, :].rearrange("(k p) f -> p k f", p=128),
                      W=[("W2", ws)], key=("ld_w2", ws))

            def stage2(uidx, tile, aslot):
                t0, n, s = tile
                ws = uidx % 2
                for dch in range(NCH):
                    b = 4 + ok[0] % 4
                    ok[0] += 1
                    mm_group(ps[b][:, 0:n], [(W2[ws][:, fc, dch * 128:(dch + 1) * 128], ACTB[aslot][:, fc, 0:n]) for fc in range(4)],
                             R=[("W2", ws), ("ACTB", aslot)], W=[f"ps{b}"])
                    stt(X[:, dch, t0:t0 + n], ps[b][:, 0:n], modc(l, 5, dch, s), X[:, dch, t0:t0 + n], ALU.mult, ALU.add,
                        R=[f"ps{b}", ("X", dch, t0), ("DV", l)], W=[("X", dch, t0)])

            load_w1(0)
            load_w2(0)
            prev = None
            for uidx in range(NU):
                e, hf = uidx // 2, uidx % 2
                ws = uidx % 2
                if uidx + 1 < NU:
                    load_w1(uidx + 1)
                for ti, tile in enumerate(mtiles):
                    t0, n, s = tile
                    gs = gk[0] % 3
                    gk[0] += 1
                    S.dma("sp", GS[gs][:, 0:n], gscr[l, e:e + 1, t0:t0 + n].broadcast_to([128, n]), R=["GSCR"], W=[("GS", gs)], key=("ld_gs", gs))
                    aslot = ak[0] % 2
                    ak[0] += 1
                    for j in range(4):
                        sl = pk[0] % 2
                        pk[0] += 1
                        pA, pB = ps[2 * sl], ps[2 * sl + 1]
                        ka, kb = f"ps{2 * sl}", f"ps{2 * sl + 1}"
                        mm_group(pA[:, 0:n], [(W1[ws][:, kc, j * 128:(j + 1) * 128], H[:, kc, t0:t0 + n]) for kc in range(8)],
                                 R=[("W1", ws, 0), ("H", t0)], W=[ka])
                        mm_group(pB[:, 0:n], [(W1[ws][:, kc, 512 + j * 128:512 + (j + 1) * 128], H[:, kc, t0:t0 + n]) for kc in range(8)],
                                 R=[("W1", ws, 1), ("H", t0)], W=[kb])
                        cj = e * 8 + hf * 4 + j
                        sb, tb, ab = SB[sl][:, 0:n], TB[sl][:, 0:n], GB_[sl][:, 0:n]
                        act(sb, pA[:, 0:n], AF.Sigmoid, R=[ka, ("DV", l)], W=[("SB", sl)], bias=dcol(l, DV_B1S + cj), scale=1.702)
                        act(ab, pA[:, 0:n], AF.Identity, R=[ka, "VEC"], W=[("GG", sl)], bias=vcol(l, V_B1 + e * 16 + hf * 4 + j), scale=1.0)
                        act(tb, pB[:, 0:n], AF.Identity, R=[kb, ("DV", l)], W=[("TB", sl)], bias=dcol(l, DV_B1B1 + cj), scale=1.0)
                        stt(ab, ab, 7.0, GS[gs][:, 0:n], ALU.min, ALU.mult, R=[("GG", sl), ("GS", gs)], W=[("GG", sl)])
                        stt(ab, sb, S7, ab, ALU.min, ALU.mult, R=[("SB", sl), ("GG", sl)], W=[("GG", sl)])
                        ts(CLIP_ENG, tb, tb, 8.0, -6.0, ALU.min, ALU.max, R=[("TB", sl)], W=[("TB", sl)])
                        tt(ACTB[aslot][:, j, 0:n], ab, tb, ALU.mult, R=[("GG", sl), ("TB", sl)], W=[("ACTB", aslot)], eng=FINAL_ENG)
                    if prev is not None:
                        stage2(*prev)
                    if ti == 0 and uidx + 1 < NU:
                        load_w2(uidx + 1)
                    prev = (uidx, tile, aslot)
            stage2(*prev)
            S.barrier()

        stopped = [stop == "adaln"]

        def cp(name):
            if stop == name:
                stopped[0] = True
            return stopped[0]

        adaln(0)
        if stopped[0]:
            adaln(1)
            S.barrier()
        if not stopped[0]:
            ln_phase(TL, None, None, 0, 0, False, entry=True)
            adaln(1)
            S.barrier()
            cp("entry")
        for l in range(DEPTH):
            if stopped[0]:
                break
            last = l == DEPTH - 1
            mtiles = TL if not last else TL[1:]
            mixer_phase(l)
            if cp(f"mixer{l}"):
                break
            pa_tmp = phase_alloc()
            for _ in range(1):
                pass
            RW = AR[:, NPERS + NPH - 256:NPERS + NPH].rearrange("p (k e) -> p k e", k=8)
            S.dma("sp", RW, rw_d[l].rearrange("(k p) e -> p k e", p=128), W=["RW"], key="ld_rw")
            ln_phase(mtiles, l, V_L1G, l, 3, False, router_l={"RW": RW, "l": l})
            S.barrier()
            if cp(f"ln1_{l}"):
                break
            moe_phase(l, None)
            if cp(f"moe{l}"):
                break
            if not last:
                ln_phase(mtiles, l, V_L2G, l + 1, 0, False)
            else:
                ln_phase(mtiles, l, V_L2G, None, 0, True)
            S.barrier()
        if stop is not None:
            S.barrier()
            dX = nc.dram_tensor("dbgX", [128, NCH, T], F32, kind="ExternalOutput").ap()
            dH = nc.dram_tensor("dbgH", [128, NCH, T], BF16, kind="ExternalOutput").ap()
            dDV = nc.dram_tensor("dbgDV", [128, DEPTH, ND], F32, kind="ExternalOutput").ap()
            dGT = nc.dram_tensor("dbgGT", [32, T], F32, kind="ExternalOutput").ap()
            S.dma("sp", dX, X, key="st_out")
            S.dma("sp", dH, H, key="st_out")
            S.dma("sp", dDV, DV, key="st_out")
            S.dma("sp", dGT, AR[0:32, PH0:PH0 + T], key="st_out")
        for c in range(NCH):
            S.dma("sp", outT[:, c, :], X[:, c, CTX:T], R=[("X", c, t0) for (t0, n, s) in TL[1:]], key="st_out")
        nout = S.cnt["st_out"]
        S.q["sp"].append(("w", "st_out", nout))

        with nc.Block() as block:
            @block.tensor
            def _(h):
                S.replay("pe", h)

            @block.scalar
            def _(h):
                S.replay("act", h)

            @block.vector
            def _(h):
                S.replay("dve", h)

            @block.gpsimd
            def _(h):
                S.replay("pool", h)

            @block.sync
            def _(h):
                S.replay("sp", h)
    return nc


def _grid_sincos(rows, cols, d):
    quarter = d // 4
    omega = (1.0 / (np.float32(10000.0) ** (np.arange(quarter, dtype=np.float32) / np.float32(quarter)))).astype(np.float32)

    def emb1d(n):
        ang = np.arange(n, dtype=np.float32)[:, None] * omega[None, :]
        return np.concatenate([np.sin(ang), np.cos(ang)], axis=-1).astype(np.float32)

    er = np.broadcast_to(emb1d(rows)[:, None, :], (rows, cols, d // 2))
    ec = np.broadcast_to(emb1d(cols)[None, :, :], (rows, cols, d // 2))
    return np.concatenate([er, ec], axis=-1).reshape(rows * cols, d).astype(np.float32)


def _rcnt_table():
    tab = np.zeros((4, PLEN), np.float32)
    for g, win in enumerate((2, 4, 8, 16)):
        t = np.arange(CTX)
        lo = np.maximum(t - win // 2, 0)
        hi = np.minimum(t + win // 2, CTX)
        tab[g, 16:16 + CTX] = 1.0 / (hi - lo)
        t = np.arange(64)
        lo = np.maximum(t - win // 2, 0)
        hi = np.minimum(t + win // 2, 64)
        r = (1.0 / (hi - lo)).astype(np.float32)
        for row in range(32):
            tab[g, 288 + row * 96 + 16:288 + row * 96 + 80] = r
    return tab


def _fm(a):
    return np.ascontiguousarray(a.T.reshape(NCH, 128, a.shape[0]).transpose(1, 0, 2))


def _pv(v):
    n = v.shape[-1] // 128
    r = v.reshape(v.shape[:-1] + (n, 128))
    return np.moveaxis(r, -1, 0)


_NC_CACHE = {}


def prep_inputs(inp):
    f = lambda k: np.asarray(inp[k], dtype=np.float32)
    x, c, ctx, c_ctx = f("x"), f("c"), f("ctx"), f("c_ctx")
    B = x.shape[0]
    pos = _grid_sincos(SEQ // 64, 64, D)
    posT = np.zeros((128, NCH, T), np.float32)
    posT[:, :, CTX:] = _fm(pos)
    vec = np.zeros((128, DEPTH, NV), np.float32)
    for l in range(DEPTH):
        vec[:, l, V_BMOD:V_BMOD + 48] = _pv(f("b_mod")[l])
        vec[:, l, V_CW:V_CW + 16] = np.moveaxis(_pv(f("conv_w")[l]), 1, 2).reshape(128, 16)
        vec[:, l, V_CB:V_CB + 4] = _pv(f("conv_b")[l])
        vec[:, l, V_GAB:V_GAB + 8] = _pv(f("gate_a_b")[l]).reshape(128, 8)
        vec[:, l, V_GXB:V_GXB + 8] = _pv(f("gate_x_b")[l]).reshape(128, 8)
        vec[:, l, V_LAM:V_LAM + 8] = _pv(f("lru_lambda")[l]).reshape(128, 8)
        vec[:, l, V_PB:V_PB + 4] = _pv(f("pool_b")[l])
        vec[:, l, V_PS:V_PS + 4] = _pv(f("pool_scale")[l])
        vec[:, l, V_L1G:V_L1G + 8] = _pv(f("ln1_g")[l])
        vec[:, l, V_L1B:V_L1B + 8] = _pv(f("ln1_b")[l])
        vec[:, l, V_L2G:V_L2G + 8] = _pv(f("ln2_g")[l])
        vec[:, l, V_L2B:V_L2B + 8] = _pv(f("ln2_b")[l])
        vec[:, l, V_B1:V_B1 + 512] = _pv(f("exp_b1")[l]).reshape(128, 512)
    gbd = np.zeros((DEPTH, 4, 128, 4, 128), np.float32)
    ga, gx = f("gate_a_w"), f("gate_x_w")
    for l in range(DEPTH):
        for d in range(2):
            for ax, gw in enumerate((ga, gx)):
                for hh in range(8):
                    cc, o = hh // 2, (hh % 2) * 64
                    gbd[l, cc, o:o + 64, d * 2 + ax, o:o + 64] = gw[l, d, hh]
    shared = {
        "posT": posT, "vecs": vec, "ident": np.eye(128, dtype=np.float32), "rb": f("router_b"), "rw": f("router_w"),
        "w_mod": f("w_mod"), "w_in": f("w_in"), "w_out": f("w_out"), "gbd": gbd, "pool_w": f("pool_w"),
        "rcnt": _rcnt_table(), "exp_w1": f("exp_w1"), "exp_w2": f("exp_w2"), "exp_b2": f("exp_b2"),
    }
    in_maps = []
    for b in range(B):
        m = dict(shared)
        m["xT"] = _fm(np.concatenate([ctx[b], x[b]], axis=0))
        cc = np.stack([c[b], c_ctx], axis=-1)
        m["cT"] = np.ascontiguousarray(cc.reshape(NCH, 128, 2).transpose(1, 0, 2).reshape(128, 16))
        in_maps.append(m)
    return in_maps


def kernel(**inputs):
    in_maps = prep_inputs(inputs)
    if "nc" not in _NC_CACHE:
        _NC_CACHE["nc"] = build_program()
    nc = _NC_CACHE["nc"]
    res = run_bass_kernel_spmd(nc, in_maps, core_ids=list(range(len(in_maps))))
    outs = []
    for r in res.results:
        o = np.asarray(r["outT"], dtype=np.float32)
        outs.append(o.transpose(2, 1, 0).reshape(SEQ, D))
    return np.stack(outs, axis=0).astype(np.float32)
```

```python
import contextlib
import numpy as np
import concourse.bass as bass
import concourse.mybir as mybir
from concourse.bass_utils import run_bass_kernel_spmd

F32 = mybir.dt.float32
BF16 = mybir.dt.bfloat16
ALU = mybir.AluOpType
AF = mybir.ActivationFunctionType
AX = mybir.AxisListType

D = 1024
NCH = 8
T = 2304
CTX = 256
SEQ = 2048
DEPTH = 2
NE = 32
TL = [(0, 256, 1), (256, 512, 0), (768, 512, 0), (1280, 512, 0), (1792, 512, 0)]
ALPHA = float((2.0 * DEPTH) ** 0.25)
LN_EPS = 1e-5
S7 = float(1.0 / (1.0 + np.exp(-1.702 * 7.0)))
NV = 644
V_BMOD, V_CW, V_CB, V_GAB, V_GXB, V_LAM, V_PB, V_PS = 0, 48, 64, 68, 76, 84, 92, 96
V_L1G, V_L1B, V_L2G, V_L2B, V_B1 = 100, 108, 116, 124, 132
ND = 192 + 8 + 256 + 256 + 8
DV_MOD, DV_C8, DV_B1S, DV_B1B1, DV_C8X2 = 0, 192, 200, 456, 712
PLEN = 288 + 32 * 96
XRLEN = 2320
CLIP_ENG = "pool"
FINAL_ENG = "pool"


class Sched:
    ENGS = ("pe", "act", "dve", "pool", "sp")

    def __init__(self, nc, stack):
        self.nc = nc
        self.stack = stack
        self.q = {e: [] for e in self.ENGS}
        self.sems = {}
        self.cnt = {}
        self.known = {e: {} for e in self.ENGS}
        self.res = {}

    def sem(self, key):
        if key not in self.sems:
            name = "s_" + "_".join(str(k) for k in (key if isinstance(key, tuple) else (key,)))
            self.sems[key] = self.stack.enter_context(self.nc.semaphore(name))
        return self.sems[key]

    def _deps(self, R, W):
        deps = {}

        def add(k, c):
            if deps.get(k, 0) < c:
                deps[k] = c

        for r in R:
            ent = self.res.get(r)
            if ent and ent[0]:
                add(*ent[0])
        for w in W:
            ent = self.res.get(w)
            if ent:
                if ent[0]:
                    add(*ent[0])
                for k, c in ent[1].items():
                    add(k, c)
        return deps

    def _commit(self, tok, R, W):
        for r in R:
            ent = self.res.setdefault(r, [None, {}])
            if ent[1].get(tok[0], 0) < tok[1]:
                ent[1][tok[0]] = tok[1]
        for w in W:
            self.res[w] = [tok, {}]

    def _waits(self, eng, deps):
        kn = self.known[eng]
        for k, c in deps.items():
            if k == eng and eng == "pe":
                continue
            if kn.get(k, 0) < c:
                self.sem(k)
                self.q[eng].append(("w", k, c))
                kn[k] = c

    def op(self, eng, fn, R=(), W=(), after=()):
        deps = self._deps(R, W)
        for k, c in after:
            if deps.get(k, 0) < c:
                deps[k] = c
        self._waits(eng, deps)
        self.sem(eng)
        self.cnt[eng] = self.cnt.get(eng, 0) + 1
        tok = (eng, self.cnt[eng])
        self.q[eng].append(("o", fn))
        self._commit(tok, R, W)
        return tok

    def dma(self, q, out, in_, R=(), W=(), key=None):
        self._waits(q, self._deps(R, W))
        self.sem(key)
        self.cnt[key] = self.cnt.get(key, 0) + 16
        tok = (key, self.cnt[key])
        self.q[q].append(("d", out, in_, key))
        self._commit(tok, R, W)
        return tok

    def barrier(self):
        snap = dict(self.cnt)
        for eng in self.ENGS:
            kn = self.known[eng]
            for k, c in snap.items():
                if k == eng:
                    continue
                if kn.get(k, 0) < c:
                    self.q[eng].append(("w", k, c))
                    kn[k] = c

    def replay(self, eng, h):
        for it in self.q[eng]:
            if it[0] == "w":
                h.wait_ge(self.sems[it[1]], it[2])
            elif it[0] == "o":
                it[1](h).then_inc(self.sems[eng], 1)
            else:
                h.dma_start(out=it[1], in_=it[2]).then_inc(self.sems[it[3]], 16)


def build_program(stop=None):
    nc = bass.Bass("TRN2", target_bir_lowering=False)
    dram = {}

    def din(name, shape):
        dram[name] = nc.dram_tensor(name, list(shape), F32, kind="ExternalInput").ap()
        return dram[name]

    xT = din("xT", [128, NCH, T])
    posT = din("posT", [128, NCH, T])
    cT = din("cT", [128, 16])
    vecs = din("vecs", [128, DEPTH, NV])
    ident_d = din("ident", [128, 128])
    rb_d = din("rb", [DEPTH, NE])
    rw_d = din("rw", [DEPTH, D, NE])
    w_mod = din("w_mod", [DEPTH, D, 6 * D])
    w_in = din("w_in", [DEPTH, D, 1536])
    w_out = din("w_out", [DEPTH, D, D])
    gbd = din("gbd", [DEPTH, 4, 128, 4, 128])
    pool_w = din("pool_w", [DEPTH, 4, 128, 128])
    rcnt_d = din("rcnt", [4, PLEN])
    w1_d = din("exp_w1", [DEPTH, NE, D, 2 * D])
    w2_d = din("exp_w2", [DEPTH, NE, D, D])
    b2_d = din("exp_b2", [DEPTH, NE, D])
    outT = nc.dram_tensor("outT", [128, NCH, SEQ], F32, kind="ExternalOutput").ap()
    gscr = nc.dram_tensor("gscr", [DEPTH, NE, T], F32, kind="Internal").ap()
    dbg_out = {}

    stack = contextlib.ExitStack()
    with stack:
        NPERS = 18432 + 9216 + DEPTH * NV + DEPTH * ND + 128 + 128 + 32 * DEPTH + 16 + 8 + 64
        NPH = 20900
        AR = stack.enter_context(nc.sbuf_tensor("arena", [128, NPERS + NPH], F32))
        ps = [stack.enter_context(nc.psum_tensor(f"ps{i}", [128, 512], F32)) for i in range(8)]
        S = Sched(nc, stack)

        off = [0]

        def palloc(n):
            o = off[0]
            off[0] += n
            return AR[:, o:o + n]

        X2 = palloc(18432)
        X = X2.rearrange("p (c t) -> p c t", c=NCH)
        H = palloc(9216).bitcast(BF16).rearrange("p (c t) -> p c t", c=NCH)
        VEC = palloc(DEPTH * NV).rearrange("p (l v) -> p l v", l=DEPTH)
        DV = palloc(DEPTH * ND).rearrange("p (l v) -> p l v", l=DEPTH)
        ONES = palloc(128)
        IDENT = palloc(128)
        RB = palloc(32 * DEPTH).rearrange("p (l e) -> p l e", l=DEPTH)
        CS = palloc(16)
        CBF = palloc(8).bitcast(BF16).rearrange("p (k s) -> p k s", s=2)
        ONE1 = palloc(8)
        PH0 = off[0]
        assert off[0] <= NPERS

        def phase_alloc():
            o = [PH0]

            def a(n):
                r = AR[:, o[0]:o[0] + n]
                o[0] += n
                assert o[0] <= NPERS + NPH, (o[0], NPERS + NPH)
                return r
            return a

        def vcol(l, j):
            return VEC[:, l, j:j + 1]

        def dcol(l, j):
            return DV[:, l, j:j + 1]

        def modc(l, which, c, s):
            return dcol(l, DV_MOD + (which * 8 + c) * 2 + s)

        def ts(eng, out, in0, s1, s2, op0, op1, R, W):
            if op1 is None:
                return S.op(eng, lambda h: h.tensor_scalar(out=out, in0=in0, scalar1=s1, scalar2=None, op0=op0), R, W)
            return S.op(eng, lambda h: h.tensor_scalar(out=out, in0=in0, scalar1=s1, scalar2=s2, op0=op0, op1=op1), R, W)

        def stt(out, in0, sc, in1, op0, op1, R, W):
            return S.op("dve", lambda h: h.scalar_tensor_tensor(out=out, in0=in0, scalar=sc, in1=in1, op0=op0, op1=op1), R, W)

        def tt(out, in0, in1, op, R, W, eng="dve"):
            return S.op(eng, lambda h: h.tensor_tensor(out=out, in0=in0, in1=in1, op=op), R, W)

        def act(out, in_, func, R, W, bias=None, scale=None, after=()):
            kw = {}
            if bias is not None:
                kw["bias"] = bias
            if scale is not None:
                kw["scale"] = scale
            return S.op("act", lambda h: h.activation(out=out, in_=in_, func=func, **kw), R, W, after)

        def opm(eng, method, R, W, *a, **k):
            return S.op(eng, lambda h: getattr(h, method)(*a, **k), R, W)

        def mm1(out, lhsT, rhs, start, stop, R, W):
            return S.op("pe", lambda h: h.matmul(out, lhsT=lhsT, rhs=rhs, start=start, stop=stop), R, W)

        def mm_group(out, pairs, R, W):
            n = len(pairs)

            def fn(h):
                inst = None
                for i, (l_, r_) in enumerate(pairs):
                    inst = h.matmul(out, lhsT=l_, rhs=r_, start=(i == 0), stop=(i == n - 1))
                return inst
            return S.op("pe", fn, R, W)

        S.dma("sp", VEC, vecs, W=["VEC"], key="ld_vec")
        S.dma("sp", IDENT, ident_d, W=["IDENT"], key="ld_ident")
        S.dma("sp", CS, cT, W=["CS"], key="ld_cs")
        for l in range(DEPTH):
            S.dma("sp", RB[:, l, :], rb_d[l:l + 1, :].broadcast_to([128, NE]), W=[("RB", l)], key=("ld_rb", l))
        opm("dve", "memset", [], ["ONES"], ONES, 1.0 / D)
        opm("dve", "memset", [], ["ONE1"], ONE1, 1.0)
        act(CBF, CS.rearrange("p (k s) -> p k s", s=2), AF.Silu, R=["CS"], W=["CBF"])

        WMB = NPERS + NPH - 256 - 4096
        WM = [AR[:, WMB + i * 2048:WMB + (i + 1) * 2048].bitcast(BF16).rearrange("p (k f) -> p k f", k=8) for i in range(2)]

        def adaln(l):
            psm = ps[2 + l]
            for n in range(12):
                slot = (l * 12 + n) % 2
                S.dma("pool", WM[slot], w_mod[l, :, n * 512:(n + 1) * 512].rearrange("(k p) f -> p k f", p=128),
                      W=[("WM", slot)], key=("ld_wm", slot))
                for qq in range(4):
                    j = n * 4 + qq
                    mm_group(psm[:, j * 2:j * 2 + 2],
                             [(WM[slot][:, kc, qq * 128:(qq + 1) * 128], CBF[:, kc, :]) for kc in range(8)],
                             R=[("WM", slot), "CBF"], W=[("psm", l, j)])
            psv = psm[:, 0:96].rearrange("p (j s) -> p j s", s=2)
            dvm = DV[:, l, DV_MOD:DV_MOD + 96].rearrange("p (j s) -> p j s", s=2)
            allj = [("psm", l, j) for j in range(48)]
            for s in range(2):
                tt(dvm[:, :, s], psv[:, :, s], VEC[:, l, V_BMOD:V_BMOD + 48], ALU.add, R=allj + ["VEC"], W=[("DV", l)])
            for which in (1, 4):
                o = DV_MOD + which * 16
                ts("dve", DV[:, l, o:o + 16], DV[:, l, o:o + 16], 1.0, None, ALU.add, None, R=[("DV", l)], W=[("DV", l)])
            c8 = DV[:, l, DV_C8:DV_C8 + 8]
            act(c8, VEC[:, l, V_LAM:V_LAM + 8], AF.Exp, R=["VEC"], W=[("DV", l)], scale=-1.0)
            ts("dve", c8, c8, 1.0, None, ALU.add, None, R=[("DV", l)], W=[("DV", l)])
            act(c8, c8, AF.Ln, R=[("DV", l)], W=[("DV", l)])
            ts("dve", c8, c8, -8.0, None, ALU.mult, None, R=[("DV", l)], W=[("DV", l)])
            b1v = VEC[:, l, V_B1:V_B1 + 512].rearrange("p (e j) -> p e j", j=16)
            ts("dve", DV[:, l, DV_B1S:DV_B1S + 256].rearrange("p (e j) -> p e j", j=8), b1v[:, :, 0:8], 1.702, None,
               ALU.mult, None, R=["VEC"], W=[("DV", l)])
            ts("dve", DV[:, l, DV_B1B1:DV_B1B1 + 256].rearrange("p (e j) -> p e j", j=8), b1v[:, :, 8:16], 1.0, None,
               ALU.add, None, R=["VEC"], W=[("DV", l)])
            ts("dve", DV[:, l, DV_C8X2:DV_C8X2 + 8], c8, 2.0, None, ALU.mult, None, R=[("DV", l)], W=[("DV", l)])


        def ln_phase(tiles, l_aff, aff_base, l_mod, which_sh, final, router_l=None, entry=False):
            pa = phase_alloc()
            GT = pa(T) if True else None
            SQ = [pa(512) for _ in range(2)]
            M2 = pa(512)
            TMP = [pa(512) for _ in range(8)]
            RSTDS = pa(512)
            H32 = pa(4096).rearrange("p (c t) -> p c t", c=8) if router_l is not None else None
            POSB = [pa(512) for _ in range(2)] if entry else None
            LGS = pa(256)
            RT = pa(256)
            B2 = pa(1024) if router_l is not None else None
            ksq = [0]
            def stats_a(ti):
                t0, n, s = tiles[ti]
                tk = t0
                if entry:
                    S.dma("sp", X[:, :, t0:t0 + n], xT[:, :, t0:t0 + n], W=[("X", c, tk) for c in range(NCH)], key=("ld_x", ti))
                    for c in range(NCH):
                        if s == 0:
                            pb = POSB[c % 2]
                            S.dma("sp", pb[:, 0:n], posT[:, c, t0:t0 + n], W=[("POSB", c % 2)], key=("ld_pos", c % 2))
                            tt(X[:, c, t0:t0 + n], X[:, c, t0:t0 + n], pb[:, 0:n], ALU.add,
                               R=[("X", c, tk), ("POSB", c % 2)], W=[("X", c, tk)])
                s1, s2 = ps[0], ps[1]
                for c in range(NCH):
                    sl = ksq[0] % 2
                    ksq[0] += 1
                    act(SQ[sl][:, 0:n], X[:, c, t0:t0 + n], AF.Square, R=[("X", c, tk)], W=[("SQ", sl)])
                    mm1(s1[:, 0:n], ONES, X[:, c, t0:t0 + n], c == 0, c == NCH - 1, R=[("X", c, tk), "ONES"], W=["ps0"])
                    mm1(s2[:, 0:n], ONES, SQ[sl][:, 0:n], c == 0, c == NCH - 1, R=[("SQ", sl), "ONES"], W=["ps1"])

            def stats_b(ti):
                t0, n, s = tiles[ti]
                s1, s2 = ps[0], ps[1]
                act(M2[:, 0:n], s1[:, 0:n], AF.Square, R=["ps0"], W=["M2"])
                stt(M2[:, 0:n], s2[:, 0:n], LN_EPS, M2[:, 0:n], ALU.add, ALU.subtract, R=["ps1", "M2"], W=["M2"])
                act(M2[:, 0:n], M2[:, 0:n], AF.Sqrt, R=["M2"], W=["M2"])
                rb_, nb_ = 4 + ti % 2, 6 + ti % 2
                RSTD, NMR = ps[rb_], ps[nb_]
                krs, knm = f"ps{rb_}", f"ps{nb_}"
                opm("dve", "reciprocal", ["M2"], ["RSTDS"], out=RSTDS[:, 0:n], in_=M2[:, 0:n])
                stt(NMR[:, 0:n], s1[:, 0:n], -1.0, RSTDS[:, 0:n], ALU.mult, ALU.mult, R=["ps0", "RSTDS"], W=[knm])
                opm("act", "copy", ["RSTDS"], [krs], RSTD[:, 0:n], RSTDS[:, 0:n])

            stats_a(0)
            stats_b(0)
            for ti, (t0, n, s) in enumerate(tiles):
                tk = t0
                if ti + 1 < len(tiles):
                    stats_a(ti + 1)
                rb_, nb_ = 4 + ti % 2, 6 + ti % 2
                RSTD, NMR = ps[rb_], ps[nb_]
                krs, knm = f"ps{rb_}", f"ps{nb_}"
                tms = [TMP[c][:, 0:n] for c in range(NCH)]
                tks = [("TMP", c) for c in range(NCH)]
                for c in range(NCH):
                    tt(tms[c], X[:, c, t0:t0 + n], RSTD[:, 0:n], ALU.mult, R=[("X", c, tk), krs], W=[tks[c]])
                for c in range(NCH):
                    tt(tms[c], tms[c], NMR[:, 0:n], ALU.add, R=[tks[c], knm], W=[tks[c]])
                if l_aff is not None:
                    for c in range(NCH):
                        ts("dve", tms[c], tms[c], vcol(l_aff, aff_base + c), vcol(l_aff, aff_base + 8 + c), ALU.mult, ALU.add,
                           R=[tks[c], "VEC"], W=[tks[c]])
                for c in range(NCH):
                    if l_mod is not None:
                        sc_ = modc(l_mod, which_sh + 1, c, s)
                        sh_ = modc(l_mod, which_sh, c, s)
                        ts("pool", H[:, c, t0:t0 + n], tms[c], sc_, sh_, ALU.mult, ALU.add, R=[tks[c], ("DV", l_mod)], W=[("H", tk)])
                        if router_l is not None:
                            ts("pool", H32[:, c, 0:n], tms[c], sc_, sh_, ALU.mult, ALU.add, R=[tks[c], ("DV", l_mod)], W=["H32"])
                    opm("act", "mul", [tks[c]], [("X", c, tk)], X[:, c, t0:t0 + n], tms[c], 1.0 if final else ALPHA)
                if router_l is not None:
                    RW = router_l["RW"]
                    rl = router_l["l"]
                    nsub = n // 128
                    subs = list(range(nsub))
                    for k in subs:
                        mm_group(ps[2][:, k * 32:(k + 1) * 32], [(H32[:, kc, k * 128:(k + 1) * 128], RW[:, kc, :]) for kc in range(8)],
                                 R=["H32", "RW"], W=["ps2"])
                    lgs = [LGS[:, k * 64:k * 64 + 32] for k in subs]
                    m8s = [LGS[:, k * 64 + 32:k * 64 + 40] for k in subs]
                    nmxs = [LGS[:, k * 64 + 40:k * 64 + 41] for k in subs]
                    sms = [LGS[:, k * 64 + 41:k * 64 + 42] for k in subs]
                    msks = [RT[:, k * 64:k * 64 + 32] for k in subs]
                    ees = [RT[:, k * 64 + 32:k * 64 + 64] for k in subs]
                    for k in subs:
                        tt(lgs[k], ps[2][:, k * 32:(k + 1) * 32], RB[:, rl, :], ALU.add, R=["ps2", ("RB", rl)], W=[("LG", k)])
                    for k in subs:
                        opm("dve", "max", [("LG", k)], [("M8", k)], out=m8s[k], in_=lgs[k])
                    for k in subs:
                        ts("dve", nmxs[k], m8s[k][:, 0:1], -1.0, None, ALU.mult, None, R=[("M8", k)], W=[("NMX", k)])
                    for k in subs:
                        ts("dve", msks[k], lgs[k], m8s[k][:, 3:4], None, ALU.is_ge, None, R=[("LG", k), ("M8", k)], W=[("MSK", k)])
                    for k in subs:
                        act(ees[k], lgs[k], AF.Exp, R=[("LG", k), ("NMX", k)], W=[("EE", k)], bias=nmxs[k], scale=1.0)
                    for k in subs:
                        tt(ees[k], ees[k], msks[k], ALU.mult, R=[("MSK", k), ("EE", k)], W=[("EE", k)])
                    for k in subs:
                        opm("dve", "tensor_reduce", [("EE", k)], [("SM", k)], out=sms[k], in_=ees[k], axis=AX.X, op=ALU.add)
                    for k in subs:
                        opm("dve", "reciprocal", [("SM", k)], [("SM", k)], out=sms[k], in_=sms[k])
                    for k in subs:
                        ts("dve", ees[k], ees[k], sms[k], None, ALU.mult, None, R=[("EE", k), ("SM", k)], W=[("EE", k)])
                    for k in subs:
                        opm("pe", "transpose", [("EE", k), "IDENT"], ["ps3"], ps[3][0:32, k * 128:(k + 1) * 128], ees[k], IDENT)
                    opm("act", "copy", ["ps3"], ["GT"], GT[0:32, t0:t0 + n], ps[3][0:32, 0:n])
                if ti + 1 < len(tiles):
                    stats_b(ti + 1)
            if router_l is not None:
                lr = router_l["l"]
                S.dma("sp", gscr[lr], GT[0:32, :], R=["GT"], W=["GSCR"], key="st_gscr")
                S.dma("sp", B2[0:32, :], b2_d[lr], W=["B2"], key="ld_b2")
                for ti, (t0, n, s) in enumerate(tiles):
                    for dch in range(NCH):
                        b = 4 + dch % 4
                        mm_group(ps[b][:, 0:n], [(B2[0:32, dch * 128:(dch + 1) * 128], GT[0:32, t0:t0 + n])], R=["B2", "GT"], W=[f"ps{b}"])
                        stt(X[:, dch, t0:t0 + n], ps[b][:, 0:n], modc(lr, 5, dch, s), X[:, dch, t0:t0 + n], ALU.mult, ALU.add,
                            R=[f"ps{b}", ("X", dch, t0), ("DV", lr)], W=[("X", dch, t0)])
            return GT

        def mixer_phase(l):
            pa = phase_alloc()
            U = pa(XRLEN)
            RA = pa(XRLEN)
            IB = pa(XRLEN)
            QH = [pa(XRLEN), pa(XRLEN)]
            UBF = pa(1152).bitcast(BF16)
            MIXC = [pa(1152).bitcast(BF16) for _ in range(2)]
            WI = [pa(512).bitcast(BF16).rearrange("p (k f) -> p k f", k=8) for _ in range(2)]
            WO = [pa(512).bitcast(BF16) for _ in range(2)]
            GB = [pa(256).bitcast(BF16).rearrange("p (m f) -> p m f", m=4) for _ in range(2)]
            mtiles = TL if l < DEPTH - 1 else TL[1:]
            wi_n = [0]
            wo_n = [0]

            def load_wi(j):
                slot = wi_n[0] % 2
                wi_n[0] += 1
                S.dma("pool", WI[slot], w_in[l, :, j * 128:(j + 1) * 128].rearrange("(k p) f -> p k f", p=128),
                      W=[("WI", slot)], key=("ld_wi", slot))
                return slot

            def load_wo(kc):
                slot = wo_n[0] % 2
                wo_n[0] += 1
                S.dma("pool", WO[slot], w_out[l, kc * 128:(kc + 1) * 128, :], W=[("WO", slot)], key=("ld_wo", slot))
                return slot

            def zmm(out, slot, t0, n, R, W):
                return mm_group(out, [(WI[slot][:, kc, :], H[:, kc, t0:t0 + n]) for kc in range(8)],
                                R=[("WI", slot), ("H", t0)] + R, W=W)

            def wout_acc_pair(MX, slots):
                for ti, (t0, n, s) in enumerate(mtiles):
                    for dch in range(NCH):
                        b = 4 + (dch % 4)
                        mm_group(ps[b][:, 0:n], [(WO[slots[i]][:, dch * 128:(dch + 1) * 128], MX[i][:, t0:t0 + n]) for i in range(2)],
                                 R=[("WO", slots[0]), ("WO", slots[1]), ("MIXC", 0, t0), ("MIXC", 1, t0)], W=[f"ps{b}"])
                        stt(X[:, dch, t0:t0 + n], ps[b][:, 0:n], modc(l, 2, dch, s), X[:, dch, t0:t0 + n], ALU.mult, ALU.add,
                            R=[f"ps{b}", ("X", dch, t0), ("DV", l)], W=[("X", dch, t0)])

            woslots = [None, None]
            for c in range(4):
                sx = load_wi(c)
                sy = load_wi(4 + c)
                gslot = c % 2
                S.dma("pool", GB[gslot], gbd[l, c], W=[("GB", gslot)], key=("ld_gb", gslot))
                woslots[c % 2] = load_wo(c)
                XR = QH[0]
                for (a, b) in ((0, 4), (260, 264), (2312, 2316)):
                    opm("dve", "memset", [], [("QH", 0)], XR[:, a:b], 0.0)
                for ti, (t0, n, s) in enumerate(TL):
                    b = ti % 2
                    zmm(ps[b][:, 0:n], sx, t0, n, [], [f"ps{b}"])
                    po = t0 + 4 if s == 1 else t0 + 8
                    opm("act", "copy", [f"ps{b}"], [("QH", 0)], XR[:, po:po + n], ps[b][:, 0:n])
                for (u0, un, xb) in ((0, 256, 2), (256, 2048, 262)):
                    for k in range(4):
                        wk = vcol(l, V_CW + c * 4 + k)
                        src = XR[:, xb + k:xb + k + un]
                        if k == 0:
                            ts("dve", U[:, u0:u0 + un], src, wk, vcol(l, V_CB + c), ALU.mult, ALU.add,
                               R=[("QH", 0), "VEC"], W=["U"])
                        else:
                            stt(U[:, u0:u0 + un], src, wk, U[:, u0:u0 + un], ALU.mult, ALU.add, R=[("QH", 0), "U", "VEC"], W=["U"])
                opm("act", "copy", ["U"], ["UBF"], UBF[:, 0:T], U[:, 0:T])
                for d in range(2):
                    Q = QH[d]
                    for ti, (t0, n, s) in enumerate(TL):
                        ba, bx = 2 * (ti % 2), 2 * (ti % 2) + 1
                        mm_group(ps[ba][:, 0:n], [(GB[gslot][:, d * 2 + 0, :], UBF[:, t0:t0 + n])], R=[("GB", gslot), "UBF"], W=[f"ps{ba}"])
                        mm_group(ps[bx][:, 0:n], [(GB[gslot][:, d * 2 + 1, :], UBF[:, t0:t0 + n])], R=[("GB", gslot), "UBF"], W=[f"ps{bx}"])
                        act(RA[:, t0:t0 + n], ps[ba][:, 0:n], AF.Sigmoid, R=[f"ps{ba}", "VEC"], W=["RA"] + [("T1", tj) for tj in range(5)],
                            bias=vcol(l, V_GAB + d * 4 + c))
                        act(IB[:, t0:t0 + n], ps[bx][:, 0:n], AF.Sigmoid, R=[f"ps{bx}", "VEC"], W=["IB"] + [("T2", tj) for tj in range(5)],
                            bias=vcol(l, V_GXB + d * 4 + c))
                    act(Q[:, 0:T], RA[:, 0:T], AF.Exp, R=["RA", ("DV", l)], W=[("QH", d)], scale=dcol(l, DV_C8X2 + d * 4 + c))
                    act(RA[:, 0:T], RA[:, 0:T], AF.Exp, R=["RA", ("DV", l)], W=["RA"], scale=dcol(l, DV_C8 + d * 4 + c))
                    act(Q[:, 0:T], Q[:, 0:T], AF.Sqrt, R=[("QH", d), "ONE1"], W=[("QH", d)], scale=-1.0, bias=ONE1[:, 0:1])
                    tt(IB[:, 0:T], IB[:, 0:T], U[:, 0:T], ALU.mult, R=["IB", "U"], W=["IB"])
                    stt(IB[:, 0:T], Q[:, 0:T], 0.0, IB[:, 0:T], ALU.max, ALU.mult, R=["IB", ("QH", d)], W=["IB"])
                    if d == 0:
                        opm("dve", "tensor_tensor_scan", ["RA", "IB"], [("QH", d)], out=Q[:, 0:T], data0=RA[:, 0:T],
                            data1=IB[:, 0:T], initial=0.0, op0=ALU.mult, op1=ALU.add)
                    else:
                        opm("dve", "tensor_tensor_scan", ["RA", "IB"], [("QH", d)], out=Q[:, 0:CTX][:, ::-1],
                            data0=RA[:, 0:CTX][:, ::-1], data1=IB[:, 0:CTX][:, ::-1], initial=0.0, op0=ALU.mult, op1=ALU.add)
                        scan_tok = opm("dve", "tensor_tensor_scan", ["RA", "IB", ("QH", d)], [("QH", d)], out=Q[:, CTX:T][:, ::-1],
                            data0=RA[:, CTX:T][:, ::-1], data1=IB[:, CTX:T][:, ::-1], initial=Q[:, 0:1], op0=ALU.mult, op1=ALU.add)
                for ti, (t0, n, s) in enumerate(mtiles):
                    b = ti % 2
                    pY = ps[b][:, 0:n]
                    zmm(pY, sy, t0, n, [], [f"ps{b}"])
                    t1 = RA[:, t0:t0 + n]
                    t2 = IB[:, t0:t0 + n]
                    k1, k2 = ("T1", ti), ("T2", ti)
                    act(t1, pY, AF.Square, R=[f"ps{b}"], W=[k1], after=[scan_tok])
                    ts("dve", t1, t1, 0.044715, 1.0, ALU.mult, ALU.add, R=[k1], W=[k1])
                    tt(t1, t1, pY, ALU.mult, R=[k1, f"ps{b}"], W=[k1])
                    act(t1, t1, AF.Sigmoid, R=[k1], W=[k1], scale=1.5957691216057308)
                    tt(t1, t1, pY, ALU.mult, R=[k1, f"ps{b}"], W=[k1])
                    tt(t2, QH[0][:, t0:t0 + n], QH[1][:, t0:t0 + n], ALU.add, R=[("QH", 0), ("QH", 1)], W=[k2])
                    tt(MIXC[c % 2][:, t0:t0 + n], t1, t2, ALU.mult, R=[k1, k2], W=[("MIXC", c % 2, t0)])
                if c % 2 == 1:
                    wout_acc_pair(MIXC, woslots)
            S.barrier()
            pa = phase_alloc()
            P0 = pa(PLEN)
            P1 = pa(PLEN)
            P2 = pa(PLEN)
            RC = pa(PLEN)
            DD = pa(1152).bitcast(BF16)
            MIXC = [pa(1152).bitcast(BF16) for _ in range(2)]
            WI = [pa(512).bitcast(BF16).rearrange("p (k f) -> p k f", k=8) for _ in range(2)]
            WO = [pa(512).bitcast(BF16) for _ in range(2)]
            PW = [pa(64).bitcast(BF16) for _ in range(2)]

            def lat3(P):
                return P[:, 288:PLEN].rearrange("p (r w) -> p r w", w=96)[:, :, 16:80]

            for g in range(4):
                sx = load_wi(8 + g)
                woslots[g % 2] = load_wo(4 + g)
                pslot = g % 2
                S.dma("pool", PW[pslot], pool_w[l, g], W=[("PW", pslot)], key=("ld_pw", pslot))
                S.dma("sp", RC, rcnt_d[g:g + 1, :].broadcast_to([128, PLEN]), W=["RC"], key="ld_rc")
                opm("dve", "memset", [], ["P0"], P0, 0.0)
                for ti, (t0, n, s) in enumerate(TL):
                    b = ti % 2
                    zmm(ps[b][:, 0:n], sx, t0, n, [], [f"ps{b}"])
                    if s == 1:
                        opm("act", "copy", [f"ps{b}"], ["P0"], P0[:, 16:272], ps[b][:, 0:256])
                    else:
                        r0 = (t0 - CTX) // 64
                        opm("act", "copy", [f"ps{b}"], ["P0"], lat3(P0)[:, r0:r0 + 8, :],
                            ps[b][:, 0:512].rearrange("p (r w) -> p r w", w=64))
                src, sname = P0, "P0"
                dsts = [(P1, "P1"), (P2, "P2")]
                for i in range(g + 1):
                    dst, dname = dsts[i % 2]
                    if i == 0:
                        tt(dst[:, 1:PLEN], src[:, 0:PLEN - 1], src[:, 1:PLEN], ALU.add, R=[sname], W=[dname])
                    else:
                        hw = 1 << (i - 1)
                        tt(dst[:, hw:PLEN - hw], src[:, 0:PLEN - 2 * hw], src[:, 2 * hw:PLEN], ALU.add, R=[sname], W=[dname])
                    src, sname = dst, dname
                oth, oname = dsts[(g + 1) % 2]
                tt(oth[:, 16:PLEN - 16], src[:, 16:PLEN - 16], RC[:, 16:PLEN - 16], ALU.mult, R=[sname, "RC"], W=[oname])
                tt(DD[:, 0:256], oth[:, 16:272], P0[:, 16:272], ALU.subtract, R=[oname, "P0"], W=["DD"])
                tt(DD[:, 256:T].rearrange("p (r w) -> p r w", w=64), lat3(oth), lat3(P0), ALU.subtract, R=[oname, "P0"], W=["DD"])
                for ti, (t0, n, s) in enumerate(mtiles):
                    b = 2 + ti % 2
                    mm_group(ps[b][:, 0:n], [(PW[pslot], DD[:, t0:t0 + n])], R=[("PW", pslot), "DD"], W=[f"ps{b}"])
                    ts("dve", MIXC[g % 2][:, t0:t0 + n], ps[b][:, 0:n], vcol(l, V_PB + g), vcol(l, V_PS + g), ALU.add, ALU.mult,
                       R=[f"ps{b}", "VEC"], W=[("MIXC", g % 2, t0)])
                if g % 2 == 1:
                    wout_acc_pair(MIXC, woslots)
            S.barrier()

        def moe_phase(l, GT_unused):
            pa = phase_alloc()
            W1 = [pa(4096).bitcast(BF16).rearrange("p (k f) -> p k f", k=8) for _ in range(2)]
            W2 = [pa(2048).bitcast(BF16).rearrange("p (k f) -> p k f", k=4) for _ in range(2)]
            ACTB = [pa(1024).bitcast(BF16).rearrange("p (k t) -> p k t", k=4) for _ in range(2)]
            SB = [pa(512) for _ in range(2)]
            TB = [pa(512) for _ in range(2)]
            GB_ = [pa(512) for _ in range(2)]
            GS = [pa(512) for _ in range(3)]
            mtiles = (TL[1:] + TL[:1]) if l < DEPTH - 1 else TL[1:]
            pk = [0]
            ok = [0]
            gk = [0]
            ak = [0]
            NU = 2 * NE

            def load_w1(uidx):
                e, hf = uidx // 2, uidx % 2
                ws = uidx % 2
                for half, c0 in ((0, hf * 512), (1, 1024 + hf * 512)):
                    S.dma("pool", W1[ws][:, :, half * 512:(half + 1) * 512],
                          w1_d[l, e, :, c0:c0 + 512].rearrange("(k p) f -> p k f", p=128), W=[("W1", ws, half)], key=("ld_w1", ws, half))

            def load_w2(uidx):
                e, hf = uidx // 2, uidx % 2
                ws = uidx % 2
                S.dma("pool", W2[ws], w2_d[l, e, hf * 512:(hf + 1) * 512, :].rearrange("(k p) f -> p k f", p=128),
                      W=[("W2", ws)], key=("ld_w2", ws))

            def stage2(uidx, tile, aslot):
                t0, n, s = tile
                ws = uidx % 2
                for dch in range(NCH):
                    b = 4 + ok[0] % 4
                    ok[0] += 1
                    mm_group(ps[b][:, 0:n], [(W2[ws][:, fc, dch * 128:(dch + 1) * 128], ACTB[aslot][:, fc, 0:n]) for fc in range(4)],
                             R=[("W2", ws), ("ACTB", aslot)], W=[f"ps{b}"])
                    stt(X[:, dch, t0:t0 + n], ps[b][:, 0:n], modc(l, 5, dch, s), X[:, dch, t0:t0 + n], ALU.mult, ALU.add,
                        R=[f"ps{b}", ("X", dch, t0), ("DV", l)], W=[("X", dch, t0)])

            load_w1(0)
            load_w2(0)
            prev = None
            for uidx in range(NU):
                e, hf = uidx // 2, uidx % 2
                ws = uidx % 2
                if uidx + 1 < NU:
                    load_w1(uidx + 1)
                for ti, tile in enumerate(mtiles):
                    t0, n, s = tile
                    gs = gk[0] % 3
                    gk[0] += 1
                    S.dma("sp", GS[gs][:, 0:n], gscr[l, e:e + 1, t0:t0 + n].broadcast_to([128, n]), R=["GSCR"], W=[("GS", gs)], key=("ld_gs", gs))
                    aslot = ak[0] % 2
                    ak[0] += 1
                    for j in range(4):
                        sl = pk[0] % 2
                        pk[0] += 1
                        pA, pB = ps[2 * sl], ps[2 * sl + 1]
                        ka, kb = f"ps{2 * sl}", f"ps{2 * sl + 1}"
                        mm_group(pA[:, 0:n], [(W1[ws][:, kc, j * 128:(j + 1) * 128], H[:, kc, t0:t0 + n]) for kc in range(8)],
                                 R=[("W1", ws, 0), ("H", t0)], W=[ka])
                        mm_group(pB[:, 0:n], [(W1[ws][:, kc, 512 + j * 128:512 + (j + 1) * 128], H[:, kc, t0:t0 + n]) for kc in range(8)],
                                 R=[("W1", ws, 1), ("H", t0)], W=[kb])
                        cj = e * 8 + hf * 4 + j
                        sb, tb, ab = SB[sl][:, 0:n], TB[sl][:, 0:n], GB_[sl][:, 0:n]
                        act(sb, pA[:, 0:n], AF.Sigmoid, R=[ka, ("DV", l)], W=[("SB", sl)], bias=dcol(l, DV_B1S + cj), scale=1.702)
                        act(ab, pA[:, 0:n], AF.Identity, R=[ka, "VEC"], W=[("GG", sl)], bias=vcol(l, V_B1 + e * 16 + hf * 4 + j), scale=1.0)
                        act(tb, pB[:, 0:n], AF.Identity, R=[kb, ("DV", l)], W=[("TB", sl)], bias=dcol(l, DV_B1B1 + cj), scale=1.0)
                        stt(ab, ab, 7.0, GS[gs][:, 0:n], ALU.min, ALU.mult, R=[("GG", sl), ("GS", gs)], W=[("GG", sl)])
                        stt(ab, sb, S7, ab, ALU.min, ALU.mult, R=[("SB", sl), ("GG", sl)], W=[("GG", sl)])
                        ts(CLIP_ENG, tb, tb, 8.0, -6.0, ALU.min, ALU.max, R=[("TB", sl)], W=[("TB", sl)])
                        tt(ACTB[aslot][:, j, 0:n], ab, tb, ALU.mult, R=[("GG", sl), ("TB", sl)], W=[("ACTB", aslot)], eng=FINAL_ENG)
                    if prev is not None:
                        stage2(*prev)
                    if ti == 0 and uidx + 1 < NU:
                        load_w2(uidx + 1)
                    prev = (uidx, tile, aslot)
            stage2(*prev)
            S.barrier()

        stopped = [stop == "adaln"]

        def cp(name):
            if stop == name:
                stopped[0] = True
            return stopped[0]

        adaln(0)
        if stopped[0]:
            adaln(1)
            S.barrier()
        if not stopped[0]:
            ln_phase(TL, None, None, 0, 0, False, entry=True)
            adaln(1)
            S.barrier()
            cp("entry")
        for l in range(DEPTH):
            if stopped[0]:
                break
            last = l == DEPTH - 1
            mtiles = TL if not last else TL[1:]
            mixer_phase(l)
            if cp(f"mixer{l}"):
                break
            pa_tmp = phase_alloc()
            for _ in range(1):
                pass
            RW = AR[:, NPERS + NPH - 256:NPERS + NPH].rearrange("p (k e) -> p k e", k=8)
            S.dma("sp", RW, rw_d[l].rearrange("(k p) e -> p k e", p=128), W=["RW"], key="ld_rw")
            ln_phase(mtiles, l, V_L1G, l, 3, False, router_l={"RW": RW, "l": l})
            S.barrier()
            if cp(f"ln1_{l}"):
                break
            moe_phase(l, None)
            if cp(f"moe{l}"):
                break
            if not last:
                ln_phase(mtiles, l, V_L2G, l + 1, 0, False)
            else:
                ln_phase(mtiles, l, V_L2G, None, 0, True)
            S.barrier()
        if stop is not None:
            S.barrier()
            dX = nc.dram_tensor("dbgX", [128, NCH, T], F32, kind="ExternalOutput").ap()
            dH = nc.dram_tensor("dbgH", [128, NCH, T], BF16, kind="ExternalOutput").ap()
            dDV = nc.dram_tensor("dbgDV", [128, DEPTH, ND], F32, kind="ExternalOutput").ap()
            dGT = nc.dram_tensor("dbgGT", [32, T], F32, kind="ExternalOutput").ap()
            S.dma("sp", dX, X, key="st_out")
            S.dma("sp", dH, H, key="st_out")
            S.dma("sp", dDV, DV, key="st_out")
            S.dma("sp", dGT, AR[0:32, PH0:PH0 + T], key="st_out")
        for c in range(NCH):
            S.dma("sp", outT[:, c, :], X[:, c, CTX:T], R=[("X", c, t0) for (t0, n, s) in TL[1:]], key="st_out")
        nout = S.cnt["st_out"]
        S.q["sp"].append(("w", "st_out", nout))

        with nc.Block() as block:
            @block.tensor
            def _(h):
                S.replay("pe", h)

            @block.scalar
            def _(h):
                S.replay("act", h)

            @block.vector
            def _(h):
                S.replay("dve", h)

            @block.gpsimd
            def _(h):
                S.replay("pool", h)

            @block.sync
            def _(h):
                S.replay("sp", h)
    return nc


def _grid_sincos(rows, cols, d):
    quarter = d // 4
    omega = (1.0 / (np.float32(10000.0) ** (np.arange(quarter, dtype=np.float32) / np.float32(quarter)))).astype(np.float32)

    def emb1d(n):
        ang = np.arange(n, dtype=np.float32)[:, None] * omega[None, :]
        return np.concatenate([np.sin(ang), np.cos(ang)], axis=-1).astype(np.float32)

    er = np.broadcast_to(emb1d(rows)[:, None, :], (rows, cols, d // 2))
    ec = np.broadcast_to(emb1d(cols)[None, :, :], (rows, cols, d // 2))
    return np.concatenate([er, ec], axis=-1).reshape(rows * cols, d).astype(np.float32)


def _rcnt_table():
    tab = np.zeros((4, PLEN), np.float32)
    for g, win in enumerate((2, 4, 8, 16)):
        t = np.arange(CTX)
        lo = np.maximum(t - win // 2, 0)
        hi = np.minimum(t + win // 2, CTX)
        tab[g, 16:16 + CTX] = 1.0 / (hi - lo)
        t = np.arange(64)
        lo = np.maximum(t - win // 2, 0)
        hi = np.minimum(t + win // 2, 64)
        r = (1.0 / (hi - lo)).astype(np.float32)
        for row in range(32):
            tab[g, 288 + row * 96 + 16:288 + row * 96 + 80] = r
    return tab


def _fm(a):
    return np.ascontiguousarray(a.T.reshape(NCH, 128, a.shape[0]).transpose(1, 0, 2))


def _pv(v):
    n = v.shape[-1] // 128
    r = v.reshape(v.shape[:-1] + (n, 128))
    return np.moveaxis(r, -1, 0)


_NC_CACHE = {}


def prep_inputs(inp):
    f = lambda k: np.asarray(inp[k], dtype=np.float32)
    x, c, ctx, c_ctx = f("x"), f("c"), f("ctx"), f("c_ctx")
    B = x.shape[0]
    pos = _grid_sincos(SEQ // 64, 64, D)
    posT = np.zeros((128, NCH, T), np.float32)
    posT[:, :, CTX:] = _fm(pos)
    vec = np.zeros((128, DEPTH, NV), np.float32)
    for l in range(DEPTH):
        vec[:, l, V_BMOD:V_BMOD + 48] = _pv(f("b_mod")[l])
        vec[:, l, V_CW:V_CW + 16] = np.moveaxis(_pv(f("conv_w")[l]), 1, 2).reshape(128, 16)
        vec[:, l, V_CB:V_CB + 4] = _pv(f("conv_b")[l])
        vec[:, l, V_GAB:V_GAB + 8] = _pv(f("gate_a_b")[l]).reshape(128, 8)
        vec[:, l, V_GXB:V_GXB + 8] = _pv(f("gate_x_b")[l]).reshape(128, 8)
        vec[:, l, V_LAM:V_LAM + 8] = _pv(f("lru_lambda")[l]).reshape(128, 8)
        vec[:, l, V_PB:V_PB + 4] = _pv(f("pool_b")[l])
        vec[:, l, V_PS:V_PS + 4] = _pv(f("pool_scale")[l])
        vec[:, l, V_L1G:V_L1G + 8] = _pv(f("ln1_g")[l])
        vec[:, l, V_L1B:V_L1B + 8] = _pv(f("ln1_b")[l])
        vec[:, l, V_L2G:V_L2G + 8] = _pv(f("ln2_g")[l])
        vec[:, l, V_L2B:V_L2B + 8] = _pv(f("ln2_b")[l])
        vec[:, l, V_B1:V_B1 + 512] = _pv(f("exp_b1")[l]).reshape(128, 512)
    gbd = np.zeros((DEPTH, 4, 128, 4, 128), np.float32)
    ga, gx = f("gate_a_w"), f("gate_x_w")
    for l in range(DEPTH):
        for d in range(2):
            for ax, gw in enumerate((ga, gx)):
                for hh in range(8):
                    cc, o = hh // 2, (hh % 2) * 64
                    gbd[l, cc, o:o + 64, d * 2 + ax, o:o + 64] = gw[l, d, hh]
    shared = {
        "posT": posT, "vecs": vec, "ident": np.eye(128, dtype=np.float32), "rb": f("router_b"), "rw": f("router_w"),
        "w_mod": f("w_mod"), "w_in": f("w_in"), "w_out": f("w_out"), "gbd": gbd, "pool_w": f("pool_w"),
        "rcnt": _rcnt_table(), "exp_w1": f("exp_w1"), "exp_w2": f("exp_w2"), "exp_b2": f("exp_b2"),
    }
    in_maps = []
    for b in range(B):
        m = dict(shared)
        m["xT"] = _fm(np.concatenate([ctx[b], x[b]], axis=0))
        cc = np.stack([c[b], c_ctx], axis=-1)
        m["cT"] = np.ascontiguousarray(cc.reshape(NCH, 128, 2).transpose(1, 0, 2).reshape(128, 16))
        in_maps.append(m)
    return in_maps


def kernel(**inputs):
    in_maps = prep_inputs(inputs)
    if "nc" not in _NC_CACHE:
        _NC_CACHE["nc"] = build_program()
    nc = _NC_CACHE["nc"]
    res = run_bass_kernel_spmd(nc, in_maps, core_ids=list(range(len(in_maps))))
    outs = []
    for r in res.results:
        o = np.asarray(r["outT"], dtype=np.float32)
        outs.append(o.transpose(2, 1, 0).reshape(SEQ, D))
    return np.stack(outs, axis=0).astype(np.float32)
```

```python
import contextlib
import numpy as np
import concourse.bass as bass
import concourse.mybir as mybir
from concourse.bass_utils import run_bass_kernel_spmd

F32 = mybir.dt.float32
BF16 = mybir.dt.bfloat16
ALU = mybir.AluOpType
AF = mybir.ActivationFunctionType
AX = mybir.AxisListType

D = 1024
NCH = 8
T = 2304
CTX = 256
SEQ = 2048
DEPTH = 2
NE = 32
TL = [(0, 256, 1), (256, 512, 0), (768, 512, 0), (1280, 512, 0), (1792, 512, 0)]
ALPHA = float((2.0 * DEPTH) ** 0.25)
LN_EPS = 1e-5
S7 = float(1.0 / (1.0 + np.exp(-1.702 * 7.0)))
NV = 644
V_BMOD, V_CW, V_CB, V_GAB, V_GXB, V_LAM, V_PB, V_PS = 0, 48, 64, 68, 76, 84, 92, 96
V_L1G, V_L1B, V_L2G, V_L2B, V_B1 = 100, 108, 116, 124, 132
ND = 192 + 8 + 256 + 256 + 8
DV_MOD, DV_C8, DV_B1S, DV_B1B1, DV_C8X2 = 0, 192, 200, 456, 712
PLEN = 288 + 32 * 96
XRLEN = 2320
CLIP_ENG = "pool"
FINAL_ENG = "pool"


class Sched:
    ENGS = ("pe", "act", "dve", "pool", "sp")

    def __init__(self, nc, stack):
        self.nc = nc
        self.stack = stack
        self.q = {e: [] for e in self.ENGS}
        self.sems = {}
        self.cnt = {}
        self.known = {e: {} for e in self.ENGS}
        self.res = {}

    def sem(self, key):
        if key not in self.sems:
            name = "s_" + "_".join(str(k) for k in (key if isinstance(key, tuple) else (key,)))
            self.sems[key] = self.stack.enter_context(self.nc.semaphore(name))
        return self.sems[key]

    def _deps(self, R, W):
        deps = {}

        def add(k, c):
            if deps.get(k, 0) < c:
                deps[k] = c

        for r in R:
            ent = self.res.get(r)
            if ent and ent[0]:
                add(*ent[0])
        for w in W:
            ent = self.res.get(w)
            if ent:
                if ent[0]:
                    add(*ent[0])
                for k, c in ent[1].items():
                    add(k, c)
        return deps

    def _commit(self, tok, R, W):
        for r in R:
            ent = self.res.setdefault(r, [None, {}])
            if ent[1].get(tok[0], 0) < tok[1]:
                ent[1][tok[0]] = tok[1]
        for w in W:
            self.res[w] = [tok, {}]

    def _waits(self, eng, deps):
        kn = self.known[eng]
        for k, c in deps.items():
            if k == eng and eng == "pe":
                continue
            if kn.get(k, 0) < c:
                self.sem(k)
                self.q[eng].append(("w", k, c))
                kn[k] = c

    def op(self, eng, fn, R=(), W=(), after=()):
        deps = self._deps(R, W)
        for k, c in after:
            if deps.get(k, 0) < c:
                deps[k] = c
        self._waits(eng, deps)
        self.sem(eng)
        self.cnt[eng] = self.cnt.get(eng, 0) + 1
        tok = (eng, self.cnt[eng])
        self.q[eng].append(("o", fn))
        self._commit(tok, R, W)
        return tok

    def dma(self, q, out, in_, R=(), W=(), key=None):
        self._waits(q, self._deps(R, W))
        self.sem(key)
        self.cnt[key] = self.cnt.get(key, 0) + 16
        tok = (key, self.cnt[key])
        self.q[q].append(("d", out, in_, key))
        self._commit(tok, R, W)
        return tok

    def barrier(self):
        snap = dict(self.cnt)
        for eng in self.ENGS:
            kn = self.known[eng]
            for k, c in snap.items():
                if k == eng:
                    continue
                if kn.get(k, 0) < c:
                    self.q[eng].append(("w", k, c))
                    kn[k] = c

    def replay(self, eng, h):
        for it in self.q[eng]:
            if it[0] == "w":
                h.wait_ge(self.sems[it[1]], it[2])
            elif it[0] == "o":
                it[1](h).then_inc(self.sems[eng], 1)
            else:
                h.dma_start(out=it[1], in_=it[2]).then_inc(self.sems[it[3]], 16)


def build_program(stop=None):
    nc = bass.Bass("TRN2", target_bir_lowering=False)
    dram = {}

    def din(name, shape):
        dram[name] = nc.dram_tensor(name, list(shape), F32, kind="ExternalInput").ap()
        return dram[name]

    xT = din("xT", [128, NCH, T])
    posT = din("posT", [128, NCH, T])
    cT = din("cT", [128, 16])
    vecs = din("vecs", [128, DEPTH, NV])
    ident_d = din("ident", [128, 128])
    rb_d = din("rb", [DEPTH, NE])
    rw_d = din("rw", [DEPTH, D, NE])
    w_mod = din("w_mod", [DEPTH, D, 6 * D])
    w_in = din("w_in", [DEPTH, D, 1536])
    w_out = din("w_out", [DEPTH, D, D])
    gbd = din("gbd", [DEPTH, 4, 128, 4, 128])
    pool_w = din("pool_w", [DEPTH, 4, 128, 128])
    rcnt_d = din("rcnt", [4, PLEN])
    w1_d = din("exp_w1", [DEPTH, NE, D, 2 * D])
    w2_d = din("exp_w2", [DEPTH, NE, D, D])
    b2_d = din("exp_b2", [DEPTH, NE, D])
    outT = nc.dram_tensor("outT", [128, NCH, SEQ], F32, kind="ExternalOutput").ap()
    gscr = nc.dram_tensor("gscr", [DEPTH, NE, T], F32, kind="Internal").ap()
    dbg_out = {}

    stack = contextlib.ExitStack()
    with stack:
        NPERS = 18432 + 9216 + DEPTH * NV + DEPTH * ND + 128 + 128 + 32 * DEPTH + 16 + 8 + 64
        NPH = 20900
        AR = stack.enter_context(nc.sbuf_tensor("arena", [128, NPERS + NPH], F32))
        ps = [stack.enter_context(nc.psum_tensor(f"ps{i}", [128, 512], F32)) for i in range(8)]
        S = Sched(nc, stack)

        off = [0]

        def palloc(n):
            o = off[0]
            off[0] += n
            return AR[:, o:o + n]

        X2 = palloc(18432)
        X = X2.rearrange("p (c t) -> p c t", c=NCH)
        H = palloc(9216).bitcast(BF16).rearrange("p (c t) -> p c t", c=NCH)
        VEC = palloc(DEPTH * NV).rearrange("p (l v) -> p l v", l=DEPTH)
        DV = palloc(DEPTH * ND).rearrange("p (l v) -> p l v", l=DEPTH)
        ONES = palloc(128)
        IDENT = palloc(128)
        RB = palloc(32 * DEPTH).rearrange("p (l e) -> p l e", l=DEPTH)
        CS = palloc(16)
        CBF = palloc(8).bitcast(BF16).rearrange("p (k s) -> p k s", s=2)
        ONE1 = palloc(8)
        PH0 = off[0]
        assert off[0] <= NPERS

        def phase_alloc():
            o = [PH0]

            def a(n):
                r = AR[:, o[0]:o[0] + n]
                o[0] += n
                assert o[0] <= NPERS + NPH, (o[0], NPERS + NPH)
                return r
            return a

        def vcol(l, j):
            return VEC[:, l, j:j + 1]

        def dcol(l, j):
            return DV[:, l, j:j + 1]

        def modc(l, which, c, s):
            return dcol(l, DV_MOD + (which * 8 + c) * 2 + s)

        def ts(eng, out, in0, s1, s2, op0, op1, R, W):
            if op1 is None:
                return S.op(eng, lambda h: h.tensor_scalar(out=out, in0=in0, scalar1=s1, scalar2=None, op0=op0), R, W)
            return S.op(eng, lambda h: h.tensor_scalar(out=out, in0=in0, scalar1=s1, scalar2=s2, op0=op0, op1=op1), R, W)

        def stt(out, in0, sc, in1, op0, op1, R, W):
            return S.op("dve", lambda h: h.scalar_tensor_tensor(out=out, in0=in0, scalar=sc, in1=in1, op0=op0, op1=op1), R, W)

        def tt(out, in0, in1, op, R, W, eng="dve"):
            return S.op(eng, lambda h: h.tensor_tensor(out=out, in0=in0, in1=in1, op=op), R, W)

        def act(out, in_, func, R, W, bias=None, scale=None, after=()):
            kw = {}
            if bias is not None:
                kw["bias"] = bias
            if scale is not None:
                kw["scale"] = scale
            return S.op("act", lambda h: h.activation(out=out, in_=in_, func=func, **kw), R, W, after)

        def opm(eng, method, R, W, *a, **k):
            return S.op(eng, lambda h: getattr(h, method)(*a, **k), R, W)

        def mm1(out, lhsT, rhs, start, stop, R, W):
            return S.op("pe", lambda h: h.matmul(out, lhsT=lhsT, rhs=rhs, start=start, stop=stop), R, W)

        def mm_group(out, pairs, R, W):
            n = len(pairs)

            def fn(h):
                inst = None
                for i, (l_, r_) in enumerate(pairs):
                    inst = h.matmul(out, lhsT=l_, rhs=r_, start=(i == 0), stop=(i == n - 1))
                return inst
            return S.op("pe", fn, R, W)

        S.dma("sp", VEC, vecs, W=["VEC"], key="ld_vec")
        S.dma("sp", IDENT, ident_d, W=["IDENT"], key="ld_ident")
        S.dma("sp", CS, cT, W=["CS"], key="ld_cs")
        for l in range(DEPTH):
            S.dma("sp", RB[:, l, :], rb_d[l:l + 1, :].broadcast_to([128, NE]), W=[("RB", l)], key=("ld_rb", l))
        opm("dve", "memset", [], ["ONES"], ONES, 1.0 / D)
        opm("dve", "memset", [], ["ONE1"], ONE1, 1.0)
        act(CBF, CS.rearrange("p (k s) -> p k s", s=2), AF.Silu, R=["CS"], W=["CBF"])

        WMB = NPERS + NPH - 256 - 4096
        WM = [AR[:, WMB + i * 2048:WMB + (i + 1) * 2048].bitcast(BF16).rearrange("p (k f) -> p k f", k=8) for i in range(2)]

        def adaln(l):
            psm = ps[2 + l]
            for n in range(12):
                slot = (l * 12 + n) % 2
                S.dma("pool", WM[slot], w_mod[l, :, n * 512:(n + 1) * 512].rearrange("(k p) f -> p k f", p=128),
                      W=[("WM", slot)], key=("ld_wm", slot))
                for qq in range(4):
                    j = n * 4 + qq
                    mm_group(psm[:, j * 2:j * 2 + 2],
                             [(WM[slot][:, kc, qq * 128:(qq + 1) * 128], CBF[:, kc, :]) for kc in range(8)],
                             R=[("WM", slot), "CBF"], W=[("psm", l, j)])
            psv = psm[:, 0:96].rearrange("p (j s) -> p j s", s=2)
            dvm = DV[:, l, DV_MOD:DV_MOD + 96].rearrange("p (j s) -> p j s", s=2)
            allj = [("psm", l, j) for j in range(48)]
            for s in range(2):
                tt(dvm[:, :, s], psv[:, :, s], VEC[:, l, V_BMOD:V_BMOD + 48], ALU.add, R=allj + ["VEC"], W=[("DV", l)])
            for which in (1, 4):
                o = DV_MOD + which * 16
                ts("dve", DV[:, l, o:o + 16], DV[:, l, o:o + 16], 1.0, None, ALU.add, None, R=[("DV", l)], W=[("DV", l)])
            c8 = DV[:, l, DV_C8:DV_C8 + 8]
            act(c8, VEC[:, l, V_LAM:V_LAM + 8], AF.Exp, R=["VEC"], W=[("DV", l)], scale=-1.0)
            ts("dve", c8, c8, 1.0, None, ALU.add, None, R=[("DV", l)], W=[("DV", l)])
            act(c8, c8, AF.Ln, R=[("DV", l)], W=[("DV", l)])
            ts("dve", c8, c8, -8.0, None, ALU.mult, None, R=[("DV", l)], W=[("DV", l)])
            b1v = VEC[:, l, V_B1:V_B1 + 512].rearrange("p (e j) -> p e j", j=16)
            ts("dve", DV[:, l, DV_B1S:DV_B1S + 256].rearrange("p (e j) -> p e j", j=8), b1v[:, :, 0:8], 1.702, None,
               ALU.mult, None, R=["VEC"], W=[("DV", l)])
            ts("dve", DV[:, l, DV_B1B1:DV_B1B1 + 256].rearrange("p (e j) -> p e j", j=8), b1v[:, :, 8:16], 1.0, None,
               ALU.add, None, R=["VEC"], W=[("DV", l)])
            ts("dve", DV[:, l, DV_C8X2:DV_C8X2 + 8], c8, 2.0, None, ALU.mult, None, R=[("DV", l)], W=[("DV", l)])


        def ln_phase(tiles, l_aff, aff_base, l_mod, which_sh, final, router_l=None, entry=False):
            pa = phase_alloc()
            GT = pa(T) if True else None
            SQ = [pa(512) for _ in range(2)]
            M2 = pa(512)
            TMP = [pa(512) for _ in range(8)]
            RSTDS = pa(512)
            H32 = pa(4096).rearrange("p (c t) -> p c t", c=8) if router_l is not None else None
            POSB = [pa(512) for _ in range(2)] if entry else None
            LGS = pa(256)
            RT = pa(256)
            B2 = pa(1024) if router_l is not None else None
            ksq = [0]
            def stats_a(ti):
                t0, n, s = tiles[ti]
                tk = t0
                if entry:
                    S.dma("sp", X[:, :, t0:t0 + n], xT[:, :, t0:t0 + n], W=[("X", c, tk) for c in range(NCH)], key=("ld_x", ti))
                    for c in range(NCH):
                        if s == 0:
                            pb = POSB[c % 2]
                            S.dma("sp", pb[:, 0:n], posT[:, c, t0:t0 + n], W=[("POSB", c % 2)], key=("ld_pos", c % 2))
                            tt(X[:, c, t0:t0 + n], X[:, c, t0:t0 + n], pb[:, 0:n], ALU.add,
                               R=[("X", c, tk), ("POSB", c % 2)], W=[("X", c, tk)])
                s1, s2 = ps[0], ps[1]
                for c in range(NCH):
                    sl = ksq[0] % 2
                    ksq[0] += 1
                    act(SQ[sl][:, 0:n], X[:, c, t0:t0 + n], AF.Square, R=[("X", c, tk)], W=[("SQ", sl)])
                    mm1(s1[:, 0:n], ONES, X[:, c, t0:t0 + n], c == 0, c == NCH - 1, R=[("X", c, tk), "ONES"], W=["ps0"])
                    mm1(s2[:, 0:n], ONES, SQ[sl][:, 0:n], c == 0, c == NCH - 1, R=[("SQ", sl), "ONES"], W=["ps1"])

            def stats_b(ti):
                t0, n, s = tiles[ti]
                s1, s2 = ps[0], ps[1]
                act(M2[:, 0:n], s1[:, 0:n], AF.Square, R=["ps0"], W=["M2"])
                stt(M2[:, 0:n], s2[:, 0:n], LN_EPS, M2[:, 0:n], ALU.add, ALU.subtract, R=["ps1", "M2"], W=["M2"])
                act(M2[:, 0:n], M2[:, 0:n], AF.Sqrt, R=["M2"], W=["M2"])
                rb_, nb_ = 4 + ti % 2, 6 + ti % 2
                RSTD, NMR = ps[rb_], ps[nb_]
                krs, knm = f"ps{rb_}", f"ps{nb_}"
                opm("dve", "reciprocal", ["M2"], ["RSTDS"], out=RSTDS[:, 0:n], in_=M2[:, 0:n])
                stt(NMR[:, 0:n], s1[:, 0:n], -1.0, RSTDS[:, 0:n], ALU.mult, ALU.mult, R=["ps0", "RSTDS"], W=[knm])
                opm("act", "copy", ["RSTDS"], [krs], RSTD[:, 0:n], RSTDS[:, 0:n])

            stats_a(0)
            stats_b(0)
            for ti, (t0, n, s) in enumerate(tiles):
                tk = t0
                if ti + 1 < len(tiles):
                    stats_a(ti + 1)
                rb_, nb_ = 4 + ti % 2, 6 + ti % 2
                RSTD, NMR = ps[rb_], ps[nb_]
                krs, knm = f"ps{rb_}", f"ps{nb_}"
                tms = [TMP[c][:, 0:n] for c in range(NCH)]
                tks = [("TMP", c) for c in range(NCH)]
                for c in range(NCH):
                    tt(tms[c], X[:, c, t0:t0 + n], RSTD[:, 0:n], ALU.mult, R=[("X", c, tk), krs], W=[tks[c]])
                for c in range(NCH):
                    tt(tms[c], tms[c], NMR[:, 0:n], ALU.add, R=[tks[c], knm], W=[tks[c]])
                if l_aff is not None:
                    for c in range(NCH):
                        ts("dve", tms[c], tms[c], vcol(l_aff, aff_base + c), vcol(l_aff, aff_base + 8 + c), ALU.mult, ALU.add,
                           R=[tks[c], "VEC"], W=[tks[c]])
                for c in range(NCH):
                    if l_mod is not None:
                        sc_ = modc(l_mod, which_sh + 1, c, s)
                        sh_ = modc(l_mod, which_sh, c, s)
                        ts("pool", H[:, c, t0:t0 + n], tms[c], sc_, sh_, ALU.mult, ALU.add, R=[tks[c], ("DV", l_mod)], W=[("H", tk)])
                        if router_l is not None:
                            ts("pool", H32[:, c, 0:n], tms[c], sc_, sh_, ALU.mult, ALU.add, R=[tks[c], ("DV", l_mod)], W=["H32"])
                    opm("act", "mul", [tks[c]], [("X", c, tk)], X[:, c, t0:t0 + n], tms[c], 1.0 if final else ALPHA)
                if router_l is not None:
                    RW = router_l["RW"]
                    rl = router_l["l"]
                    nsub = n // 128
                    subs = list(range(nsub))
                    for k in subs:
                        mm_group(ps[2][:, k * 32:(k + 1) * 32], [(H32[:, kc, k * 128:(k + 1) * 128], RW[:, kc, :]) for kc in range(8)],
                                 R=["H32", "RW"], W=["ps2"])
                    lgs = [LGS[:, k * 64:k * 64 + 32] for k in subs]
                    m8s = [LGS[:, k * 64 + 32:k * 64 + 40] for k in subs]
                    nmxs = [LGS[:, k * 64 + 40:k * 64 + 41] for k in subs]
                    sms = [LGS[:, k * 64 + 41:k * 64 + 42] for k in subs]
                    msks = [RT[:, k * 64:k * 64 + 32] for k in subs]
                    ees = [RT[:, k * 64 + 32:k * 64 + 64] for k in subs]
                    for k in subs:
                        tt(lgs[k], ps[2][:, k * 32:(k + 1) * 32], RB[:, rl, :], ALU.add, R=["ps2", ("RB", rl)], W=[("LG", k)])
                    for k in subs:
                        opm("dve", "max", [("LG", k)], [("M8", k)], out=m8s[k], in_=lgs[k])
                    for k in subs:
                        ts("dve", nmxs[k], m8s[k][:, 0:1], -1.0, None, ALU.mult, None, R=[("M8", k)], W=[("NMX", k)])
                    for k in subs:
                        ts("dve", msks[k], lgs[k], m8s[k][:, 3:4], None, ALU.is_ge, None, R=[("LG", k), ("M8", k)], W=[("MSK", k)])
                    for k in subs:
                        act(ees[k], lgs[k], AF.Exp, R=[("LG", k), ("NMX", k)], W=[("EE", k)], bias=nmxs[k], scale=1.0)
                    for k in subs:
                        tt(ees[k], ees[k], msks[k], ALU.mult, R=[("MSK", k), ("EE", k)], W=[("EE", k)])
                    for k in subs:
                        opm("dve", "tensor_reduce", [("EE", k)], [("SM", k)], out=sms[k], in_=ees[k], axis=AX.X, op=ALU.add)
                    for k in subs:
                        opm("dve", "reciprocal", [("SM", k)], [("SM", k)], out=sms[k], in_=sms[k])
                    for k in subs:
                        ts("dve", ees[k], ees[k], sms[k], None, ALU.mult, None, R=[("EE", k), ("SM", k)], W=[("EE", k)])
                    for k in subs:
                        opm("pe", "transpose", [("EE", k), "IDENT"], ["ps3"], ps[3][0:32, k * 128:(k + 1) * 128], ees[k], IDENT)
                    opm("act", "copy", ["ps3"], ["GT"], GT[0:32, t0:t0 + n], ps[3][0:32, 0:n])
                if ti + 1 < len(tiles):
                    stats_b(ti + 1)
            if router_l is not None:
                lr = router_l["l"]
                S.dma("sp", gscr[lr], GT[0:32, :], R=["GT"], W=["GSCR"], key="st_gscr")
                S.dma("sp", B2[0:32, :], b2_d[lr], W=["B2"], key="ld_b2")
                for ti, (t0, n, s) in enumerate(tiles):
                    for dch in range(NCH):
                        b = 4 + dch % 4
                        mm_group(ps[b][:, 0:n], [(B2[0:32, dch * 128:(dch + 1) * 128], GT[0:32, t0:t0 + n])], R=["B2", "GT"], W=[f"ps{b}"])
                        stt(X[:, dch, t0:t0 + n], ps[b][:, 0:n], modc(lr, 5, dch, s), X[:, dch, t0:t0 + n], ALU.mult, ALU.add,
                            R=[f"ps{b}", ("X", dch, t0), ("DV", lr)], W=[("X", dch, t0)])
            return GT

        def mixer_phase(l):
            pa = phase_alloc()
            U = pa(XRLEN)
            RA = pa(XRLEN)
            IB = pa(XRLEN)
            QH = [pa(XRLEN), pa(XRLEN)]
            UBF = pa(1152).bitcast(BF16)
            MIXC = [pa(1152).bitcast(BF16) for _ in range(2)]
            WI = [pa(512).bitcast(BF16).rearrange("p (k f) -> p k f", k=8) for _ in range(2)]
            WO = [pa(512).bitcast(BF16) for _ in range(2)]
            GB = [pa(256).bitcast(BF16).rearrange("p (m f) -> p m f", m=4) for _ in range(2)]
            mtiles = TL if l < DEPTH - 1 else TL[1:]
            wi_n = [0]
            wo_n = [0]

            def load_wi(j):
                slot = wi_n[0] % 2
                wi_n[0] += 1
                S.dma("pool", WI[slot], w_in[l, :, j * 128:(j + 1) * 128].rearrange("(k p) f -> p k f", p=128),
                      W=[("WI", slot)], key=("ld_wi", slot))
                return slot

            def load_wo(kc):
                slot = wo_n[0] % 2
                wo_n[0] += 1
                S.dma("pool", WO[slot], w_out[l, kc * 128:(kc + 1) * 128, :], W=[("WO", slot)], key=("ld_wo", slot))
                return slot

            def zmm(out, slot, t0, n, R, W):
                return mm_group(out, [(WI[slot][:, kc, :], H[:, kc, t0:t0 + n]) for kc in range(8)],
                                R=[("WI", slot), ("H", t0)] + R, W=W)

            def wout_acc_pair(MX, slots):
                for ti, (t0, n, s) in enumerate(mtiles):
                    for dch in range(NCH):
                        b = 4 + (dch % 4)
                        mm_group(ps[b][:, 0:n], [(WO[slots[i]][:, dch * 128:(dch + 1) * 128], MX[i][:, t0:t0 + n]) for i in range(2)],
                                 R=[("WO", slots[0]), ("WO", slots[1]), ("MIXC", 0, t0), ("MIXC", 1, t0)], W=[f"ps{b}"])
                        stt(X[:, dch, t0:t0 + n], ps[b][:, 0:n], modc(l, 2, dch, s), X[:, dch, t0:t0 + n], ALU.mult, ALU.add,
                            R=[f"ps{b}", ("X", dch, t0), ("DV", l)], W=[("X", dch, t0)])

            woslots = [None, None]
            for c in range(4):
                sx = load_wi(c)
                sy = load_wi(4 + c)
                gslot = c % 2
                S.dma("pool", GB[gslot], gbd[l, c], W=[("GB", gslot)], key=("ld_gb", gslot))
                woslots[c % 2] = load_wo(c)
                XR = QH[0]
                for (a, b) in ((0, 4), (260, 264), (2312, 2316)):
                    opm("dve", "memset", [], [("QH", 0)], XR[:, a:b], 0.0)
                for ti, (t0, n, s) in enumerate(TL):
                    b = ti % 2
                    zmm(ps[b][:, 0:n], sx, t0, n, [], [f"ps{b}"])
                    po = t0 + 4 if s == 1 else t0 + 8
                    opm("act", "copy", [f"ps{b}"], [("QH", 0)], XR[:, po:po + n], ps[b][:, 0:n])
                for (u0, un, xb) in ((0, 256, 2), (256, 2048, 262)):
                    for k in range(4):
                        wk = vcol(l, V_CW + c * 4 + k)
                        src = XR[:, xb + k:xb + k + un]
                        if k == 0:
                            ts("dve", U[:, u0:u0 + un], src, wk, vcol(l, V_CB + c), ALU.mult, ALU.add,
                               R=[("QH", 0), "VEC"], W=["U"])
                        else:
                            stt(U[:, u0:u0 + un], src, wk, U[:, u0:u0 + un], ALU.mult, ALU.add, R=[("QH", 0), "U", "VEC"], W=["U"])
                opm("act", "copy", ["U"], ["UBF"], UBF[:, 0:T], U[:, 0:T])
                for d in range(2):
                    Q = QH[d]
                    for ti, (t0, n, s) in enumerate(TL):
                        ba, bx = 2 * (ti % 2), 2 * (ti % 2) + 1
                        mm_group(ps[ba][:, 0:n], [(GB[gslot][:, d * 2 + 0, :], UBF[:, t0:t0 + n])], R=[("GB", gslot), "UBF"], W=[f"ps{ba}"])
                        mm_group(ps[bx][:, 0:n], [(GB[gslot][:, d * 2 + 1, :], UBF[:, t0:t0 + n])], R=[("GB", gslot), "UBF"], W=[f"ps{bx}"])
                        act(RA[:, t0:t0 + n], ps[ba][:, 0:n], AF.Sigmoid, R=[f"ps{ba}", "VEC"], W=["RA"] + [("T1", tj) for tj in range(5)],
                            bias=vcol(l, V_GAB + d * 4 + c))
                        act(IB[:, t0:t0 + n], ps[bx][:, 0:n], AF.Sigmoid, R=[f"ps{bx}", "VEC"], W=["IB"] + [("T2", tj) for tj in range(5)],
                            bias=vcol(l, V_GXB + d * 4 + c))
                    act(Q[:, 0:T], RA[:, 0:T], AF.Exp, R=["RA", ("DV", l)], W=[("QH", d)], scale=dcol(l, DV_C8X2 + d * 4 + c))
                    act(RA[:, 0:T], RA[:, 0:T], AF.Exp, R=["RA", ("DV", l)], W=["RA"], scale=dcol(l, DV_C8 + d * 4 + c))
                    act(Q[:, 0:T], Q[:, 0:T], AF.Sqrt, R=[("QH", d), "ONE1"], W=[("QH", d)], scale=-1.0, bias=ONE1[:, 0:1])
                    tt(IB[:, 0:T], IB[:, 0:T], U[:, 0:T], ALU.mult, R=["IB", "U"], W=["IB"])
                    stt(IB[:, 0:T], Q[:, 0:T], 0.0, IB[:, 0:T], ALU.max, ALU.mult, R=["IB", ("QH", d)], W=["IB"])
                    if d == 0:
                        opm("dve", "tensor_tensor_scan", ["RA", "IB"], [("QH", d)], out=Q[:, 0:T], data0=RA[:, 0:T],
                            data1=IB[:, 0:T], initial=0.0, op0=ALU.mult, op1=ALU.add)
                    else:
                        opm("dve", "tensor_tensor_scan", ["RA", "IB"], [("QH", d)], out=Q[:, 0:CTX][:, ::-1],
                            data0=RA[:, 0:CTX][:, ::-1], data1=IB[:, 0:CTX][:, ::-1], initial=0.0, op0=ALU.mult, op1=ALU.add)
                        scan_tok = opm("dve", "tensor_tensor_scan", ["RA", "IB", ("QH", d)], [("QH", d)], out=Q[:, CTX:T][:, ::-1],
                            data0=RA[:, CTX:T][:, ::-1], data1=IB[:, CTX:T][:, ::-1], initial=Q[:, 0:1], op0=ALU.mult, op1=ALU.add)
                for ti, (t0, n, s) in enumerate(mtiles):
                    b = ti % 2
                    pY = ps[b][:, 0:n]
                    zmm(pY, sy, t0, n, [], [f"ps{b}"])
                    t1 = RA[:, t0:t0 + n]
                    t2 = IB[:, t0:t0 + n]
                    k1, k2 = ("T1", ti), ("T2", ti)
                    act(t1, pY, AF.Square, R=[f"ps{b}"], W=[k1], after=[scan_tok], scale=0.21145921592590276)
                    stt(t1, t1, 1.0, pY, ALU.add, ALU.mult, R=[k1, f"ps{b}"], W=[k1])
                    act(t1, t1, AF.Sigmoid, R=[k1], W=[k1], scale=1.5957691216057308)
                    tt(t1, t1, pY, ALU.mult, R=[k1, f"ps{b}"], W=[k1])
                    tt(t2, QH[0][:, t0:t0 + n], QH[1][:, t0:t0 + n], ALU.add, R=[("QH", 0), ("QH", 1)], W=[k2], eng="pool")
                    tt(MIXC[c % 2][:, t0:t0 + n], t1, t2, ALU.mult, R=[k1, k2], W=[("MIXC", c % 2, t0)])
                if c % 2 == 1:
                    wout_acc_pair(MIXC, woslots)
            S.barrier()
            pa = phase_alloc()
            P0 = pa(PLEN)
            P1 = pa(PLEN)
            P2 = pa(PLEN)
            RC = pa(PLEN)
            DD = pa(1152).bitcast(BF16)
            MIXC = [pa(1152).bitcast(BF16) for _ in range(2)]
            WI = [pa(512).bitcast(BF16).rearrange("p (k f) -> p k f", k=8) for _ in range(2)]
            WO = [pa(512).bitcast(BF16) for _ in range(2)]
            PW = [pa(64).bitcast(BF16) for _ in range(2)]

            def lat3(P):
                return P[:, 288:PLEN].rearrange("p (r w) -> p r w", w=96)[:, :, 16:80]

            for g in range(4):
                sx = load_wi(8 + g)
                woslots[g % 2] = load_wo(4 + g)
                pslot = g % 2
                S.dma("pool", PW[pslot], pool_w[l, g], W=[("PW", pslot)], key=("ld_pw", pslot))
                S.dma("sp", RC, rcnt_d[g:g + 1, :].broadcast_to([128, PLEN]), W=["RC"], key="ld_rc")
                opm("dve", "memset", [], ["P0"], P0, 0.0)
                for ti, (t0, n, s) in enumerate(TL):
                    b = ti % 2
                    zmm(ps[b][:, 0:n], sx, t0, n, [], [f"ps{b}"])
                    if s == 1:
                        opm("act", "copy", [f"ps{b}"], ["P0"], P0[:, 16:272], ps[b][:, 0:256])
                    else:
                        r0 = (t0 - CTX) // 64
                        opm("act", "copy", [f"ps{b}"], ["P0"], lat3(P0)[:, r0:r0 + 8, :],
                            ps[b][:, 0:512].rearrange("p (r w) -> p r w", w=64))
                src, sname = P0, "P0"
                dsts = [(P1, "P1"), (P2, "P2")]
                for i in range(g + 1):
                    dst, dname = dsts[i % 2]
                    if i == 0:
                        tt(dst[:, 1:PLEN], src[:, 0:PLEN - 1], src[:, 1:PLEN], ALU.add, R=[sname], W=[dname])
                    else:
                        hw = 1 << (i - 1)
                        tt(dst[:, hw:PLEN - hw], src[:, 0:PLEN - 2 * hw], src[:, 2 * hw:PLEN], ALU.add, R=[sname], W=[dname])
                    src, sname = dst, dname
                oth, oname = dsts[(g + 1) % 2]
                tt(oth[:, 16:PLEN - 16], src[:, 16:PLEN - 16], RC[:, 16:PLEN - 16], ALU.mult, R=[sname, "RC"], W=[oname])
                tt(DD[:, 0:256], oth[:, 16:272], P0[:, 16:272], ALU.subtract, R=[oname, "P0"], W=["DD"])
                tt(DD[:, 256:T].rearrange("p (r w) -> p r w", w=64), lat3(oth), lat3(P0), ALU.subtract, R=[oname, "P0"], W=["DD"])
                for ti, (t0, n, s) in enumerate(mtiles):
                    b = 2 + ti % 2
                    mm_group(ps[b][:, 0:n], [(PW[pslot], DD[:, t0:t0 + n])], R=[("PW", pslot), "DD"], W=[f"ps{b}"])
                    ts("dve", MIXC[g % 2][:, t0:t0 + n], ps[b][:, 0:n], vcol(l, V_PB + g), vcol(l, V_PS + g), ALU.add, ALU.mult,
                       R=[f"ps{b}", "VEC"], W=[("MIXC", g % 2, t0)])
                if g % 2 == 1:
                    wout_acc_pair(MIXC, woslots)
            S.barrier()

        W1F = AR[:, PH0 + 14400:PH0 + 18496].bitcast(BF16).rearrange("p (k f) -> p k f", k=8)
        W2F = AR[:, PH0 + 18496:PH0 + 20544].bitcast(BF16).rearrange("p (k f) -> p k f", k=4)

        def moe_prefetch(l):
            for half, c0 in ((0, 0), (1, 1024)):
                S.dma("pool", W1F[:, :, half * 512:(half + 1) * 512],
                      w1_d[l, 0, :, c0:c0 + 512].rearrange("(k p) f -> p k f", p=128), W=[("W1", 0, half)], key=("ld_w1", 0, half))
            S.dma("pool", W2F, w2_d[l, 0, 0:512, :].rearrange("(k p) f -> p k f", p=128), W=[("W2", 0)], key=("ld_w2", 0))

        def moe_phase(l, GT_unused):
            pa = phase_alloc()
            W1 = [W1F, pa(4096).bitcast(BF16).rearrange("p (k f) -> p k f", k=8)]
            W2 = [W2F, pa(2048).bitcast(BF16).rearrange("p (k f) -> p k f", k=4)]
            ACTB = [pa(1024).bitcast(BF16).rearrange("p (k t) -> p k t", k=4) for _ in range(2)]
            SB = [pa(512) for _ in range(2)]
            TB = [pa(512) for _ in range(2)]
            GB_ = [pa(512) for _ in range(2)]
            GS = [pa(512) for _ in range(3)]
            mtiles = (TL[1:] + TL[:1]) if l < DEPTH - 1 else TL[1:]
            pk = [0]
            ok = [0]
            gk = [0]
            ak = [0]
            NU = 2 * NE

            def load_w1(uidx, l=l):
                e, hf = uidx // 2, uidx % 2
                ws = uidx % 2
                for half, c0 in ((0, hf * 512), (1, 1024 + hf * 512)):
                    S.dma("pool", W1[ws][:, :, half * 512:(half + 1) * 512],
                          w1_d[l, e, :, c0:c0 + 512].rearrange("(k p) f -> p k f", p=128), W=[("W1", ws, half)], key=("ld_w1", ws, half))

            def load_w2(uidx):
                e, hf = uidx // 2, uidx % 2
                ws = uidx % 2
                S.dma("pool", W2[ws], w2_d[l, e, hf * 512:(hf + 1) * 512, :].rearrange("(k p) f -> p k f", p=128),
                      W=[("W2", ws)], key=("ld_w2", ws))

            def stage2(uidx, tile, aslot):
                t0, n, s = tile
                ws = uidx % 2
                for dch in range(NCH):
                    b = 4 + ok[0] % 4
                    ok[0] += 1
                    mm_group(ps[b][:, 0:n], [(W2[ws][:, fc, dch * 128:(dch + 1) * 128], ACTB[aslot][:, fc, 0:n]) for fc in range(4)],
                             R=[("W2", ws), ("ACTB", aslot)], W=[f"ps{b}"])
                    stt(X[:, dch, t0:t0 + n], ps[b][:, 0:n], modc(l, 5, dch, s), X[:, dch, t0:t0 + n], ALU.mult, ALU.add,
                        R=[f"ps{b}", ("X", dch, t0), ("DV", l)], W=[("X", dch, t0)])

            prev = None
            for uidx in range(NU):
                e, hf = uidx // 2, uidx % 2
                ws = uidx % 2
                if uidx + 1 < NU:
                    load_w1(uidx + 1)
                for ti, tile in enumerate(mtiles):
                    t0, n, s = tile
                    gs = gk[0] % 3
                    gk[0] += 1
                    S.dma("sp", GS[gs][:, 0:n], gscr[l, e:e + 1, t0:t0 + n].broadcast_to([128, n]), R=["GSCR"], W=[("GS", gs)], key=("ld_gs", gs))
                    aslot = ak[0] % 2
                    ak[0] += 1
                    for j in range(4):
                        sl = pk[0] % 2
                        pk[0] += 1
                        pA, pB = ps[2 * sl], ps[2 * sl + 1]
                        ka, kb = f"ps{2 * sl}", f"ps{2 * sl + 1}"
                        mm_group(pA[:, 0:n], [(W1[ws][:, kc, j * 128:(j + 1) * 128], H[:, kc, t0:t0 + n]) for kc in range(8)],
                                 R=[("W1", ws, 0), ("H", t0)], W=[ka])
                        mm_group(pB[:, 0:n], [(W1[ws][:, kc, 512 + j * 128:512 + (j + 1) * 128], H[:, kc, t0:t0 + n]) for kc in range(8)],
                                 R=[("W1", ws, 1), ("H", t0)], W=[kb])
                        cj = e * 8 + hf * 4 + j
                        sb, tb, ab = SB[sl][:, 0:n], TB[sl][:, 0:n], GB_[sl][:, 0:n]
                        act(sb, pA[:, 0:n], AF.Sigmoid, R=[ka, ("DV", l)], W=[("SB", sl)], bias=dcol(l, DV_B1S + cj), scale=1.702)
                        act(ab, pA[:, 0:n], AF.Identity, R=[ka, "VEC"], W=[("GG", sl)], bias=vcol(l, V_B1 + e * 16 + hf * 4 + j), scale=1.0)
                        act(tb, pB[:, 0:n], AF.Identity, R=[kb, ("DV", l)], W=[("TB", sl)], bias=dcol(l, DV_B1B1 + cj), scale=1.0)
                        stt(ab, ab, 7.0, GS[gs][:, 0:n], ALU.min, ALU.mult, R=[("GG", sl), ("GS", gs)], W=[("GG", sl)])
                        stt(ab, sb, S7, ab, ALU.min, ALU.mult, R=[("SB", sl), ("GG", sl)], W=[("GG", sl)])
                        ts(CLIP_ENG, tb, tb, 8.0, -6.0, ALU.min, ALU.max, R=[("TB", sl)], W=[("TB", sl)])
                        tt(ACTB[aslot][:, j, 0:n], ab, tb, ALU.mult, R=[("GG", sl), ("TB", sl)], W=[("ACTB", aslot)], eng=FINAL_ENG)
                    if prev is not None:
                        stage2(*prev)
                    if ti == 0 and uidx + 1 < NU:
                        load_w2(uidx + 1)
                    prev = (uidx, tile, aslot)
            stage2(*prev)
            S.barrier()

        stopped = [stop == "adaln"]

        def cp(name):
            if stop == name:
                stopped[0] = True
            return stopped[0]

        adaln(0)
        if stopped[0]:
            adaln(1)
            S.barrier()
        if not stopped[0]:
            ln_phase(TL, None, None, 0, 0, False, entry=True)
            adaln(1)
            S.barrier()
            cp("entry")
        for l in range(DEPTH):
            if stopped[0]:
                break
            last = l == DEPTH - 1
            mtiles = TL if not last else TL[1:]
            mixer_phase(l)
            if cp(f"mixer{l}"):
                break
            pa_tmp = phase_alloc()
            for _ in range(1):
                pass
            RW = AR[:, NPERS + NPH - 256:NPERS + NPH].rearrange("p (k e) -> p k e", k=8)
            S.dma("sp", RW, rw_d[l].rearrange("(k p) e -> p k e", p=128), W=["RW"], key="ld_rw")
            moe_prefetch(l)
            ln_phase(mtiles, l, V_L1G, l, 3, False, router_l={"RW": RW, "l": l})
            S.barrier()
            if cp(f"ln1_{l}"):
                break
            moe_phase(l, None)
            if cp(f"moe{l}"):
                break
            if not last:
                ln_phase(mtiles, l, V_L2G, l + 1, 0, False)
            else:
                ln_phase(mtiles, l, V_L2G, None, 0, True)
            S.barrier()
        if stop is not None:
            S.barrier()
            dX = nc.dram_tensor("dbgX", [128, NCH, T], F32, kind="ExternalOutput").ap()
            dH = nc.dram_tensor("dbgH", [128, NCH, T], BF16, kind="ExternalOutput").ap()
            dDV = nc.dram_tensor("dbgDV", [128, DEPTH, ND], F32, kind="ExternalOutput").ap()
            dGT = nc.dram_tensor("dbgGT", [32, T], F32, kind="ExternalOutput").ap()
            S.dma("sp", dX, X, key="st_out")
            S.dma("sp", dH, H, key="st_out")
            S.dma("sp", dDV, DV, key="st_out")
            S.dma("sp", dGT, AR[0:32, PH0:PH0 + T], key="st_out")
        for c in range(NCH):
            S.dma("sp", outT[:, c, :], X[:, c, CTX:T], R=[("X", c, t0) for (t0, n, s) in TL[1:]], key="st_out")
        nout = S.cnt["st_out"]
        S.q["sp"].append(("w", "st_out", nout))

        with nc.Block() as block:
            @block.tensor
            def _(h):
                S.replay("pe", h)

            @block.scalar
            def _(h):
                S.replay("act", h)

            @block.vector
            def _(h):
                S.replay("dve", h)

            @block.gpsimd
            def _(h):
                S.replay("pool", h)

            @block.sync
            def _(h):
                S.replay("sp", h)
    return nc


def _grid_sincos(rows, cols, d):
    quarter = d // 4
    omega = (1.0 / (np.float32(10000.0) ** (np.arange(quarter, dtype=np.float32) / np.float32(quarter)))).astype(np.float32)

    def emb1d(n):
        ang = np.arange(n, dtype=np.float32)[:, None] * omega[None, :]
        return np.concatenate([np.sin(ang), np.cos(ang)], axis=-1).astype(np.float32)

    er = np.broadcast_to(emb1d(rows)[:, None, :], (rows, cols, d // 2))
    ec = np.broadcast_to(emb1d(cols)[None, :, :], (rows, cols, d // 2))
    return np.concatenate([er, ec], axis=-1).reshape(rows * cols, d).astype(np.float32)


def _rcnt_table():
    tab = np.zeros((4, PLEN), np.float32)
    for g, win in enumerate((2, 4, 8, 16)):
        t = np.arange(CTX)
        lo = np.maximum(t - win // 2, 0)
        hi = np.minimum(t + win // 2, CTX)
        tab[g, 16:16 + CTX] = 1.0 / (hi - lo)
        t = np.arange(64)
        lo = np.maximum(t - win // 2, 0)
        hi = np.minimum(t + win // 2, 64)
        r = (1.0 / (hi - lo)).astype(np.float32)
        for row in range(32):
            tab[g, 288 + row * 96 + 16:288 + row * 96 + 80] = r
    return tab


def _fm(a):
    return np.ascontiguousarray(a.T.reshape(NCH, 128, a.shape[0]).transpose(1, 0, 2))


def _pv(v):
    n = v.shape[-1] // 128
    r = v.reshape(v.shape[:-1] + (n, 128))
    return np.moveaxis(r, -1, 0)


_NC_CACHE = {}


def prep_inputs(inp):
    f = lambda k: np.asarray(inp[k], dtype=np.float32)
    x, c, ctx, c_ctx = f("x"), f("c"), f("ctx"), f("c_ctx")
    B = x.shape[0]
    pos = _grid_sincos(SEQ // 64, 64, D)
    posT = np.zeros((128, NCH, T), np.float32)
    posT[:, :, CTX:] = _fm(pos)
    vec = np.zeros((128, DEPTH, NV), np.float32)
    for l in range(DEPTH):
        vec[:, l, V_BMOD:V_BMOD + 48] = _pv(f("b_mod")[l])
        vec[:, l, V_CW:V_CW + 16] = np.moveaxis(_pv(f("conv_w")[l]), 1, 2).reshape(128, 16)
        vec[:, l, V_CB:V_CB + 4] = _pv(f("conv_b")[l])
        vec[:, l, V_GAB:V_GAB + 8] = _pv(f("gate_a_b")[l]).reshape(128, 8)
        vec[:, l, V_GXB:V_GXB + 8] = _pv(f("gate_x_b")[l]).reshape(128, 8)
        vec[:, l, V_LAM:V_LAM + 8] = _pv(f("lru_lambda")[l]).reshape(128, 8)
        vec[:, l, V_PB:V_PB + 4] = _pv(f("pool_b")[l])
        vec[:, l, V_PS:V_PS + 4] = _pv(f("pool_scale")[l])
        vec[:, l, V_L1G:V_L1G + 8] = _pv(f("ln1_g")[l])
        vec[:, l, V_L1B:V_L1B + 8] = _pv(f("ln1_b")[l])
        vec[:, l, V_L2G:V_L2G + 8] = _pv(f("ln2_g")[l])
        vec[:, l, V_L2B:V_L2B + 8] = _pv(f("ln2_b")[l])
        vec[:, l, V_B1:V_B1 + 512] = _pv(f("exp_b1")[l]).reshape(128, 512)
    gbd = np.zeros((DEPTH, 4, 128, 4, 128), np.float32)
    ga, gx = f("gate_a_w"), f("gate_x_w")
    for l in range(DEPTH):
        for d in range(2):
            for ax, gw in enumerate((ga, gx)):
                for hh in range(8):
                    cc, o = hh // 2, (hh % 2) * 64
                    gbd[l, cc, o:o + 64, d * 2 + ax, o:o + 64] = gw[l, d, hh]
    shared = {
        "posT": posT, "vecs": vec, "ident": np.eye(128, dtype=np.float32), "rb": f("router_b"), "rw": f("router_w"),
        "w_mod": f("w_mod"), "w_in": f("w_in"), "w_out": f("w_out"), "gbd": gbd, "pool_w": f("pool_w"),
        "rcnt": _rcnt_table(), "exp_w1": f("exp_w1"), "exp_w2": f("exp_w2"), "exp_b2": f("exp_b2"),
    }
    in_maps = []
    for b in range(B):
        m = dict(shared)
        m["xT"] = _fm(np.concatenate([ctx[b], x[b]], axis=0))
        cc = np.stack([c[b], c_ctx], axis=-1)
        m["cT"] = np.ascontiguousarray(cc.reshape(NCH, 128, 2).transpose(1, 0, 2).reshape(128, 16))
        in_maps.append(m)
    return in_maps


def kernel(**inputs):
    in_maps = prep_inputs(inputs)
    if "nc" not in _NC_CACHE:
        _NC_CACHE["nc"] = build_program()
    nc = _NC_CACHE["nc"]
    res = run_bass_kernel_spmd(nc, in_maps, core_ids=list(range(len(in_maps))))
    outs = []
    for r in res.results:
        o = np.asarray(r["outT"], dtype=np.float32)
        outs.append(o.transpose(2, 1, 0).reshape(SEQ, D))
    return np.stack(outs, axis=0).astype(np.float32)
```

```python
import contextlib
import numpy as np
import concourse.bass as bass
import concourse.mybir as mybir
from concourse.bass_utils import run_bass_kernel_spmd

F32 = mybir.dt.float32
BF16 = mybir.dt.bfloat16
ALU = mybir.AluOpType
AF = mybir.ActivationFunctionType
AX = mybir.AxisListType

D = 1024
NCH = 8
T = 2304
CTX = 256
SEQ = 2048
DEPTH = 2
NE = 32
TL = [(0, 256, 1), (256, 512, 0), (768, 512, 0), (1280, 512, 0), (1792, 512, 0)]
ALPHA = float((2.0 * DEPTH) ** 0.25)
LN_EPS = 1e-5
S7 = float(1.0 / (1.0 + np.exp(-1.702 * 7.0)))
NV = 644
V_BMOD, V_CW, V_CB, V_GAB, V_GXB, V_LAM, V_PB, V_PS = 0, 48, 64, 68, 76, 84, 92, 96
V_L1G, V_L1B, V_L2G, V_L2B, V_B1 = 100, 108, 116, 124, 132
ND = 192 + 8 + 256 + 256 + 8
DV_MOD, DV_C8, DV_B1S, DV_B1B1, DV_C8X2 = 0, 192, 200, 456, 712
PLEN = 288 + 32 * 96
XRLEN = 2320
CLIP_ENG = "pool"
FINAL_ENG = "pool"


class Sched:
    ENGS = ("pe", "act", "dve", "pool", "sp")

    def __init__(self, nc, stack):
        self.nc = nc
        self.stack = stack
        self.q = {e: [] for e in self.ENGS}
        self.sems = {}
        self.cnt = {}
        self.known = {e: {} for e in self.ENGS}
        self.res = {}

    def sem(self, key):
        if key not in self.sems:
            name = "s_" + "_".join(str(k) for k in (key if isinstance(key, tuple) else (key,)))
            self.sems[key] = self.stack.enter_context(self.nc.semaphore(name))
        return self.sems[key]

    def _deps(self, R, W):
        deps = {}

        def add(k, c):
            if deps.get(k, 0) < c:
                deps[k] = c

        for r in R:
            ent = self.res.get(r)
            if ent and ent[0]:
                add(*ent[0])
        for w in W:
            ent = self.res.get(w)
            if ent:
                if ent[0]:
                    add(*ent[0])
                for k, c in ent[1].items():
                    add(k, c)
        return deps

    def _commit(self, tok, R, W):
        for r in R:
            ent = self.res.setdefault(r, [None, {}])
            if ent[1].get(tok[0], 0) < tok[1]:
                ent[1][tok[0]] = tok[1]
        for w in W:
            self.res[w] = [tok, {}]

    def _waits(self, eng, deps):
        kn = self.known[eng]
        for k, c in deps.items():
            if k == eng and eng == "pe":
                continue
            if kn.get(k, 0) < c:
                self.sem(k)
                self.q[eng].append(("w", k, c))
                kn[k] = c

    def op(self, eng, fn, R=(), W=(), after=()):
        deps = self._deps(R, W)
        for k, c in after:
            if deps.get(k, 0) < c:
                deps[k] = c
        self._waits(eng, deps)
        self.sem(eng)
        self.cnt[eng] = self.cnt.get(eng, 0) + 1
        tok = (eng, self.cnt[eng])
        self.q[eng].append(("o", fn))
        self._commit(tok, R, W)
        return tok

    def dma(self, q, out, in_, R=(), W=(), key=None):
        self._waits(q, self._deps(R, W))
        self.sem(key)
        self.cnt[key] = self.cnt.get(key, 0) + 16
        tok = (key, self.cnt[key])
        self.q[q].append(("d", out, in_, key))
        self._commit(tok, R, W)
        return tok

    def barrier(self):
        snap = dict(self.cnt)
        for eng in self.ENGS:
            kn = self.known[eng]
            for k, c in snap.items():
                if k == eng:
                    continue
                if kn.get(k, 0) < c:
                    self.q[eng].append(("w", k, c))
                    kn[k] = c

    def replay(self, eng, h):
        for it in self.q[eng]:
            if it[0] == "w":
                h.wait_ge(self.sems[it[1]], it[2])
            elif it[0] == "o":
                it[1](h).then_inc(self.sems[eng], 1)
            else:
                h.dma_start(out=it[1], in_=it[2]).then_inc(self.sems[it[3]], 16)


def build_program(stop=None):
    nc = bass.Bass("TRN2", target_bir_lowering=False)
    dram = {}

    def din(name, shape):
        dram[name] = nc.dram_tensor(name, list(shape), F32, kind="ExternalInput").ap()
        return dram[name]

    xT = din("xT", [128, NCH, T])
    posT = din("posT", [128, NCH, T])
    cT = din("cT", [128, 16])
    vecs = din("vecs", [128, DEPTH, NV])
    ident_d = din("ident", [128, 128])
    rb_d = din("rb", [DEPTH, NE])
    rw_d = din("rw", [DEPTH, D, NE])
    w_mod = din("w_mod", [DEPTH, D, 6 * D])
    w_in = din("w_in", [DEPTH, D, 1536])
    w_out = din("w_out", [DEPTH, D, D])
    gbd = din("gbd", [DEPTH, 4, 128, 4, 128])
    pool_w = din("pool_w", [DEPTH, 4, 128, 128])
    rcnt_d = din("rcnt", [4, PLEN])
    w1_d = din("exp_w1", [DEPTH, NE, D, 2 * D])
    w2_d = din("exp_w2", [DEPTH, NE, D, D])
    b2_d = din("exp_b2", [DEPTH, NE, D])
    outT = nc.dram_tensor("outT", [128, NCH, SEQ], F32, kind="ExternalOutput").ap()
    gscr = nc.dram_tensor("gscr", [DEPTH, NE, T], F32, kind="Internal").ap()
    dbg_out = {}

    stack = contextlib.ExitStack()
    with stack:
        NPERS = 18432 + 9216 + DEPTH * NV + DEPTH * ND + 128 + 128 + 32 * DEPTH + 16 + 8 + 64
        NPH = 20900
        AR = stack.enter_context(nc.sbuf_tensor("arena", [128, NPERS + NPH], F32))
        ps = [stack.enter_context(nc.psum_tensor(f"ps{i}", [128, 512], F32)) for i in range(8)]
        S = Sched(nc, stack)

        off = [0]

        def palloc(n):
            o = off[0]
            off[0] += n
            return AR[:, o:o + n]

        X2 = palloc(18432)
        X = X2.rearrange("p (c t) -> p c t", c=NCH)
        H = palloc(9216).bitcast(BF16).rearrange("p (c t) -> p c t", c=NCH)
        VEC = palloc(DEPTH * NV).rearrange("p (l v) -> p l v", l=DEPTH)
        DV = palloc(DEPTH * ND).rearrange("p (l v) -> p l v", l=DEPTH)
        ONES = palloc(128)
        IDENT = palloc(128)
        RB = palloc(32 * DEPTH).rearrange("p (l e) -> p l e", l=DEPTH)
        CS = palloc(16)
        CBF = palloc(8).bitcast(BF16).rearrange("p (k s) -> p k s", s=2)
        ONE1 = palloc(8)
        PH0 = off[0]
        assert off[0] <= NPERS

        def phase_alloc():
            o = [PH0]

            def a(n):
                r = AR[:, o[0]:o[0] + n]
                o[0] += n
                assert o[0] <= NPERS + NPH, (o[0], NPERS + NPH)
                return r
            return a

        def vcol(l, j):
            return VEC[:, l, j:j + 1]

        def dcol(l, j):
            return DV[:, l, j:j + 1]

        def modc(l, which, c, s):
            return dcol(l, DV_MOD + (which * 8 + c) * 2 + s)

        def ts(eng, out, in0, s1, s2, op0, op1, R, W):
            if op1 is None:
                return S.op(eng, lambda h: h.tensor_scalar(out=out, in0=in0, scalar1=s1, scalar2=None, op0=op0), R, W)
            return S.op(eng, lambda h: h.tensor_scalar(out=out, in0=in0, scalar1=s1, scalar2=s2, op0=op0, op1=op1), R, W)

        def stt(out, in0, sc, in1, op0, op1, R, W):
            return S.op("dve", lambda h: h.scalar_tensor_tensor(out=out, in0=in0, scalar=sc, in1=in1, op0=op0, op1=op1), R, W)

        def tt(out, in0, in1, op, R, W, eng="dve"):
            return S.op(eng, lambda h: h.tensor_tensor(out=out, in0=in0, in1=in1, op=op), R, W)

        def act(out, in_, func, R, W, bias=None, scale=None, after=()):
            kw = {}
            if bias is not None:
                kw["bias"] = bias
            if scale is not None:
                kw["scale"] = scale
            return S.op("act", lambda h: h.activation(out=out, in_=in_, func=func, **kw), R, W, after)

        def opm(eng, method, R, W, *a, **k):
            return S.op(eng, lambda h: getattr(h, method)(*a, **k), R, W)

        def mm1(out, lhsT, rhs, start, stop, R, W):
            return S.op("pe", lambda h: h.matmul(out, lhsT=lhsT, rhs=rhs, start=start, stop=stop), R, W)

        def mm_group(out, pairs, R, W):
            n = len(pairs)

            def fn(h):
                inst = None
                for i, (l_, r_) in enumerate(pairs):
                    inst = h.matmul(out, lhsT=l_, rhs=r_, start=(i == 0), stop=(i == n - 1))
                return inst
            return S.op("pe", fn, R, W)

        S.dma("sp", VEC, vecs, W=["VEC"], key="ld_vec")
        S.dma("sp", IDENT, ident_d, W=["IDENT"], key="ld_ident")
        S.dma("sp", CS, cT, W=["CS"], key="ld_cs")
        for l in range(DEPTH):
            S.dma("sp", RB[:, l, :], rb_d[l:l + 1, :].broadcast_to([128, NE]), W=[("RB", l)], key=("ld_rb", l))
        opm("dve", "memset", [], ["ONES"], ONES, 1.0 / D)
        opm("dve", "memset", [], ["ONE1"], ONE1, 1.0)
        act(CBF, CS.rearrange("p (k s) -> p k s", s=2), AF.Silu, R=["CS"], W=["CBF"])

        WMB = NPERS + NPH - 256 - 4096
        WM = [AR[:, WMB + i * 2048:WMB + (i + 1) * 2048].bitcast(BF16).rearrange("p (k f) -> p k f", k=8) for i in range(2)]

        def adaln(l):
            psm = ps[2 + l]
            for n in range(12):
                slot = (l * 12 + n) % 2
                S.dma("pool", WM[slot], w_mod[l, :, n * 512:(n + 1) * 512].rearrange("(k p) f -> p k f", p=128),
                      W=[("WM", slot)], key=("ld_wm", slot))
                for qq in range(4):
                    j = n * 4 + qq
                    mm_group(psm[:, j * 2:j * 2 + 2],
                             [(WM[slot][:, kc, qq * 128:(qq + 1) * 128], CBF[:, kc, :]) for kc in range(8)],
                             R=[("WM", slot), "CBF"], W=[("psm", l, j)])
            adaln_finish(l, psm)

        def adaln_finish(l, psm):
            psv = psm[:, 0:96].rearrange("p (j s) -> p j s", s=2)
            dvm = DV[:, l, DV_MOD:DV_MOD + 96].rearrange("p (j s) -> p j s", s=2)
            allj = [("psm", l, j) for j in range(48)]
            for s in range(2):
                tt(dvm[:, :, s], psv[:, :, s], VEC[:, l, V_BMOD:V_BMOD + 48], ALU.add, R=allj + ["VEC"], W=[("DV", l)])
            for which in (1, 4):
                o = DV_MOD + which * 16
                ts("dve", DV[:, l, o:o + 16], DV[:, l, o:o + 16], 1.0, None, ALU.add, None, R=[("DV", l)], W=[("DV", l)])
            c8 = DV[:, l, DV_C8:DV_C8 + 8]
            act(c8, VEC[:, l, V_LAM:V_LAM + 8], AF.Exp, R=["VEC"], W=[("DV", l)], scale=-1.0)
            ts("dve", c8, c8, 1.0, None, ALU.add, None, R=[("DV", l)], W=[("DV", l)])
            act(c8, c8, AF.Ln, R=[("DV", l)], W=[("DV", l)])
            ts("dve", c8, c8, -8.0, None, ALU.mult, None, R=[("DV", l)], W=[("DV", l)])
            b1v = VEC[:, l, V_B1:V_B1 + 512].rearrange("p (e j) -> p e j", j=16)
            ts("dve", DV[:, l, DV_B1S:DV_B1S + 256].rearrange("p (e j) -> p e j", j=8), b1v[:, :, 0:8], 1.702, None,
               ALU.mult, None, R=["VEC"], W=[("DV", l)])
            ts("dve", DV[:, l, DV_B1B1:DV_B1B1 + 256].rearrange("p (e j) -> p e j", j=8), b1v[:, :, 8:16], 1.0, None,
               ALU.add, None, R=["VEC"], W=[("DV", l)])
            ts("dve", DV[:, l, DV_C8X2:DV_C8X2 + 8], c8, 2.0, None, ALU.mult, None, R=[("DV", l)], W=[("DV", l)])


        def ln_phase(tiles, l_aff, aff_base, l_mod, which_sh, final, router_l=None, entry=False):
            pa = phase_alloc()
            GT = pa(T) if True else None
            SQ = [pa(512) for _ in range(2)]
            M2 = pa(512)
            TMP = [pa(512) for _ in range(8)]
            RSTDS = pa(512)
            H32 = pa(4096).rearrange("p (c t) -> p c t", c=8) if router_l is not None else None
            POSB = [pa(512) for _ in range(2)] if entry else None
            LGS = pa(256)
            RT = pa(256)
            B2 = pa(1024) if router_l is not None else None
            ksq = [0]
            def stats_a(ti):
                t0, n, s = tiles[ti]
                tk = t0
                if entry:
                    S.dma("sp", X[:, :, t0:t0 + n], xT[:, :, t0:t0 + n], W=[("X", c, tk) for c in range(NCH)], key=("ld_x", ti))
                    for c in range(NCH):
                        if s == 0:
                            pb = POSB[c % 2]
                            S.dma("sp", pb[:, 0:n], posT[:, c, t0:t0 + n], W=[("POSB", c % 2)], key=("ld_pos", c % 2))
                            tt(X[:, c, t0:t0 + n], X[:, c, t0:t0 + n], pb[:, 0:n], ALU.add,
                               R=[("X", c, tk), ("POSB", c % 2)], W=[("X", c, tk)])
                s1, s2 = ps[0], ps[1]
                for c in range(NCH):
                    sl = ksq[0] % 2
                    ksq[0] += 1
                    act(SQ[sl][:, 0:n], X[:, c, t0:t0 + n], AF.Square, R=[("X", c, tk)], W=[("SQ", sl)])
                    mm1(s1[:, 0:n], ONES, X[:, c, t0:t0 + n], c == 0, c == NCH - 1, R=[("X", c, tk), "ONES"], W=["ps0"])
                    mm1(s2[:, 0:n], ONES, SQ[sl][:, 0:n], c == 0, c == NCH - 1, R=[("SQ", sl), "ONES"], W=["ps1"])

            def stats_b(ti):
                t0, n, s = tiles[ti]
                s1, s2 = ps[0], ps[1]
                act(M2[:, 0:n], s1[:, 0:n], AF.Square, R=["ps0"], W=["M2"])
                stt(M2[:, 0:n], s2[:, 0:n], LN_EPS, M2[:, 0:n], ALU.add, ALU.subtract, R=["ps1", "M2"], W=["M2"])
                act(M2[:, 0:n], M2[:, 0:n], AF.Sqrt, R=["M2"], W=["M2"])
                rb_, nb_ = 4 + ti % 2, 6 + ti % 2
                RSTD, NMR = ps[rb_], ps[nb_]
                krs, knm = f"ps{rb_}", f"ps{nb_}"
                opm("dve", "reciprocal", ["M2"], ["RSTDS"], out=RSTDS[:, 0:n], in_=M2[:, 0:n])
                stt(NMR[:, 0:n], s1[:, 0:n], -1.0, RSTDS[:, 0:n], ALU.mult, ALU.mult, R=["ps0", "RSTDS"], W=[knm])
                opm("act", "copy", ["RSTDS"], [krs], RSTD[:, 0:n], RSTDS[:, 0:n])

            stats_a(0)
            stats_b(0)
            for ti, (t0, n, s) in enumerate(tiles):
                tk = t0
                if ti + 1 < len(tiles):
                    stats_a(ti + 1)
                rb_, nb_ = 4 + ti % 2, 6 + ti % 2
                RSTD, NMR = ps[rb_], ps[nb_]
                krs, knm = f"ps{rb_}", f"ps{nb_}"
                tms = [TMP[c][:, 0:n] for c in range(NCH)]
                tks = [("TMP", c) for c in range(NCH)]
                for c in range(NCH):
                    tt(tms[c], X[:, c, t0:t0 + n], RSTD[:, 0:n], ALU.mult, R=[("X", c, tk), krs], W=[tks[c]])
                for c in range(NCH):
                    tt(tms[c], tms[c], NMR[:, 0:n], ALU.add, R=[tks[c], knm], W=[tks[c]])
                if l_aff is not None:
                    for c in range(NCH):
                        ts("dve", tms[c], tms[c], vcol(l_aff, aff_base + c), vcol(l_aff, aff_base + 8 + c), ALU.mult, ALU.add,
                           R=[tks[c], "VEC"], W=[tks[c]])
                for c in range(NCH):
                    if l_mod is not None:
                        sc_ = modc(l_mod, which_sh + 1, c, s)
                        sh_ = modc(l_mod, which_sh, c, s)
                        ts("pool", H[:, c, t0:t0 + n], tms[c], sc_, sh_, ALU.mult, ALU.add, R=[tks[c], ("DV", l_mod)], W=[("H", tk)])
                        if router_l is not None:
                            ts("pool", H32[:, c, 0:n], tms[c], sc_, sh_, ALU.mult, ALU.add, R=[tks[c], ("DV", l_mod)], W=["H32"])
                    opm("act", "mul", [tks[c]], [("X", c, tk)], X[:, c, t0:t0 + n], tms[c], 1.0 if final else ALPHA)
                if router_l is not None:
                    RW = router_l["RW"]
                    rl = router_l["l"]
                    nsub = n // 128
                    subs = list(range(nsub))
                    for k in subs:
                        mm_group(ps[2][:, k * 32:(k + 1) * 32], [(H32[:, kc, k * 128:(k + 1) * 128], RW[:, kc, :]) for kc in range(8)],
                                 R=["H32", "RW"], W=["ps2"])
                    lgs = [LGS[:, k * 64:k * 64 + 32] for k in subs]
                    m8s = [LGS[:, k * 64 + 32:k * 64 + 40] for k in subs]
                    nmxs = [LGS[:, k * 64 + 40:k * 64 + 41] for k in subs]
                    sms = [LGS[:, k * 64 + 41:k * 64 + 42] for k in subs]
                    msks = [RT[:, k * 64:k * 64 + 32] for k in subs]
                    ees = [RT[:, k * 64 + 32:k * 64 + 64] for k in subs]
                    for k in subs:
                        tt(lgs[k], ps[2][:, k * 32:(k + 1) * 32], RB[:, rl, :], ALU.add, R=["ps2", ("RB", rl)], W=[("LG", k)])
                    for k in subs:
                        opm("dve", "max", [("LG", k)], [("M8", k)], out=m8s[k], in_=lgs[k])
                    for k in subs:
                        ts("dve", nmxs[k], m8s[k][:, 0:1], -1.0, None, ALU.mult, None, R=[("M8", k)], W=[("NMX", k)])
                    for k in subs:
                        ts("dve", msks[k], lgs[k], m8s[k][:, 3:4], None, ALU.is_ge, None, R=[("LG", k), ("M8", k)], W=[("MSK", k)])
                    for k in subs:
                        act(ees[k], lgs[k], AF.Exp, R=[("LG", k), ("NMX", k)], W=[("EE", k)], bias=nmxs[k], scale=1.0)
                    for k in subs:
                        tt(ees[k], ees[k], msks[k], ALU.mult, R=[("MSK", k), ("EE", k)], W=[("EE", k)])
                    for k in subs:
                        opm("dve", "tensor_reduce", [("EE", k)], [("SM", k)], out=sms[k], in_=ees[k], axis=AX.X, op=ALU.add)
                    for k in subs:
                        opm("dve", "reciprocal", [("SM", k)], [("SM", k)], out=sms[k], in_=sms[k])
                    for k in subs:
                        ts("dve", ees[k], ees[k], sms[k], None, ALU.mult, None, R=[("EE", k), ("SM", k)], W=[("EE", k)])
                    for k in subs:
                        opm("pe", "transpose", [("EE", k), "IDENT"], ["ps3"], ps[3][0:32, k * 128:(k + 1) * 128], ees[k], IDENT)
                    opm("act", "copy", ["ps3"], ["GT"], GT[0:32, t0:t0 + n], ps[3][0:32, 0:n])
                if ti + 1 < len(tiles):
                    stats_b(ti + 1)
            if router_l is not None:
                lr = router_l["l"]
                S.dma("sp", gscr[lr], GT[0:32, :], R=["GT"], W=["GSCR"], key="st_gscr")
                S.dma("sp", B2[0:32, :], b2_d[lr], W=["B2"], key="ld_b2")
                for ti, (t0, n, s) in enumerate(tiles):
                    for dch in range(NCH):
                        b = 4 + dch % 4
                        mm_group(ps[b][:, 0:n], [(B2[0:32, dch * 128:(dch + 1) * 128], GT[0:32, t0:t0 + n])], R=["B2", "GT"], W=[f"ps{b}"])
                        stt(X[:, dch, t0:t0 + n], ps[b][:, 0:n], modc(lr, 5, dch, s), X[:, dch, t0:t0 + n], ALU.mult, ALU.add,
                            R=[f"ps{b}", ("X", dch, t0), ("DV", lr)], W=[("X", dch, t0)])
            return GT

        def mixer_phase(l):
            pa = phase_alloc()
            U = pa(XRLEN)
            RA = pa(XRLEN)
            IB = pa(XRLEN)
            QH = [pa(XRLEN), pa(XRLEN)]
            UBF = pa(1152).bitcast(BF16)
            MIXC = [pa(1152).bitcast(BF16) for _ in range(2)]
            WI = [pa(512).bitcast(BF16).rearrange("p (k f) -> p k f", k=8) for _ in range(2)]
            WO = [pa(512).bitcast(BF16) for _ in range(2)]
            GB = [pa(256).bitcast(BF16).rearrange("p (m f) -> p m f", m=4) for _ in range(2)]
            mtiles = TL if l < DEPTH - 1 else TL[1:]
            wi_n = [0]
            wo_n = [0]

            def load_wi(j):
                slot = wi_n[0] % 2
                wi_n[0] += 1
                S.dma("pool", WI[slot], w_in[l, :, j * 128:(j + 1) * 128].rearrange("(k p) f -> p k f", p=128),
                      W=[("WI", slot)], key=("ld_wi", slot))
                return slot

            def load_wo(kc):
                slot = wo_n[0] % 2
                wo_n[0] += 1
                S.dma("pool", WO[slot], w_out[l, kc * 128:(kc + 1) * 128, :], W=[("WO", slot)], key=("ld_wo", slot))
                return slot

            def zmm(out, slot, t0, n, R, W):
                return mm_group(out, [(WI[slot][:, kc, :], H[:, kc, t0:t0 + n]) for kc in range(8)],
                                R=[("WI", slot), ("H", t0)] + R, W=W)

            def wout_acc_pair(MX, slots):
                for ti, (t0, n, s) in enumerate(mtiles):
                    for dch in range(NCH):
                        b = 4 + (dch % 4)
                        mm_group(ps[b][:, 0:n], [(WO[slots[i]][:, dch * 128:(dch + 1) * 128], MX[i][:, t0:t0 + n]) for i in range(2)],
                                 R=[("WO", slots[0]), ("WO", slots[1]), ("MIXC", 0, t0), ("MIXC", 1, t0)], W=[f"ps{b}"])
                        stt(X[:, dch, t0:t0 + n], ps[b][:, 0:n], modc(l, 2, dch, s), X[:, dch, t0:t0 + n], ALU.mult, ALU.add,
                            R=[f"ps{b}", ("X", dch, t0), ("DV", l)], W=[("X", dch, t0)])

            woslots = [None, None]
            for c in range(4):
                sx = load_wi(c)
                sy = load_wi(4 + c)
                gslot = c % 2
                S.dma("pool", GB[gslot], gbd[l, c], W=[("GB", gslot)], key=("ld_gb", gslot))
                woslots[c % 2] = load_wo(c)
                XR = QH[0]
                for (a, b) in ((0, 4), (260, 264), (2312, 2316)):
                    opm("dve", "memset", [], [("QH", 0)], XR[:, a:b], 0.0)
                for ti, (t0, n, s) in enumerate(TL):
                    b = ti % 2
                    zmm(ps[b][:, 0:n], sx, t0, n, [], [f"ps{b}"])
                    po = t0 + 4 if s == 1 else t0 + 8
                    opm("act", "copy", [f"ps{b}"], [("QH", 0)], XR[:, po:po + n], ps[b][:, 0:n])
                for (u0, un, xb) in ((0, 256, 2), (256, 2048, 262)):
                    for k in range(4):
                        wk = vcol(l, V_CW + c * 4 + k)
                        src = XR[:, xb + k:xb + k + un]
                        if k == 0:
                            ts("dve", U[:, u0:u0 + un], src, wk, vcol(l, V_CB + c), ALU.mult, ALU.add,
                               R=[("QH", 0), "VEC"], W=["U"])
                        else:
                            stt(U[:, u0:u0 + un], src, wk, U[:, u0:u0 + un], ALU.mult, ALU.add, R=[("QH", 0), "U", "VEC"], W=["U"])
                opm("act", "copy", ["U"], ["UBF"], UBF[:, 0:T], U[:, 0:T])
                for d in range(2):
                    Q = QH[d]
                    for ti, (t0, n, s) in enumerate(TL):
                        ba, bx = 2 * (ti % 2), 2 * (ti % 2) + 1
                        mm_group(ps[ba][:, 0:n], [(GB[gslot][:, d * 2 + 0, :], UBF[:, t0:t0 + n])], R=[("GB", gslot), "UBF"], W=[f"ps{ba}"])
                        mm_group(ps[bx][:, 0:n], [(GB[gslot][:, d * 2 + 1, :], UBF[:, t0:t0 + n])], R=[("GB", gslot), "UBF"], W=[f"ps{bx}"])
                        act(RA[:, t0:t0 + n], ps[ba][:, 0:n], AF.Sigmoid, R=[f"ps{ba}", "VEC"], W=["RA"] + [("T1", tj) for tj in range(5)],
                            bias=vcol(l, V_GAB + d * 4 + c))
                        act(IB[:, t0:t0 + n], ps[bx][:, 0:n], AF.Sigmoid, R=[f"ps{bx}", "VEC"], W=["IB"] + [("T2", tj) for tj in range(5)],
                            bias=vcol(l, V_GXB + d * 4 + c))
                    act(Q[:, 0:T], RA[:, 0:T], AF.Exp, R=["RA", ("DV", l)], W=[("QH", d)], scale=dcol(l, DV_C8X2 + d * 4 + c))
                    act(RA[:, 0:T], RA[:, 0:T], AF.Exp, R=["RA", ("DV", l)], W=["RA"], scale=dcol(l, DV_C8 + d * 4 + c))
                    act(Q[:, 0:T], Q[:, 0:T], AF.Sqrt, R=[("QH", d), "ONE1"], W=[("QH", d)], scale=-1.0, bias=ONE1[:, 0:1])
                    tt(IB[:, 0:T], IB[:, 0:T], U[:, 0:T], ALU.mult, R=["IB", "U"], W=["IB"])
                    stt(IB[:, 0:T], Q[:, 0:T], 0.0, IB[:, 0:T], ALU.max, ALU.mult, R=["IB", ("QH", d)], W=["IB"])
                    if d == 0:
                        opm("dve", "tensor_tensor_scan", ["RA", "IB"], [("QH", d)], out=Q[:, 0:T], data0=RA[:, 0:T],
                            data1=IB[:, 0:T], initial=0.0, op0=ALU.mult, op1=ALU.add)
                    else:
                        opm("dve", "tensor_tensor_scan", ["RA", "IB"], [("QH", d)], out=Q[:, 0:CTX][:, ::-1],
                            data0=RA[:, 0:CTX][:, ::-1], data1=IB[:, 0:CTX][:, ::-1], initial=0.0, op0=ALU.mult, op1=ALU.add)
                        scan_tok = opm("dve", "tensor_tensor_scan", ["RA", "IB", ("QH", d)], [("QH", d)], out=Q[:, CTX:T][:, ::-1],
                            data0=RA[:, CTX:T][:, ::-1], data1=IB[:, CTX:T][:, ::-1], initial=Q[:, 0:1], op0=ALU.mult, op1=ALU.add)
                for ti, (t0, n, s) in enumerate(mtiles):
                    b = ti % 2
                    pY = ps[b][:, 0:n]
                    zmm(pY, sy, t0, n, [], [f"ps{b}"])
                    t1 = RA[:, t0:t0 + n]
                    t2 = IB[:, t0:t0 + n]
                    k1, k2 = ("T1", ti), ("T2", ti)
                    act(t1, pY, AF.Square, R=[f"ps{b}"], W=[k1], after=[scan_tok], scale=0.21145921592590276)
                    stt(t1, t1, 1.0, pY, ALU.add, ALU.mult, R=[k1, f"ps{b}"], W=[k1])
                    act(t1, t1, AF.Sigmoid, R=[k1], W=[k1], scale=1.5957691216057308)
                    tt(t1, t1, pY, ALU.mult, R=[k1, f"ps{b}"], W=[k1])
                    tt(t2, QH[0][:, t0:t0 + n], QH[1][:, t0:t0 + n], ALU.add, R=[("QH", 0), ("QH", 1)], W=[k2], eng="pool")
                    tt(MIXC[c % 2][:, t0:t0 + n], t1, t2, ALU.mult, R=[k1, k2], W=[("MIXC", c % 2, t0)])
                if c % 2 == 1:
                    wout_acc_pair(MIXC, woslots)
            S.barrier()
            pa = phase_alloc()
            P0 = pa(PLEN)
            P1 = pa(PLEN)
            P2 = pa(PLEN)
            RC = pa(PLEN)
            DD = pa(1152).bitcast(BF16)
            MIXC = [pa(1152).bitcast(BF16) for _ in range(2)]
            WI = [pa(512).bitcast(BF16).rearrange("p (k f) -> p k f", k=8) for _ in range(2)]
            WO = [pa(512).bitcast(BF16) for _ in range(2)]
            PW = [pa(64).bitcast(BF16) for _ in range(2)]

            def lat3(P):
                return P[:, 288:PLEN].rearrange("p (r w) -> p r w", w=96)[:, :, 16:80]

            for g in range(4):
                sx = load_wi(8 + g)
                woslots[g % 2] = load_wo(4 + g)
                pslot = g % 2
                S.dma("pool", PW[pslot], pool_w[l, g], W=[("PW", pslot)], key=("ld_pw", pslot))
                S.dma("sp", RC, rcnt_d[g:g + 1, :].broadcast_to([128, PLEN]), W=["RC"], key="ld_rc")
                opm("dve", "memset", [], ["P0"], P0, 0.0)
                for ti, (t0, n, s) in enumerate(TL):
                    b = ti % 2
                    zmm(ps[b][:, 0:n], sx, t0, n, [], [f"ps{b}"])
                    if s == 1:
                        opm("act", "copy", [f"ps{b}"], ["P0"], P0[:, 16:272], ps[b][:, 0:256])
                    else:
                        r0 = (t0 - CTX) // 64
                        opm("act", "copy", [f"ps{b}"], ["P0"], lat3(P0)[:, r0:r0 + 8, :],
                            ps[b][:, 0:512].rearrange("p (r w) -> p r w", w=64))
                src, sname = P0, "P0"
                dsts = [(P1, "P1"), (P2, "P2")]
                for i in range(g + 1):
                    dst, dname = dsts[i % 2]
                    if i == 0:
                        tt(dst[:, 1:PLEN], src[:, 0:PLEN - 1], src[:, 1:PLEN], ALU.add, R=[sname], W=[dname])
                    else:
                        hw = 1 << (i - 1)
                        tt(dst[:, hw:PLEN - hw], src[:, 0:PLEN - 2 * hw], src[:, 2 * hw:PLEN], ALU.add, R=[sname], W=[dname])
                    src, sname = dst, dname
                oth, oname = dsts[(g + 1) % 2]
                tt(oth[:, 16:PLEN - 16], src[:, 16:PLEN - 16], RC[:, 16:PLEN - 16], ALU.mult, R=[sname, "RC"], W=[oname])
                tt(DD[:, 0:256], oth[:, 16:272], P0[:, 16:272], ALU.subtract, R=[oname, "P0"], W=["DD"])
                tt(DD[:, 256:T].rearrange("p (r w) -> p r w", w=64), lat3(oth), lat3(P0), ALU.subtract, R=[oname, "P0"], W=["DD"])
                for ti, (t0, n, s) in enumerate(mtiles):
                    b = 2 + ti % 2
                    mm_group(ps[b][:, 0:n], [(PW[pslot], DD[:, t0:t0 + n])], R=[("PW", pslot), "DD"], W=[f"ps{b}"])
                    ts("dve", MIXC[g % 2][:, t0:t0 + n], ps[b][:, 0:n], vcol(l, V_PB + g), vcol(l, V_PS + g), ALU.add, ALU.mult,
                       R=[f"ps{b}", "VEC"], W=[("MIXC", g % 2, t0)])
                if g % 2 == 1:
                    wout_acc_pair(MIXC, woslots)
            S.barrier()

        W1F = AR[:, PH0 + 14400:PH0 + 18496].bitcast(BF16).rearrange("p (k f) -> p k f", k=8)
        W2F = AR[:, PH0 + 18496:PH0 + 20544].bitcast(BF16).rearrange("p (k f) -> p k f", k=4)

        def moe_prefetch(l):
            for half, c0 in ((0, 0), (1, 1024)):
                S.dma("pool", W1F[:, :, half * 512:(half + 1) * 512],
                      w1_d[l, 0, :, c0:c0 + 512].rearrange("(k p) f -> p k f", p=128), W=[("W1", 0, half)], key=("ld_w1", 0, half))
            S.dma("pool", W2F, w2_d[l, 0, 0:512, :].rearrange("(k p) f -> p k f", p=128), W=[("W2", 0)], key=("ld_w2", 0))

        def moe_phase(l, GT_unused):
            pa = phase_alloc()
            W1 = [W1F, pa(4096).bitcast(BF16).rearrange("p (k f) -> p k f", k=8)]
            W2 = [W2F, pa(2048).bitcast(BF16).rearrange("p (k f) -> p k f", k=4)]
            ACTB = [pa(1024).bitcast(BF16).rearrange("p (k t) -> p k t", k=4) for _ in range(2)]
            SB = [pa(512) for _ in range(2)]
            TB = [pa(512) for _ in range(2)]
            GB_ = [pa(512) for _ in range(2)]
            GS = [pa(512) for _ in range(3)]
            mtiles = (TL[1:] + TL[:1]) if l < DEPTH - 1 else TL[1:]
            embed = (l + 1 < DEPTH)
            WMS = [pa(512).bitcast(BF16).rearrange("p (k f) -> p k f", k=8) for _ in range(2)] if embed else None
            nob = 3 if embed else 4
            step = [0]

            def adaln_step():
                k = step[0]
                step[0] += 1
                if 1 <= k <= 48:
                    j = k - 1
                    mm_group(ps[7][:, j * 2:j * 2 + 2], [(WMS[j % 2][:, kc, :], CBF[:, kc, :]) for kc in range(8)],
                             R=[("WMS", j % 2), "CBF"], W=[("psm", l + 1, j)])
                    if j == 47:
                        adaln_finish(l + 1, ps[7])
                if k < 48:
                    S.dma("pool", WMS[k % 2], w_mod[l + 1, :, k * 128:(k + 1) * 128].rearrange("(k p) f -> p k f", p=128),
                          W=[("WMS", k % 2)], key=("ld_wms", k % 2))
            pk = [0]
            ok = [0]
            gk = [0]
            ak = [0]
            NU = 2 * NE

            def load_w1(uidx, l=l):
                e, hf = uidx // 2, uidx % 2
                ws = uidx % 2
                for half, c0 in ((0, hf * 512), (1, 1024 + hf * 512)):
                    S.dma("pool", W1[ws][:, :, half * 512:(half + 1) * 512],
                          w1_d[l, e, :, c0:c0 + 512].rearrange("(k p) f -> p k f", p=128), W=[("W1", ws, half)], key=("ld_w1", ws, half))

            def load_w2(uidx):
                e, hf = uidx // 2, uidx % 2
                ws = uidx % 2
                S.dma("pool", W2[ws], w2_d[l, e, hf * 512:(hf + 1) * 512, :].rearrange("(k p) f -> p k f", p=128),
                      W=[("W2", ws)], key=("ld_w2", ws))

            def stage2(uidx, tile, aslot):
                t0, n, s = tile
                ws = uidx % 2
                for dch in range(NCH):
                    b = 4 + ok[0] % nob
                    ok[0] += 1
                    mm_group(ps[b][:, 0:n], [(W2[ws][:, fc, dch * 128:(dch + 1) * 128], ACTB[aslot][:, fc, 0:n]) for fc in range(4)],
                             R=[("W2", ws), ("ACTB", aslot)], W=[f"ps{b}"])
                    stt(X[:, dch, t0:t0 + n], ps[b][:, 0:n], modc(l, 5, dch, s), X[:, dch, t0:t0 + n], ALU.mult, ALU.add,
                        R=[f"ps{b}", ("X", dch, t0), ("DV", l)], W=[("X", dch, t0)])

            prev = None
            for uidx in range(NU):
                e, hf = uidx // 2, uidx % 2
                ws = uidx % 2
                if uidx + 1 < NU:
                    load_w1(uidx + 1)
                for ti, tile in enumerate(mtiles):
                    t0, n, s = tile
                    if embed:
                        adaln_step()
                    gs = gk[0] % 3
                    gk[0] += 1
                    S.dma("sp", GS[gs][:, 0:n], gscr[l, e:e + 1, t0:t0 + n].broadcast_to([128, n]), R=["GSCR"], W=[("GS", gs)], key=("ld_gs", gs))
                    aslot = ak[0] % 2
                    ak[0] += 1
                    for j in range(4):
                        sl = pk[0] % 2
                        pk[0] += 1
                        pA, pB = ps[2 * sl], ps[2 * sl + 1]
                        ka, kb = f"ps{2 * sl}", f"ps{2 * sl + 1}"
                        mm_group(pA[:, 0:n], [(W1[ws][:, kc, j * 128:(j + 1) * 128], H[:, kc, t0:t0 + n]) for kc in range(8)],
                                 R=[("W1", ws, 0), ("H", t0)], W=[ka])
                        mm_group(pB[:, 0:n], [(W1[ws][:, kc, 512 + j * 128:512 + (j + 1) * 128], H[:, kc, t0:t0 + n]) for kc in range(8)],
                                 R=[("W1", ws, 1), ("H", t0)], W=[kb])
                        cj = e * 8 + hf * 4 + j
                        sb, tb, ab = SB[sl][:, 0:n], TB[sl][:, 0:n], GB_[sl][:, 0:n]
                        act(sb, pA[:, 0:n], AF.Sigmoid, R=[ka, ("DV", l)], W=[("SB", sl)], bias=dcol(l, DV_B1S + cj), scale=1.702)
                        act(ab, pA[:, 0:n], AF.Identity, R=[ka, "VEC"], W=[("GG", sl)], bias=vcol(l, V_B1 + e * 16 + hf * 4 + j), scale=1.0)
                        act(tb, pB[:, 0:n], AF.Identity, R=[kb, ("DV", l)], W=[("TB", sl)], bias=dcol(l, DV_B1B1 + cj), scale=1.0)
                        stt(ab, ab, 7.0, GS[gs][:, 0:n], ALU.min, ALU.mult, R=[("GG", sl), ("GS", gs)], W=[("GG", sl)])
                        stt(ab, sb, S7, ab, ALU.min, ALU.mult, R=[("SB", sl), ("GG", sl)], W=[("GG", sl)])
                        ts(CLIP_ENG, tb, tb, 8.0, -6.0, ALU.min, ALU.max, R=[("TB", sl)], W=[("TB", sl)])
                        tt(ACTB[aslot][:, j, 0:n], ab, tb, ALU.mult, R=[("GG", sl), ("TB", sl)], W=[("ACTB", aslot)], eng=FINAL_ENG)
                    if prev is not None:
                        stage2(*prev)
                    if ti == 0 and uidx + 1 < NU:
                        load_w2(uidx + 1)
                    prev = (uidx, tile, aslot)
            stage2(*prev)
            S.barrier()

        stopped = [stop == "adaln"]

        def cp(name):
            if stop == name:
                stopped[0] = True
            return stopped[0]

        adaln(0)
        if stopped[0]:
            adaln(1)
            S.barrier()
        if not stopped[0]:
            ln_phase(TL, None, None, 0, 0, False, entry=True)
            S.barrier()
            cp("entry")
        for l in range(DEPTH):
            if stopped[0]:
                break
            last = l == DEPTH - 1
            mtiles = TL if not last else TL[1:]
            mixer_phase(l)
            if cp(f"mixer{l}"):
                break
            pa_tmp = phase_alloc()
            for _ in range(1):
                pass
            RW = AR[:, NPERS + NPH - 256:NPERS + NPH].rearrange("p (k e) -> p k e", k=8)
            S.dma("sp", RW, rw_d[l].rearrange("(k p) e -> p k e", p=128), W=["RW"], key="ld_rw")
            moe_prefetch(l)
            ln_phase(mtiles, l, V_L1G, l, 3, False, router_l={"RW": RW, "l": l})
            S.barrier()
            if cp(f"ln1_{l}"):
                break
            moe_phase(l, None)
            if cp(f"moe{l}"):
                break
            if not last:
                ln_phase(mtiles, l, V_L2G, l + 1, 0, False)
            else:
                ln_phase(mtiles, l, V_L2G, None, 0, True)
            S.barrier()
        if stop is not None:
            S.barrier()
            dX = nc.dram_tensor("dbgX", [128, NCH, T], F32, kind="ExternalOutput").ap()
            dH = nc.dram_tensor("dbgH", [128, NCH, T], BF16, kind="ExternalOutput").ap()
            dDV = nc.dram_tensor("dbgDV", [128, DEPTH, ND], F32, kind="ExternalOutput").ap()
            dGT = nc.dram_tensor("dbgGT", [32, T], F32, kind="ExternalOutput").ap()
            S.dma("sp", dX, X, key="st_out")
            S.dma("sp", dH, H, key="st_out")
            S.dma("sp", dDV, DV, key="st_out")
            S.dma("sp", dGT, AR[0:32, PH0:PH0 + T], key="st_out")
        for c in range(NCH):
            S.dma("sp", outT[:, c, :], X[:, c, CTX:T], R=[("X", c, t0) for (t0, n, s) in TL[1:]], key="st_out")
        nout = S.cnt["st_out"]
        S.q["sp"].append(("w", "st_out", nout))

        with nc.Block() as block:
            @block.tensor
            def _(h):
                S.replay("pe", h)

            @block.scalar
            def _(h):
                S.replay("act", h)

            @block.vector
            def _(h):
                S.replay("dve", h)

            @block.gpsimd
            def _(h):
                S.replay("pool", h)

            @block.sync
            def _(h):
                S.replay("sp", h)
    return nc


def _grid_sincos(rows, cols, d):
    quarter = d // 4
    omega = (1.0 / (np.float32(10000.0) ** (np.arange(quarter, dtype=np.float32) / np.float32(quarter)))).astype(np.float32)

    def emb1d(n):
        ang = np.arange(n, dtype=np.float32)[:, None] * omega[None, :]
        return np.concatenate([np.sin(ang), np.cos(ang)], axis=-1).astype(np.float32)

    er = np.broadcast_to(emb1d(rows)[:, None, :], (rows, cols, d // 2))
    ec = np.broadcast_to(emb1d(cols)[None, :, :], (rows, cols, d // 2))
    return np.concatenate([er, ec], axis=-1).reshape(rows * cols, d).astype(np.float32)


def _rcnt_table():
    tab = np.zeros((4, PLEN), np.float32)
    for g, win in enumerate((2, 4, 8, 16)):
        t = np.arange(CTX)
        lo = np.maximum(t - win // 2, 0)
        hi = np.minimum(t + win // 2, CTX)
        tab[g, 16:16 + CTX] = 1.0 / (hi - lo)
        t = np.arange(64)
        lo = np.maximum(t - win // 2, 0)
        hi = np.minimum(t + win // 2, 64)
        r = (1.0 / (hi - lo)).astype(np.float32)
        for row in range(32):
            tab[g, 288 + row * 96 + 16:288 + row * 96 + 80] = r
    return tab


def _fm(a):
    return np.ascontiguousarray(a.T.reshape(NCH, 128, a.shape[0]).transpose(1, 0, 2))


def _pv(v):
    n = v.shape[-1] // 128
    r = v.reshape(v.shape[:-1] + (n, 128))
    return np.moveaxis(r, -1, 0)


_NC_CACHE = {}


def prep_inputs(inp):
    f = lambda k: np.asarray(inp[k], dtype=np.float32)
    x, c, ctx, c_ctx = f("x"), f("c"), f("ctx"), f("c_ctx")
    B = x.shape[0]
    pos = _grid_sincos(SEQ // 64, 64, D)
    posT = np.zeros((128, NCH, T), np.float32)
    posT[:, :, CTX:] = _fm(pos)
    vec = np.zeros((128, DEPTH, NV), np.float32)
    for l in range(DEPTH):
        vec[:, l, V_BMOD:V_BMOD + 48] = _pv(f("b_mod")[l])
        vec[:, l, V_CW:V_CW + 16] = np.moveaxis(_pv(f("conv_w")[l]), 1, 2).reshape(128, 16)
        vec[:, l, V_CB:V_CB + 4] = _pv(f("conv_b")[l])
        vec[:, l, V_GAB:V_GAB + 8] = _pv(f("gate_a_b")[l]).reshape(128, 8)
        vec[:, l, V_GXB:V_GXB + 8] = _pv(f("gate_x_b")[l]).reshape(128, 8)
        vec[:, l, V_LAM:V_LAM + 8] = _pv(f("lru_lambda")[l]).reshape(128, 8)
        vec[:, l, V_PB:V_PB + 4] = _pv(f("pool_b")[l])
        vec[:, l, V_PS:V_PS + 4] = _pv(f("pool_scale")[l])
        vec[:, l, V_L1G:V_L1G + 8] = _pv(f("ln1_g")[l])
        vec[:, l, V_L1B:V_L1B + 8] = _pv(f("ln1_b")[l])
        vec[:, l, V_L2G:V_L2G + 8] = _pv(f("ln2_g")[l])
        vec[:, l, V_L2B:V_L2B + 8] = _pv(f("ln2_b")[l])
        vec[:, l, V_B1:V_B1 + 512] = _pv(f("exp_b1")[l]).reshape(128, 512)
    gbd = np.zeros((DEPTH, 4, 128, 4, 128), np.float32)
    ga, gx = f("gate_a_w"), f("gate_x_w")
    for l in range(DEPTH):
        for d in range(2):
            for ax, gw in enumerate((ga, gx)):
                for hh in range(8):
                    cc, o = hh // 2, (hh % 2) * 64
                    gbd[l, cc, o:o + 64, d * 2 + ax, o:o + 64] = gw[l, d, hh]
    shared = {
        "posT": posT, "vecs": vec, "ident": np.eye(128, dtype=np.float32), "rb": f("router_b"), "rw": f("router_w"),
        "w_mod": f("w_mod"), "w_in": f("w_in"), "w_out": f("w_out"), "gbd": gbd, "pool_w": f("pool_w"),
        "rcnt": _rcnt_table(), "exp_w1": f("exp_w1"), "exp_w2": f("exp_w2"), "exp_b2": f("exp_b2"),
    }
    in_maps = []
    for b in range(B):
        m = dict(shared)
        m["xT"] = _fm(np.concatenate([ctx[b], x[b]], axis=0))
        cc = np.stack([c[b], c_ctx], axis=-1)
        m["cT"] = np.ascontiguousarray(cc.reshape(NCH, 128, 2).transpose(1, 0, 2).reshape(128, 16))
        in_maps.append(m)
    return in_maps


def kernel(**inputs):
    in_maps = prep_inputs(inputs)
    if "nc" not in _NC_CACHE:
        _NC_CACHE["nc"] = build_program()
    nc = _NC_CACHE["nc"]
    res = run_bass_kernel_spmd(nc, in_maps, core_ids=list(range(len(in_maps))))
    outs = []
    for r in res.results:
        o = np.asarray(r["outT"], dtype=np.float32)
        outs.append(o.transpose(2, 1, 0).reshape(SEQ, D))
    return np.stack(outs, axis=0).astype(np.float32)
```

```python
import contextlib
import numpy as np
import concourse.bass as bass
import concourse.mybir as mybir
from concourse.bass_utils import run_bass_kernel_spmd

F32 = mybir.dt.float32
BF16 = mybir.dt.bfloat16
ALU = mybir.AluOpType
AF = mybir.ActivationFunctionType
AX = mybir.AxisListType

D = 1024
NCH = 8
T = 2304
CTX = 256
SEQ = 2048
DEPTH = 2
NE = 32
TL = [(0, 256, 1), (256, 512, 0), (768, 512, 0), (1280, 512, 0), (1792, 512, 0)]
ALPHA = float((2.0 * DEPTH) ** 0.25)
LN_EPS = 1e-5
S7 = float(1.0 / (1.0 + np.exp(-1.702 * 7.0)))
NV = 644
V_BMOD, V_CW, V_CB, V_GAB, V_GXB, V_LAM, V_PB, V_PS = 0, 48, 64, 68, 76, 84, 92, 96
V_L1G, V_L1B, V_L2G, V_L2B, V_B1 = 100, 108, 116, 124, 132
ND = 192 + 8 + 256 + 256 + 8
DV_MOD, DV_C8, DV_B1S, DV_B1B1, DV_C8X2 = 0, 192, 200, 456, 712
PLEN = 288 + 32 * 96
XRLEN = 2320
CLIP_ENG = "pool"
FINAL_ENG = "pool"


class Sched:
    ENGS = ("pe", "act", "dve", "pool", "sp")

    def __init__(self, nc, stack):
        self.nc = nc
        self.stack = stack
        self.q = {e: [] for e in self.ENGS}
        self.sems = {}
        self.cnt = {}
        self.known = {e: {} for e in self.ENGS}
        self.res = {}

    def sem(self, key):
        if key not in self.sems:
            name = "s_" + "_".join(str(k) for k in (key if isinstance(key, tuple) else (key,)))
            self.sems[key] = self.stack.enter_context(self.nc.semaphore(name))
        return self.sems[key]

    def _deps(self, R, W):
        deps = {}

        def add(k, c):
            if deps.get(k, 0) < c:
                deps[k] = c

        for r in R:
            ent = self.res.get(r)
            if ent and ent[0]:
                add(*ent[0])
        for w in W:
            ent = self.res.get(w)
            if ent:
                if ent[0]:
                    add(*ent[0])
                for k, c in ent[1].items():
                    add(k, c)
        return deps

    def _commit(self, tok, R, W):
        for r in R:
            ent = self.res.setdefault(r, [None, {}])
            if ent[1].get(tok[0], 0) < tok[1]:
                ent[1][tok[0]] = tok[1]
        for w in W:
            self.res[w] = [tok, {}]

    def _waits(self, eng, deps):
        kn = self.known[eng]
        for k, c in deps.items():
            if k == eng and eng == "pe":
                continue
            if kn.get(k, 0) < c:
                self.sem(k)
                self.q[eng].append(("w", k, c))
                kn[k] = c

    def op(self, eng, fn, R=(), W=(), after=()):
        deps = self._deps(R, W)
        for k, c in after:
            if deps.get(k, 0) < c:
                deps[k] = c
        self._waits(eng, deps)
        self.sem(eng)
        self.cnt[eng] = self.cnt.get(eng, 0) + 1
        tok = (eng, self.cnt[eng])
        self.q[eng].append(("o", fn))
        self._commit(tok, R, W)
        return tok

    def dma(self, q, out, in_, R=(), W=(), key=None):
        self._waits(q, self._deps(R, W))
        self.sem(key)
        self.cnt[key] = self.cnt.get(key, 0) + 16
        tok = (key, self.cnt[key])
        self.q[q].append(("d", out, in_, key))
        self._commit(tok, R, W)
        return tok

    def barrier(self):
        snap = dict(self.cnt)
        for eng in self.ENGS:
            kn = self.known[eng]
            for k, c in snap.items():
                if k == eng:
                    continue
                if kn.get(k, 0) < c:
                    self.q[eng].append(("w", k, c))
                    kn[k] = c

    def replay(self, eng, h):
        for it in self.q[eng]:
            if it[0] == "w":
                h.wait_ge(self.sems[it[1]], it[2])
            elif it[0] == "o":
                it[1](h).then_inc(self.sems[eng], 1)
            else:
                h.dma_start(out=it[1], in_=it[2]).then_inc(self.sems[it[3]], 16)


def build_program(stop=None):
    nc = bass.Bass("TRN2", target_bir_lowering=False)
    dram = {}

    def din(name, shape):
        dram[name] = nc.dram_tensor(name, list(shape), F32, kind="ExternalInput").ap()
        return dram[name]

    xT = din("xT", [128, NCH, T])
    posT = din("posT", [128, NCH, T])
    cT = din("cT", [128, 16])
    vecs = din("vecs", [128, DEPTH, NV])
    ident_d = din("ident", [128, 128])
    rb_d = din("rb", [DEPTH, NE])
    rw_d = din("rw", [DEPTH, D, NE])
    w_mod = din("w_mod", [DEPTH, D, 6 * D])
    w_in = din("w_in", [DEPTH, D, 1536])
    w_out = din("w_out", [DEPTH, D, D])
    gbd = din("gbd", [DEPTH, 4, 128, 4, 128])
    pool_w = din("pool_w", [DEPTH, 4, 128, 128])
    rcnt_d = din("rcnt", [4, PLEN])
    w1_d = din("exp_w1", [DEPTH, NE, D, 2 * D])
    w2_d = din("exp_w2", [DEPTH, NE, D, D])
    b2_d = din("exp_b2", [DEPTH, NE, D])
    outT = nc.dram_tensor("outT", [128, NCH, SEQ], F32, kind="ExternalOutput").ap()
    gscr = nc.dram_tensor("gscr", [DEPTH, NE, T], F32, kind="Internal").ap()
    dbg_out = {}

    stack = contextlib.ExitStack()
    with stack:
        NPERS = 18432 + 9216 + DEPTH * NV + DEPTH * ND + 128 + 128 + 32 * DEPTH + 16 + 8 + 64
        NPH = 21000
        AR = stack.enter_context(nc.sbuf_tensor("arena", [128, NPERS + NPH], F32))
        ps = [stack.enter_context(nc.psum_tensor(f"ps{i}", [128, 512], F32)) for i in range(8)]
        S = Sched(nc, stack)

        off = [0]

        def palloc(n):
            o = off[0]
            off[0] += n
            return AR[:, o:o + n]

        X2 = palloc(18432)
        X = X2.rearrange("p (c t) -> p c t", c=NCH)
        H = palloc(9216).bitcast(BF16).rearrange("p (c t) -> p c t", c=NCH)
        VEC = palloc(DEPTH * NV).rearrange("p (l v) -> p l v", l=DEPTH)
        DV = palloc(DEPTH * ND).rearrange("p (l v) -> p l v", l=DEPTH)
        ONES = palloc(128)
        IDENT = palloc(128)
        RB = palloc(32 * DEPTH).rearrange("p (l e) -> p l e", l=DEPTH)
        CS = palloc(16)
        CBF = palloc(8).bitcast(BF16).rearrange("p (k s) -> p k s", s=2)
        ONE1 = palloc(8)
        PH0 = off[0]
        assert off[0] <= NPERS

        def phase_alloc():
            o = [PH0]

            def a(n):
                r = AR[:, o[0]:o[0] + n]
                o[0] += n
                assert o[0] <= NPERS + NPH, (o[0], NPERS + NPH)
                return r
            return a

        def vcol(l, j):
            return VEC[:, l, j:j + 1]

        def dcol(l, j):
            return DV[:, l, j:j + 1]

        def modc(l, which, c, s):
            return dcol(l, DV_MOD + (which * 8 + c) * 2 + s)

        def ts(eng, out, in0, s1, s2, op0, op1, R, W):
            if op1 is None:
                return S.op(eng, lambda h: h.tensor_scalar(out=out, in0=in0, scalar1=s1, scalar2=None, op0=op0), R, W)
            return S.op(eng, lambda h: h.tensor_scalar(out=out, in0=in0, scalar1=s1, scalar2=s2, op0=op0, op1=op1), R, W)

        def stt(out, in0, sc, in1, op0, op1, R, W):
            return S.op("dve", lambda h: h.scalar_tensor_tensor(out=out, in0=in0, scalar=sc, in1=in1, op0=op0, op1=op1), R, W)

        def tt(out, in0, in1, op, R, W, eng="dve"):
            return S.op(eng, lambda h: h.tensor_tensor(out=out, in0=in0, in1=in1, op=op), R, W)

        def act(out, in_, func, R, W, bias=None, scale=None, after=()):
            kw = {}
            if bias is not None:
                kw["bias"] = bias
            if scale is not None:
                kw["scale"] = scale
            return S.op("act", lambda h: h.activation(out=out, in_=in_, func=func, **kw), R, W, after)

        def opm(eng, method, R, W, *a, **k):
            return S.op(eng, lambda h: getattr(h, method)(*a, **k), R, W)

        def mm1(out, lhsT, rhs, start, stop, R, W):
            return S.op("pe", lambda h: h.matmul(out, lhsT=lhsT, rhs=rhs, start=start, stop=stop), R, W)

        def mm_group(out, pairs, R, W):
            n = len(pairs)

            def fn(h):
                inst = None
                for i, (l_, r_) in enumerate(pairs):
                    inst = h.matmul(out, lhsT=l_, rhs=r_, start=(i == 0), stop=(i == n - 1))
                return inst
            return S.op("pe", fn, R, W)

        S.dma("sp", VEC, vecs, W=["VEC"], key="ld_vec")
        S.dma("sp", IDENT, ident_d, W=["IDENT"], key="ld_ident")
        S.dma("sp", CS, cT, W=["CS"], key="ld_cs")
        for l in range(DEPTH):
            S.dma("sp", RB[:, l, :], rb_d[l:l + 1, :].broadcast_to([128, NE]), W=[("RB", l)], key=("ld_rb", l))
        opm("dve", "memset", [], ["ONES"], ONES, 1.0 / D)
        opm("dve", "memset", [], ["ONE1"], ONE1, 1.0)
        act(CBF, CS.rearrange("p (k s) -> p k s", s=2), AF.Silu, R=["CS"], W=["CBF"])

        WMB = NPERS + NPH - 256 - 4096
        WM = [AR[:, WMB + i * 2048:WMB + (i + 1) * 2048].bitcast(BF16).rearrange("p (k f) -> p k f", k=8) for i in range(2)]

        def adaln(l):
            psm = ps[2 + l]
            for n in range(12):
                slot = (l * 12 + n) % 2
                S.dma("pool", WM[slot], w_mod[l, :, n * 512:(n + 1) * 512].rearrange("(k p) f -> p k f", p=128),
                      W=[("WM", slot)], key=("ld_wm", slot))
                for qq in range(4):
                    j = n * 4 + qq
                    mm_group(psm[:, j * 2:j * 2 + 2],
                             [(WM[slot][:, kc, qq * 128:(qq + 1) * 128], CBF[:, kc, :]) for kc in range(8)],
                             R=[("WM", slot), "CBF"], W=[("psm", l, j)])
            adaln_finish(l, psm)

        def adaln_finish(l, psm):
            psv = psm[:, 0:96].rearrange("p (j s) -> p j s", s=2)
            dvm = DV[:, l, DV_MOD:DV_MOD + 96].rearrange("p (j s) -> p j s", s=2)
            allj = [("psm", l, j) for j in range(48)]
            for s in range(2):
                tt(dvm[:, :, s], psv[:, :, s], VEC[:, l, V_BMOD:V_BMOD + 48], ALU.add, R=allj + ["VEC"], W=[("DV", l)])
            for which in (1, 4):
                o = DV_MOD + which * 16
                ts("dve", DV[:, l, o:o + 16], DV[:, l, o:o + 16], 1.0, None, ALU.add, None, R=[("DV", l)], W=[("DV", l)])
            c8 = DV[:, l, DV_C8:DV_C8 + 8]
            act(c8, VEC[:, l, V_LAM:V_LAM + 8], AF.Exp, R=["VEC"], W=[("DV", l)], scale=-1.0)
            ts("dve", c8, c8, 1.0, None, ALU.add, None, R=[("DV", l)], W=[("DV", l)])
            act(c8, c8, AF.Ln, R=[("DV", l)], W=[("DV", l)])
            ts("dve", c8, c8, -8.0, None, ALU.mult, None, R=[("DV", l)], W=[("DV", l)])
            b1v = VEC[:, l, V_B1:V_B1 + 512].rearrange("p (e j) -> p e j", j=16)
            ts("dve", DV[:, l, DV_B1S:DV_B1S + 256].rearrange("p (e j) -> p e j", j=8), b1v[:, :, 0:8], 1.702, None,
               ALU.mult, None, R=["VEC"], W=[("DV", l)])
            ts("dve", DV[:, l, DV_B1B1:DV_B1B1 + 256].rearrange("p (e j) -> p e j", j=8), b1v[:, :, 8:16], 1.0, None,
               ALU.add, None, R=["VEC"], W=[("DV", l)])
            ts("dve", DV[:, l, DV_C8X2:DV_C8X2 + 8], c8, 2.0, None, ALU.mult, None, R=[("DV", l)], W=[("DV", l)])


        def ln_phase(tiles, l_aff, aff_base, l_mod, which_sh, final, router_l=None, entry=False):
            pa = phase_alloc()
            GT = pa(T) if True else None
            SQ = [pa(512) for _ in range(2)]
            M2 = pa(512)
            TMP = [pa(512) for _ in range(8)]
            RSTDS = pa(512)
            H32 = pa(4096).rearrange("p (c t) -> p c t", c=8) if router_l is not None else None
            POSB = [pa(512) for _ in range(2)] if entry else None
            LGS = pa(256)
            RT = pa(256)
            B2 = pa(1024) if router_l is not None else None
            ksq = [0]
            def stats_a(ti):
                t0, n, s = tiles[ti]
                tk = t0
                if entry:
                    S.dma("sp", X[:, :, t0:t0 + n], xT[:, :, t0:t0 + n], W=[("X", c, tk) for c in range(NCH)], key=("ld_x", ti))
                    for c in range(NCH):
                        if s == 0:
                            pb = POSB[c % 2]
                            S.dma("sp", pb[:, 0:n], posT[:, c, t0:t0 + n], W=[("POSB", c % 2)], key=("ld_pos", c % 2))
                            tt(X[:, c, t0:t0 + n], X[:, c, t0:t0 + n], pb[:, 0:n], ALU.add,
                               R=[("X", c, tk), ("POSB", c % 2)], W=[("X", c, tk)])
                s1, s2 = ps[0], ps[1]
                for c in range(NCH):
                    sl = ksq[0] % 2
                    ksq[0] += 1
                    act(SQ[sl][:, 0:n], X[:, c, t0:t0 + n], AF.Square, R=[("X", c, tk)], W=[("SQ", sl)])
                    mm1(s1[:, 0:n], ONES, X[:, c, t0:t0 + n], c == 0, c == NCH - 1, R=[("X", c, tk), "ONES"], W=["ps0"])
                    mm1(s2[:, 0:n], ONES, SQ[sl][:, 0:n], c == 0, c == NCH - 1, R=[("SQ", sl), "ONES"], W=["ps1"])

            def stats_b(ti):
                t0, n, s = tiles[ti]
                s1, s2 = ps[0], ps[1]
                act(M2[:, 0:n], s1[:, 0:n], AF.Square, R=["ps0"], W=["M2"])
                stt(M2[:, 0:n], s2[:, 0:n], LN_EPS, M2[:, 0:n], ALU.add, ALU.subtract, R=["ps1", "M2"], W=["M2"])
                act(M2[:, 0:n], M2[:, 0:n], AF.Sqrt, R=["M2"], W=["M2"])
                rb_, nb_ = 4 + ti % 2, 6 + ti % 2
                RSTD, NMR = ps[rb_], ps[nb_]
                krs, knm = f"ps{rb_}", f"ps{nb_}"
                opm("dve", "reciprocal", ["M2"], ["RSTDS"], out=RSTDS[:, 0:n], in_=M2[:, 0:n])
                stt(NMR[:, 0:n], s1[:, 0:n], -1.0, RSTDS[:, 0:n], ALU.mult, ALU.mult, R=["ps0", "RSTDS"], W=[knm])
                opm("act", "copy", ["RSTDS"], [krs], RSTD[:, 0:n], RSTDS[:, 0:n])

            stats_a(0)
            stats_b(0)
            for ti, (t0, n, s) in enumerate(tiles):
                tk = t0
                if ti + 1 < len(tiles):
                    stats_a(ti + 1)
                rb_, nb_ = 4 + ti % 2, 6 + ti % 2
                RSTD, NMR = ps[rb_], ps[nb_]
                krs, knm = f"ps{rb_}", f"ps{nb_}"
                tms = [TMP[c][:, 0:n] for c in range(NCH)]
                tks = [("TMP", c) for c in range(NCH)]
                for c in range(NCH):
                    tt(tms[c], X[:, c, t0:t0 + n], RSTD[:, 0:n], ALU.mult, R=[("X", c, tk), krs], W=[tks[c]])
                for c in range(NCH):
                    tt(tms[c], tms[c], NMR[:, 0:n], ALU.add, R=[tks[c], knm], W=[tks[c]])
                if l_aff is not None:
                    for c in range(NCH):
                        ts("dve", tms[c], tms[c], vcol(l_aff, aff_base + c), vcol(l_aff, aff_base + 8 + c), ALU.mult, ALU.add,
                           R=[tks[c], "VEC"], W=[tks[c]])
                for c in range(NCH):
                    if l_mod is not None:
                        sc_ = modc(l_mod, which_sh + 1, c, s)
                        sh_ = modc(l_mod, which_sh, c, s)
                        ts("pool", H[:, c, t0:t0 + n], tms[c], sc_, sh_, ALU.mult, ALU.add, R=[tks[c], ("DV", l_mod)], W=[("H", tk)])
                        if router_l is not None:
                            ts("pool", H32[:, c, 0:n], tms[c], sc_, sh_, ALU.mult, ALU.add, R=[tks[c], ("DV", l_mod)], W=["H32"])
                    opm("act", "mul", [tks[c]], [("X", c, tk)], X[:, c, t0:t0 + n], tms[c], 1.0 if final else ALPHA)
                if router_l is not None:
                    RW = router_l["RW"]
                    rl = router_l["l"]
                    nsub = n // 128
                    subs = list(range(nsub))
                    for k in subs:
                        mm_group(ps[2][:, k * 32:(k + 1) * 32], [(H32[:, kc, k * 128:(k + 1) * 128], RW[:, kc, :]) for kc in range(8)],
                                 R=["H32", "RW"], W=["ps2"])
                    lgs = [LGS[:, k * 64:k * 64 + 32] for k in subs]
                    m8s = [LGS[:, k * 64 + 32:k * 64 + 40] for k in subs]
                    nmxs = [LGS[:, k * 64 + 40:k * 64 + 41] for k in subs]
                    sms = [LGS[:, k * 64 + 41:k * 64 + 42] for k in subs]
                    msks = [RT[:, k * 64:k * 64 + 32] for k in subs]
                    ees = [RT[:, k * 64 + 32:k * 64 + 64] for k in subs]
                    for k in subs:
                        tt(lgs[k], ps[2][:, k * 32:(k + 1) * 32], RB[:, rl, :], ALU.add, R=["ps2", ("RB", rl)], W=[("LG", k)])
                    for k in subs:
                        opm("dve", "max", [("LG", k)], [("M8", k)], out=m8s[k], in_=lgs[k])
                    for k in subs:
                        ts("dve", nmxs[k], m8s[k][:, 0:1], -1.0, None, ALU.mult, None, R=[("M8", k)], W=[("NMX", k)])
                    for k in subs:
                        ts("dve", msks[k], lgs[k], m8s[k][:, 3:4], None, ALU.is_ge, None, R=[("LG", k), ("M8", k)], W=[("MSK", k)])
                    for k in subs:
                        act(ees[k], lgs[k], AF.Exp, R=[("LG", k), ("NMX", k)], W=[("EE", k)], bias=nmxs[k], scale=1.0)
                    for k in subs:
                        tt(ees[k], ees[k], msks[k], ALU.mult, R=[("MSK", k), ("EE", k)], W=[("EE", k)])
                    for k in subs:
                        opm("dve", "tensor_reduce", [("EE", k)], [("SM", k)], out=sms[k], in_=ees[k], axis=AX.X, op=ALU.add)
                    for k in subs:
                        opm("dve", "reciprocal", [("SM", k)], [("SM", k)], out=sms[k], in_=sms[k])
                    for k in subs:
                        ts("dve", ees[k], ees[k], sms[k], None, ALU.mult, None, R=[("EE", k), ("SM", k)], W=[("EE", k)])
                    for k in subs:
                        opm("pe", "transpose", [("EE", k), "IDENT"], ["ps3"], ps[3][0:32, k * 128:(k + 1) * 128], ees[k], IDENT)
                    opm("act", "copy", ["ps3"], ["GT"], GT[0:32, t0:t0 + n], ps[3][0:32, 0:n])
                if ti + 1 < len(tiles):
                    stats_b(ti + 1)
            if router_l is not None:
                lr = router_l["l"]
                S.dma("sp", gscr[lr], GT[0:32, :], R=["GT"], W=["GSCR"], key="st_gscr")
                S.dma("sp", B2[0:32, :], b2_d[lr], W=["B2"], key="ld_b2")
                for ti, (t0, n, s) in enumerate(tiles):
                    for dch in range(NCH):
                        b = 4 + dch % 4
                        mm_group(ps[b][:, 0:n], [(B2[0:32, dch * 128:(dch + 1) * 128], GT[0:32, t0:t0 + n])], R=["B2", "GT"], W=[f"ps{b}"])
                        stt(X[:, dch, t0:t0 + n], ps[b][:, 0:n], modc(lr, 5, dch, s), X[:, dch, t0:t0 + n], ALU.mult, ALU.add,
                            R=[f"ps{b}", ("X", dch, t0), ("DV", lr)], W=[("X", dch, t0)])
            return GT

        def mixer_phase(l):
            pa = phase_alloc()
            U = pa(XRLEN)
            RA = pa(XRLEN)
            IB = pa(XRLEN)
            QH = [pa(XRLEN), pa(XRLEN)]
            UBF = pa(1152).bitcast(BF16)
            MIXC = [pa(1152).bitcast(BF16) for _ in range(4)]
            WI = [pa(512).bitcast(BF16).rearrange("p (k f) -> p k f", k=8) for _ in range(2)]
            WO = [pa(512).bitcast(BF16) for _ in range(4)]
            GB = [pa(256).bitcast(BF16).rearrange("p (m f) -> p m f", m=4) for _ in range(2)]
            mtiles = TL if l < DEPTH - 1 else TL[1:]
            wi_n = [0]
            wo_n = [0]

            def load_wi(j):
                slot = wi_n[0] % 2
                wi_n[0] += 1
                S.dma("pool", WI[slot], w_in[l, :, j * 128:(j + 1) * 128].rearrange("(k p) f -> p k f", p=128),
                      W=[("WI", slot)], key=("ld_wi", slot))
                return slot

            def load_wo(kc):
                slot = wo_n[0] % len(WO)
                wo_n[0] += 1
                S.dma("pool", WO[slot], w_out[l, kc * 128:(kc + 1) * 128, :], W=[("WO", slot)], key=("ld_wo", slot))
                return slot

            def zmm(out, slot, t0, n, R, W):
                return mm_group(out, [(WI[slot][:, kc, :], H[:, kc, t0:t0 + n]) for kc in range(8)],
                                R=[("WI", slot), ("H", t0)] + R, W=W)

            def wout_acc_pair(MX, slots):
                for ti, (t0, n, s) in enumerate(mtiles):
                    for dch in range(NCH):
                        b = 4 + (dch % 4)
                        mm_group(ps[b][:, 0:n], [(WO[slots[i]][:, dch * 128:(dch + 1) * 128], MX[i][:, t0:t0 + n]) for i in range(len(MX))],
                                 R=[("WO", slots[i]) for i in range(len(MX))] + [("MIXC", i, t0) for i in range(len(MX))], W=[f"ps{b}"])
                        stt(X[:, dch, t0:t0 + n], ps[b][:, 0:n], modc(l, 2, dch, s), X[:, dch, t0:t0 + n], ALU.mult, ALU.add,
                            R=[f"ps{b}", ("X", dch, t0), ("DV", l)], W=[("X", dch, t0)])

            woslots = [None, None, None, None]
            for c in range(4):
                sx = load_wi(c)
                sy = load_wi(4 + c)
                gslot = c % 2
                S.dma("pool", GB[gslot], gbd[l, c], W=[("GB", gslot)], key=("ld_gb", gslot))
                woslots[c] = load_wo(c)
                XR = QH[0]
                for (a, b) in ((0, 4), (260, 264), (2312, 2316)):
                    opm("dve", "memset", [], [("QH", 0)], XR[:, a:b], 0.0)
                for ti, (t0, n, s) in enumerate(TL):
                    b = ti % 2
                    zmm(ps[b][:, 0:n], sx, t0, n, [], [f"ps{b}"])
                    po = t0 + 4 if s == 1 else t0 + 8
                    opm("act", "copy", [f"ps{b}"], [("QH", 0)], XR[:, po:po + n], ps[b][:, 0:n])
                for (u0, un, xb) in ((0, 256, 2), (256, 2048, 262)):
                    for k in range(4):
                        wk = vcol(l, V_CW + c * 4 + k)
                        src = XR[:, xb + k:xb + k + un]
                        if k == 0:
                            ts("dve", U[:, u0:u0 + un], src, wk, vcol(l, V_CB + c), ALU.mult, ALU.add,
                               R=[("QH", 0), "VEC"], W=["U"])
                        else:
                            stt(U[:, u0:u0 + un], src, wk, U[:, u0:u0 + un], ALU.mult, ALU.add, R=[("QH", 0), "U", "VEC"], W=["U"])
                opm("act", "copy", ["U"], ["UBF"], UBF[:, 0:T], U[:, 0:T])
                for d in range(2):
                    Q = QH[d]
                    for ti, (t0, n, s) in enumerate(TL):
                        ba, bx = 2 * (ti % 2), 2 * (ti % 2) + 1
                        mm_group(ps[ba][:, 0:n], [(GB[gslot][:, d * 2 + 0, :], UBF[:, t0:t0 + n])], R=[("GB", gslot), "UBF"], W=[f"ps{ba}"])
                        mm_group(ps[bx][:, 0:n], [(GB[gslot][:, d * 2 + 1, :], UBF[:, t0:t0 + n])], R=[("GB", gslot), "UBF"], W=[f"ps{bx}"])
                        act(RA[:, t0:t0 + n], ps[ba][:, 0:n], AF.Sigmoid, R=[f"ps{ba}", "VEC"], W=["RA"] + [("T1", tj) for tj in range(5)],
                            bias=vcol(l, V_GAB + d * 4 + c))
                        act(IB[:, t0:t0 + n], ps[bx][:, 0:n], AF.Sigmoid, R=[f"ps{bx}", "VEC"], W=["IB"] + [("T2", tj) for tj in range(5)],
                            bias=vcol(l, V_GXB + d * 4 + c))
                    act(Q[:, 0:T], RA[:, 0:T], AF.Exp, R=["RA", ("DV", l)], W=[("QH", d)], scale=dcol(l, DV_C8X2 + d * 4 + c))
                    act(RA[:, 0:T], RA[:, 0:T], AF.Exp, R=["RA", ("DV", l)], W=["RA"], scale=dcol(l, DV_C8 + d * 4 + c))
                    act(Q[:, 0:T], Q[:, 0:T], AF.Sqrt, R=[("QH", d), "ONE1"], W=[("QH", d)], scale=-1.0, bias=ONE1[:, 0:1])
                    tt(IB[:, 0:T], IB[:, 0:T], U[:, 0:T], ALU.mult, R=["IB", "U"], W=["IB"])
                    stt(IB[:, 0:T], Q[:, 0:T], 0.0, IB[:, 0:T], ALU.max, ALU.mult, R=["IB", ("QH", d)], W=["IB"])
                    if d == 0:
                        opm("dve", "tensor_tensor_scan", ["RA", "IB"], [("QH", d)], out=Q[:, 0:T], data0=RA[:, 0:T],
                            data1=IB[:, 0:T], initial=0.0, op0=ALU.mult, op1=ALU.add)
                    else:
                        opm("dve", "tensor_tensor_scan", ["RA", "IB"], [("QH", d)], out=Q[:, 0:CTX][:, ::-1],
                            data0=RA[:, 0:CTX][:, ::-1], data1=IB[:, 0:CTX][:, ::-1], initial=0.0, op0=ALU.mult, op1=ALU.add)
                        scan_tok = opm("dve", "tensor_tensor_scan", ["RA", "IB", ("QH", d)], [("QH", d)], out=Q[:, CTX:T][:, ::-1],
                            data0=RA[:, CTX:T][:, ::-1], data1=IB[:, CTX:T][:, ::-1], initial=Q[:, 0:1], op0=ALU.mult, op1=ALU.add)
                for ti, (t0, n, s) in enumerate(mtiles):
                    b = ti % 2
                    pY = ps[b][:, 0:n]
                    zmm(pY, sy, t0, n, [], [f"ps{b}"])
                    t1 = RA[:, t0:t0 + n]
                    t2 = IB[:, t0:t0 + n]
                    k1, k2 = ("T1", ti), ("T2", ti)
                    act(t1, pY, AF.Square, R=[f"ps{b}"], W=[k1], after=[scan_tok], scale=0.21145921592590276)
                    stt(t1, t1, 1.0, pY, ALU.add, ALU.mult, R=[k1, f"ps{b}"], W=[k1])
                    act(t1, t1, AF.Sigmoid, R=[k1], W=[k1], scale=1.5957691216057308)
                    tt(t1, t1, pY, ALU.mult, R=[k1, f"ps{b}"], W=[k1])
                    tt(t2, QH[0][:, t0:t0 + n], QH[1][:, t0:t0 + n], ALU.add, R=[("QH", 0), ("QH", 1)], W=[k2], eng="pool")
                    tt(MIXC[c][:, t0:t0 + n], t1, t2, ALU.mult, R=[k1, k2], W=[("MIXC", c, t0)])
                if c == 3:
                    wout_acc_pair(MIXC, woslots)
            S.barrier()
            pa = phase_alloc()
            P0 = pa(PLEN)
            P1 = pa(PLEN)
            P2 = pa(PLEN)
            RC = pa(PLEN)
            DD = pa(1152).bitcast(BF16)
            MIXC = [pa(1152).bitcast(BF16) for _ in range(2)]
            WI = [pa(512).bitcast(BF16).rearrange("p (k f) -> p k f", k=8) for _ in range(2)]
            WO = [pa(512).bitcast(BF16) for _ in range(2)]
            PW = [pa(64).bitcast(BF16) for _ in range(2)]

            def lat3(P):
                return P[:, 288:PLEN].rearrange("p (r w) -> p r w", w=96)[:, :, 16:80]

            for g in range(4):
                sx = load_wi(8 + g)
                woslots[g % 2] = load_wo(4 + g)
                pslot = g % 2
                S.dma("pool", PW[pslot], pool_w[l, g], W=[("PW", pslot)], key=("ld_pw", pslot))
                S.dma("sp", RC, rcnt_d[g:g + 1, :].broadcast_to([128, PLEN]), W=["RC"], key="ld_rc")
                opm("dve", "memset", [], ["P0"], P0, 0.0)
                for ti, (t0, n, s) in enumerate(TL):
                    b = ti % 2
                    zmm(ps[b][:, 0:n], sx, t0, n, [], [f"ps{b}"])
                    if s == 1:
                        opm("act", "copy", [f"ps{b}"], ["P0"], P0[:, 16:272], ps[b][:, 0:256])
                    else:
                        r0 = (t0 - CTX) // 64
                        opm("act", "copy", [f"ps{b}"], ["P0"], lat3(P0)[:, r0:r0 + 8, :],
                            ps[b][:, 0:512].rearrange("p (r w) -> p r w", w=64))
                src, sname = P0, "P0"
                dsts = [(P1, "P1"), (P2, "P2")]
                for i in range(g + 1):
                    dst, dname = dsts[i % 2]
                    if i == 0:
                        tt(dst[:, 1:PLEN], src[:, 0:PLEN - 1], src[:, 1:PLEN], ALU.add, R=[sname], W=[dname])
                    else:
                        hw = 1 << (i - 1)
                        tt(dst[:, hw:PLEN - hw], src[:, 0:PLEN - 2 * hw], src[:, 2 * hw:PLEN], ALU.add, R=[sname], W=[dname])
                    src, sname = dst, dname
                oth, oname = dsts[(g + 1) % 2]
                tt(oth[:, 16:PLEN - 16], src[:, 16:PLEN - 16], RC[:, 16:PLEN - 16], ALU.mult, R=[sname, "RC"], W=[oname])
                tt(DD[:, 0:256], oth[:, 16:272], P0[:, 16:272], ALU.subtract, R=[oname, "P0"], W=["DD"])
                tt(DD[:, 256:T].rearrange("p (r w) -> p r w", w=64), lat3(oth), lat3(P0), ALU.subtract, R=[oname, "P0"], W=["DD"])
                for ti, (t0, n, s) in enumerate(mtiles):
                    b = 2 + ti % 2
                    mm_group(ps[b][:, 0:n], [(PW[pslot], DD[:, t0:t0 + n])], R=[("PW", pslot), "DD"], W=[f"ps{b}"])
                    ts("dve", MIXC[g % 2][:, t0:t0 + n], ps[b][:, 0:n], vcol(l, V_PB + g), vcol(l, V_PS + g), ALU.add, ALU.mult,
                       R=[f"ps{b}", "VEC"], W=[("MIXC", g % 2, t0)])
                if g % 2 == 1:
                    wout_acc_pair(MIXC, woslots[0:2])
            S.barrier()

        W1F = AR[:, PH0 + 14400:PH0 + 18496].bitcast(BF16).rearrange("p (k f) -> p k f", k=8)
        W2F = AR[:, PH0 + 18496:PH0 + 20544].bitcast(BF16).rearrange("p (k f) -> p k f", k=4)

        def moe_prefetch(l):
            for half, c0 in ((0, 0), (1, 1024)):
                S.dma("pool", W1F[:, :, half * 512:(half + 1) * 512],
                      w1_d[l, 0, :, c0:c0 + 512].rearrange("(k p) f -> p k f", p=128), W=[("W1", 0, half)], key=("ld_w1", 0, half))
            S.dma("pool", W2F, w2_d[l, 0, 0:512, :].rearrange("(k p) f -> p k f", p=128), W=[("W2", 0)], key=("ld_w2", 0))

        def moe_phase(l, GT_unused):
            pa = phase_alloc()
            W1 = [W1F, pa(4096).bitcast(BF16).rearrange("p (k f) -> p k f", k=8)]
            W2 = [W2F, pa(2048).bitcast(BF16).rearrange("p (k f) -> p k f", k=4)]
            ACTB = [pa(1024).bitcast(BF16).rearrange("p (k t) -> p k t", k=4) for _ in range(2)]
            SB = [pa(512) for _ in range(2)]
            TB = [pa(512) for _ in range(2)]
            GB_ = [pa(512) for _ in range(2)]
            GS = [pa(512) for _ in range(3)]
            mtiles = (TL[1:] + TL[:1]) if l < DEPTH - 1 else TL[1:]
            embed = (l + 1 < DEPTH)
            WMS = [pa(512).bitcast(BF16).rearrange("p (k f) -> p k f", k=8) for _ in range(2)] if embed else None
            nob = 3 if embed else 4
            step = [0]

            def adaln_step():
                k = step[0]
                step[0] += 1
                if 1 <= k <= 48:
                    j = k - 1
                    mm_group(ps[7][:, j * 2:j * 2 + 2], [(WMS[j % 2][:, kc, :], CBF[:, kc, :]) for kc in range(8)],
                             R=[("WMS", j % 2), "CBF"], W=[("psm", l + 1, j)])
                    if j == 47:
                        adaln_finish(l + 1, ps[7])
                if k < 48:
                    S.dma("pool", WMS[k % 2], w_mod[l + 1, :, k * 128:(k + 1) * 128].rearrange("(k p) f -> p k f", p=128),
                          W=[("WMS", k % 2)], key=("ld_wms", k % 2))
            pk = [0]
            ok = [0]
            gk = [0]
            ak = [0]
            NU = 2 * NE

            def load_w1(uidx, l=l):
                e, hf = uidx // 2, uidx % 2
                ws = uidx % 2
                for half, c0 in ((0, hf * 512), (1, 1024 + hf * 512)):
                    S.dma("pool", W1[ws][:, :, half * 512:(half + 1) * 512],
                          w1_d[l, e, :, c0:c0 + 512].rearrange("(k p) f -> p k f", p=128), W=[("W1", ws, half)], key=("ld_w1", ws, half))

            def load_w2(uidx):
                e, hf = uidx // 2, uidx % 2
                ws = uidx % 2
                S.dma("pool", W2[ws], w2_d[l, e, hf * 512:(hf + 1) * 512, :].rearrange("(k p) f -> p k f", p=128),
                      W=[("W2", ws)], key=("ld_w2", ws))

            def stage2(uidx, tile, aslot):
                t0, n, s = tile
                ws = uidx % 2
                for dch in range(NCH):
                    b = 4 + ok[0] % nob
                    ok[0] += 1
                    mm_group(ps[b][:, 0:n], [(W2[ws][:, fc, dch * 128:(dch + 1) * 128], ACTB[aslot][:, fc, 0:n]) for fc in range(4)],
                             R=[("W2", ws), ("ACTB", aslot)], W=[f"ps{b}"])
                    stt(X[:, dch, t0:t0 + n], ps[b][:, 0:n], modc(l, 5, dch, s), X[:, dch, t0:t0 + n], ALU.mult, ALU.add,
                        R=[f"ps{b}", ("X", dch, t0), ("DV", l)], W=[("X", dch, t0)])

            prev = None
            for uidx in range(NU):
                e, hf = uidx // 2, uidx % 2
                ws = uidx % 2
                if uidx + 1 < NU:
                    load_w1(uidx + 1)
                for ti, tile in enumerate(mtiles):
                    t0, n, s = tile
                    if embed:
                        adaln_step()
                    gs = gk[0] % 3
                    gk[0] += 1
                    S.dma("sp", GS[gs][:, 0:n], gscr[l, e:e + 1, t0:t0 + n].broadcast_to([128, n]), R=["GSCR"], W=[("GS", gs)], key=("ld_gs", gs))
                    aslot = ak[0] % 2
                    ak[0] += 1
                    for j in range(4):
                        sl = pk[0] % 2
                        pk[0] += 1
                        pA, pB = ps[2 * sl], ps[2 * sl + 1]
                        ka, kb = f"ps{2 * sl}", f"ps{2 * sl + 1}"
                        mm_group(pA[:, 0:n], [(W1[ws][:, kc, j * 128:(j + 1) * 128], H[:, kc, t0:t0 + n]) for kc in range(8)],
                                 R=[("W1", ws, 0), ("H", t0)], W=[ka])
                        mm_group(pB[:, 0:n], [(W1[ws][:, kc, 512 + j * 128:512 + (j + 1) * 128], H[:, kc, t0:t0 + n]) for kc in range(8)],
                                 R=[("W1", ws, 1), ("H", t0)], W=[kb])
                        cj = e * 8 + hf * 4 + j
                        sb, tb, ab = SB[sl][:, 0:n], TB[sl][:, 0:n], GB_[sl][:, 0:n]
                        act(sb, pA[:, 0:n], AF.Sigmoid, R=[ka, ("DV", l)], W=[("SB", sl)], bias=dcol(l, DV_B1S + cj), scale=1.702)
                        act(ab, pA[:, 0:n], AF.Identity, R=[ka, "VEC"], W=[("GG", sl)], bias=vcol(l, V_B1 + e * 16 + hf * 4 + j), scale=1.0)
                        act(tb, pB[:, 0:n], AF.Identity, R=[kb, ("DV", l)], W=[("TB", sl)], bias=dcol(l, DV_B1B1 + cj), scale=1.0)
                        stt(ab, ab, 7.0, GS[gs][:, 0:n], ALU.min, ALU.mult, R=[("GG", sl), ("GS", gs)], W=[("GG", sl)])
                        stt(ab, sb, S7, ab, ALU.min, ALU.mult, R=[("SB", sl), ("GG", sl)], W=[("GG", sl)])
                        ts(CLIP_ENG, tb, tb, 8.0, -6.0, ALU.min, ALU.max, R=[("TB", sl)], W=[("TB", sl)])
                        tt(ACTB[aslot][:, j, 0:n], ab, tb, ALU.mult, R=[("GG", sl), ("TB", sl)], W=[("ACTB", aslot)], eng=FINAL_ENG)
                    if prev is not None:
                        stage2(*prev)
                    if ti == 0 and uidx + 1 < NU:
                        load_w2(uidx + 1)
                    prev = (uidx, tile, aslot)
            stage2(*prev)
            S.barrier()

        stopped = [stop == "adaln"]

        def cp(name):
            if stop == name:
                stopped[0] = True
            return stopped[0]

        adaln(0)
        if stopped[0]:
            adaln(1)
            S.barrier()
        if not stopped[0]:
            ln_phase(TL, None, None, 0, 0, False, entry=True)
            S.barrier()
            cp("entry")
        for l in range(DEPTH):
            if stopped[0]:
                break
            last = l == DEPTH - 1
            mtiles = TL if not last else TL[1:]
            mixer_phase(l)
            if cp(f"mixer{l}"):
                break
            pa_tmp = phase_alloc()
            for _ in range(1):
                pass
            RW = AR[:, NPERS + NPH - 256:NPERS + NPH].rearrange("p (k e) -> p k e", k=8)
            S.dma("sp", RW, rw_d[l].rearrange("(k p) e -> p k e", p=128), W=["RW"], key="ld_rw")
            moe_prefetch(l)
            ln_phase(mtiles, l, V_L1G, l, 3, False, router_l={"RW": RW, "l": l})
            S.barrier()
            if cp(f"ln1_{l}"):
                break
            moe_phase(l, None)
            if cp(f"moe{l}"):
                break
            if not last:
                ln_phase(mtiles, l, V_L2G, l + 1, 0, False)
            else:
                ln_phase(mtiles, l, V_L2G, None, 0, True)
            S.barrier()
        if stop is not None:
            S.barrier()
            dX = nc.dram_tensor("dbgX", [128, NCH, T], F32, kind="ExternalOutput").ap()
            dH = nc.dram_tensor("dbgH", [128, NCH, T], BF16, kind="ExternalOutput").ap()
            dDV = nc.dram_tensor("dbgDV", [128, DEPTH, ND], F32, kind="ExternalOutput").ap()
            dGT = nc.dram_tensor("dbgGT", [32, T], F32, kind="ExternalOutput").ap()
            S.dma("sp", dX, X, key="st_out")
            S.dma("sp", dH, H, key="st_out")
            S.dma("sp", dDV, DV, key="st_out")
            S.dma("sp", dGT, AR[0:32, PH0:PH0 + T], key="st_out")
        for c in range(NCH):
            S.dma("sp", outT[:, c, :], X[:, c, CTX:T], R=[("X", c, t0) for (t0, n, s) in TL[1:]], key="st_out")
        nout = S.cnt["st_out"]
        S.q["sp"].append(("w", "st_out", nout))

        with nc.Block() as block:
            @block.tensor
            def _(h):
                S.replay("pe", h)

            @block.scalar
            def _(h):
                S.replay("act", h)

            @block.vector
            def _(h):
                S.replay("dve", h)

            @block.gpsimd
            def _(h):
                S.replay("pool", h)

            @block.sync
            def _(h):
                S.replay("sp", h)
    return nc


def _grid_sincos(rows, cols, d):
    quarter = d // 4
    omega = (1.0 / (np.float32(10000.0) ** (np.arange(quarter, dtype=np.float32) / np.float32(quarter)))).astype(np.float32)

    def emb1d(n):
        ang = np.arange(n, dtype=np.float32)[:, None] * omega[None, :]
        return np.concatenate([np.sin(ang), np.cos(ang)], axis=-1).astype(np.float32)

    er = np.broadcast_to(emb1d(rows)[:, None, :], (rows, cols, d // 2))
    ec = np.broadcast_to(emb1d(cols)[None, :, :], (rows, cols, d // 2))
    return np.concatenate([er, ec], axis=-1).reshape(rows * cols, d).astype(np.float32)


def _rcnt_table():
    tab = np.zeros((4, PLEN), np.float32)
    for g, win in enumerate((2, 4, 8, 16)):
        t = np.arange(CTX)
        lo = np.maximum(t - win // 2, 0)
        hi = np.minimum(t + win // 2, CTX)
        tab[g, 16:16 + CTX] = 1.0 / (hi - lo)
        t = np.arange(64)
        lo = np.maximum(t - win // 2, 0)
        hi = np.minimum(t + win // 2, 64)
        r = (1.0 / (hi - lo)).astype(np.float32)
        for row in range(32):
            tab[g, 288 + row * 96 + 16:288 + row * 96 + 80] = r
    return tab


def _fm(a):
    return np.ascontiguousarray(a.T.reshape(NCH, 128, a.shape[0]).transpose(1, 0, 2))


def _pv(v):
    n = v.shape[-1] // 128
    r = v.reshape(v.shape[:-1] + (n, 128))
    return np.moveaxis(r, -1, 0)


_NC_CACHE = {}


def prep_inputs(inp):
    f = lambda k: np.asarray(inp[k], dtype=np.float32)
    x, c, ctx, c_ctx = f("x"), f("c"), f("ctx"), f("c_ctx")
    B = x.shape[0]
    pos = _grid_sincos(SEQ // 64, 64, D)
    posT = np.zeros((128, NCH, T), np.float32)
    posT[:, :, CTX:] = _fm(pos)
    vec = np.zeros((128, DEPTH, NV), np.float32)
    for l in range(DEPTH):
        vec[:, l, V_BMOD:V_BMOD + 48] = _pv(f("b_mod")[l])
        vec[:, l, V_CW:V_CW + 16] = np.moveaxis(_pv(f("conv_w")[l]), 1, 2).reshape(128, 16)
        vec[:, l, V_CB:V_CB + 4] = _pv(f("conv_b")[l])
        vec[:, l, V_GAB:V_GAB + 8] = _pv(f("gate_a_b")[l]).reshape(128, 8)
        vec[:, l, V_GXB:V_GXB + 8] = _pv(f("gate_x_b")[l]).reshape(128, 8)
        vec[:, l, V_LAM:V_LAM + 8] = _pv(f("lru_lambda")[l]).reshape(128, 8)
        vec[:, l, V_PB:V_PB + 4] = _pv(f("pool_b")[l])
        vec[:, l, V_PS:V_PS + 4] = _pv(f("pool_scale")[l])
        vec[:, l, V_L1G:V_L1G + 8] = _pv(f("ln1_g")[l])
        vec[:, l, V_L1B:V_L1B + 8] = _pv(f("ln1_b")[l])
        vec[:, l, V_L2G:V_L2G + 8] = _pv(f("ln2_g")[l])
        vec[:, l, V_L2B:V_L2B + 8] = _pv(f("ln2_b")[l])
        vec[:, l, V_B1:V_B1 + 512] = _pv(f("exp_b1")[l]).reshape(128, 512)
    gbd = np.zeros((DEPTH, 4, 128, 4, 128), np.float32)
    ga, gx = f("gate_a_w"), f("gate_x_w")
    for l in range(DEPTH):
        for d in range(2):
            for ax, gw in enumerate((ga, gx)):
                for hh in range(8):
                    cc, o = hh // 2, (hh % 2) * 64
                    gbd[l, cc, o:o + 64, d * 2 + ax, o:o + 64] = gw[l, d, hh]
    shared = {
        "posT": posT, "vecs": vec, "ident": np.eye(128, dtype=np.float32), "rb": f("router_b"), "rw": f("router_w"),
        "w_mod": f("w_mod"), "w_in": f("w_in"), "w_out": f("w_out"), "gbd": gbd, "pool_w": f("pool_w"),
        "rcnt": _rcnt_table(), "exp_w1": f("exp_w1"), "exp_w2": f("exp_w2"), "exp_b2": f("exp_b2"),
    }
    in_maps = []
    for b in range(B):
        m = dict(shared)
        m["xT"] = _fm(np.concatenate([ctx[b], x[b]], axis=0))
        cc = np.stack([c[b], c_ctx], axis=-1)
        m["cT"] = np.ascontiguousarray(cc.reshape(NCH, 128, 2).transpose(1, 0, 2).reshape(128, 16))
        in_maps.append(m)
    return in_maps


def kernel(**inputs):
    in_maps = prep_inputs(inputs)
    if "nc" not in _NC_CACHE:
        _NC_CACHE["nc"] = build_program()
    nc = _NC_CACHE["nc"]
    res = run_bass_kernel_spmd(nc, in_maps, core_ids=list(range(len(in_maps))))
    outs = []
    for r in res.results:
        o = np.asarray(r["outT"], dtype=np.float32)
        outs.append(o.transpose(2, 1, 0).reshape(SEQ, D))
    return np.stack(outs, axis=0).astype(np.float32)
```
